# Optimizing a Trainium2 kernel written in Bass

```python
import jax, jax.numpy as jnp
from jax import lax
import numpy as np

D_MODEL = 1024
BATCH = 8
SEQ = 2048
DEPTH = 1
DEC_BATCH = 32
DEC_SEQ = 1
PAST_LEN = 16384
PAGE_SIZE = 128

HEAD_DIM = 64
D_MIX = D_MODEL
C_CONV = D_MIX // 2
D_ATT = D_MIX - C_CONV
N_HEADS = D_ATT // HEAD_DIM
N_KV_HEADS = 2
GRP = N_HEADS // N_KV_HEADS
KV_W = N_KV_HEADS * HEAD_DIM
CONV_W = 31
CMP_LEN = 32
CMP_STRIDE = 16
N_SUB = CMP_LEN // CMP_STRIDE
CMP_HID = 2 * HEAD_DIM
SEL_BLK = 64
TOPK = 16
WINDOW = 512
ROT_DIM = HEAD_DIM // 4
ROPE_THETA = 500000.0
D_FF = 2816
N_IN = 2 * C_CONV + D_ATT + 6 * KV_W + 3 * N_HEADS
QBLK = 64
NEG_INF = -1e30
FORCE_BONUS = 1e4
EPS = 1e-6

kernel_name = 'hybrid_conformer_conv_nsa_macaron_step'


def rms_norm(x, g):
    xf = x.astype(jnp.float32)
    y = xf * lax.rsqrt(jnp.mean(xf * xf, axis=-1, keepdims=True) + EPS)
    return (y * g.astype(jnp.float32)).astype(x.dtype)


def layer_norm(x, g, b):
    xf = x.astype(jnp.float32)
    mu = jnp.mean(xf, axis=-1, keepdims=True)
    var = jnp.mean(jnp.square(xf - mu), axis=-1, keepdims=True)
    y = (xf - mu) * lax.rsqrt(var + EPS) * g.astype(jnp.float32) + b.astype(jnp.float32)
    return y.astype(x.dtype)


def swiglu(h, w_in, w_out):
    a, b = jnp.split(h @ w_in, 2, axis=-1)
    return (jax.nn.silu(a) * b) @ w_out


def masked_softmax(s, mask):
    s = jnp.where(mask, s, NEG_INF)
    m = jnp.max(s, axis=-1, keepdims=True)
    e = jnp.where(mask, jnp.exp(s - m), 0.0)
    return e / jnp.maximum(jnp.sum(e, axis=-1, keepdims=True), 1e-30)


def rope(x, pos):
    half = ROT_DIM // 2
    inv = ROPE_THETA ** (-(jnp.arange(half, dtype=jnp.float32) * 2.0 / ROT_DIM))
    ang = pos.astype(jnp.float32)[:, None] * inv[None, :]
    cos = jnp.cos(ang)[:, None, :]
    sin = jnp.sin(ang)[:, None, :]
    xr = x[..., :ROT_DIM].astype(jnp.float32)
    x1, x2 = xr[..., :half], xr[..., half:]
    rot = jnp.concatenate([x1 * cos - x2 * sin, x1 * sin + x2 * cos], axis=-1).astype(x.dtype)
    return jnp.concatenate([rot, x[..., ROT_DIM:]], axis=-1)


def compress(k_raw, pe, w1, w2):
    B, L, G, D = k_raw.shape
    n_chunk = L // CMP_STRIDE
    n_cmp = n_chunk - N_SUB + 1
    ch = k_raw[:, :n_chunk * CMP_STRIDE].reshape(B, n_chunk, CMP_STRIDE, G, D)
    pe = pe.reshape(N_SUB, CMP_STRIDE, D)
    w1 = w1.reshape(N_SUB, CMP_STRIDE, D, CMP_HID)
    h = jnp.einsum('bcsgd,sdh->bcgh', ch + pe[0][:, None, :], w1[0])[:, :n_cmp]
    for m in range(1, N_SUB):
        h = h + jnp.einsum('bcsgd,sdh->bcgh', ch + pe[m][:, None, :], w1[m])[:, m:m + n_cmp]
    return jnp.einsum('bngh,hd->bngd', jax.nn.silu(h), w2)


def cmp_to_sel(n_cmp, n_sel):
    cs = jnp.arange(n_cmp)[:, None] * CMP_STRIDE
    ss = jnp.arange(n_sel)[None, :] * SEL_BLK
    ov = jnp.clip(jnp.minimum(cs + CMP_LEN, ss + SEL_BLK) - jnp.maximum(cs, ss), 0, None)
    return ov.astype(jnp.float32) / CMP_LEN


def nsa_attention(q, gates, q_pos0, k_cmp, v_cmp, cmp_end, k_sel, v_sel, k_win, v_win, win_pos0):
    B, Tq = q.shape[0], q.shape[1]
    f32 = jnp.float32
    scale = HEAD_DIM ** -0.5
    qb = QBLK if Tq % QBLK == 0 else Tq
    n_qb = Tq // qb
    Ls = k_sel.shape[1]
    n_sel = -(-Ls // SEL_BLK)
    pad = n_sel * SEL_BLK - Ls

    def to_blocks(t):
        t = jnp.pad(t, ((0, 0), (0, pad), (0, 0), (0, 0)))
        return t.reshape(B, n_sel, SEL_BLK, N_KV_HEADS, HEAD_DIM).transpose(0, 3, 1, 2, 4)

    kb, vb = to_blocks(k_sel), to_blocks(v_sel)
    k_eff = min(TOPK, n_sel)
    ov = cmp_to_sel(k_cmp.shape[1], n_sel)
    kw_pad = jnp.pad(k_win, ((0, 0), (WINDOW, 0), (0, 0), (0, 0)))
    vw_pad = jnp.pad(v_win, ((0, 0), (WINDOW, 0), (0, 0), (0, 0)))
    kc32, vc32 = k_cmp.astype(f32), v_cmp.astype(f32)
    bi = jnp.arange(B)[:, None, None, None]
    gi = jnp.arange(N_KV_HEADS)[None, :, None, None]
    jsel = jnp.arange(n_sel)

    def block(i):
        off = i * qb
        qpos = q_pos0 + off + jnp.arange(qb)
        qg = lax.dynamic_slice_in_dim(q, off, qb, 1).reshape(B, qb, N_KV_HEADS, GRP, HEAD_DIM).astype(f32)
        g = lax.dynamic_slice_in_dim(gates, off, qb, 1).reshape(B, qb, N_KV_HEADS, GRP, 3).astype(f32)
        s_c = jnp.einsum('bqgrd,bngd->bqgrn', qg, kc32) * scale
        mask_c = (cmp_end[None, :] <= qpos[:, None])[None, :, None, None, :]
        p_c = masked_softmax(s_c, mask_c)
        o_c = jnp.einsum('bqgrn,bngd->bqgrd', p_c, vc32)
        imp = jnp.einsum('bqgn,ns->bqgs', jnp.sum(p_c, axis=3), ov)
        cur = qpos // SEL_BLK
        valid = jsel[None, :] <= cur[:, None]
        forced = (jsel[None, :] == 0) | (jsel[None, :] == cur[:, None]) | (jsel[None, :] == cur[:, None] - 1)
        score = jnp.where(valid[None, :, None, :],
                          imp + jnp.where(forced, FORCE_BONUS, 0.0)[None, :, None, :], NEG_INF)
        _, idx = lax.top_k(score, k_eff)
        idx_t = idx.transpose(0, 2, 1, 3)
        ks_g = kb[bi, gi, idx_t].reshape(B, N_KV_HEADS, qb, k_eff * SEL_BLK, HEAD_DIM)
        vs_g = vb[bi, gi, idx_t].reshape(B, N_KV_HEADS, qb, k_eff * SEL_BLK, HEAD_DIM)
        kpos = (idx_t[..., None] * SEL_BLK + jnp.arange(SEL_BLK)).reshape(B, N_KV_HEADS, qb, k_eff * SEL_BLK)
        mask_s = (kpos <= qpos[None, None, :, None]).transpose(0, 2, 1, 3)[:, :, :, None, :]
        s_s = jnp.einsum('bqgrd,bgqnd->bqgrn', qg, ks_g.astype(f32)) * scale
        p_s = masked_softmax(s_s, mask_s)
        o_s = jnp.einsum('bqgrn,bgqnd->bqgrd', p_s, vs_g.astype(f32))
        start = q_pos0 + off - win_pos0
        kw = lax.dynamic_slice_in_dim(kw_pad, start, WINDOW + qb, 1).astype(f32)
        vw = lax.dynamic_slice_in_dim(vw_pad, start, WINDOW + qb, 1).astype(f32)
        kpos_w = q_pos0 + off - WINDOW + jnp.arange(WINDOW + qb)
        dist = qpos[:, None] - kpos_w[None, :]
        mask_w = ((kpos_w[None, :] >= win_pos0) & (dist >= 0) & (dist < WINDOW))[None, :, None, None, :]
        s_w = jnp.einsum('bqgrd,bkgd->bqgrk', qg, kw) * scale
        p_w = masked_softmax(s_w, mask_w)
        o_w = jnp.einsum('bqgrk,bkgd->bqgrd', p_w, vw)
        o = g[..., 0:1] * o_c + g[..., 1:2] * o_s + g[..., 2:3] * o_w
        return o.reshape(B, qb, D_ATT).astype(q.dtype)

    o = lax.map(block, jnp.arange(n_qb))
    return jnp.moveaxis(o, 0, 1).reshape(B, Tq, D_ATT)


def token_mixers(h, pos0, conv_prev, pk_cmp, pv_cmp, pk_sel, pv_sel, win_k_prev, win_v_prev, p):
    B, T, _ = h.shape
    widths = [2 * C_CONV, D_ATT, KV_W, KV_W, KV_W, KV_W, KV_W, KV_W, 3 * N_HEADS]
    bounds = np.cumsum(widths)[:-1].tolist()
    glu_in, q, k_c, v_c, k_s, v_s, k_w, v_w, g_logit = jnp.split(h @ p['w_in'], bounds, axis=-1)

    a, b = jnp.split(glu_in, 2, axis=-1)
    u = a * jax.nn.sigmoid(b)
    ubuf = jnp.concatenate([conv_prev, u], axis=1)
    c = lax.conv_general_dilated(ubuf, p['conv_w'][:, None, :], window_strides=(1,), padding='VALID',
                                 dimension_numbers=('NWC', 'WIO', 'NWC'),
                                 feature_group_count=C_CONV) + p['conv_b']
    c = jax.nn.silu(layer_norm(c, p['conv_ln_g'], p['conv_ln_b']))
    new_conv = ubuf[:, ubuf.shape[1] - (CONV_W - 1):]

    pos = pos0 + jnp.arange(T)
    q = rope(rms_norm(q.reshape(B, T, N_HEADS, HEAD_DIM), p['q_norm']), pos)
    k_c = k_c.reshape(B, T, N_KV_HEADS, HEAD_DIM)
    v_c = v_c.reshape(B, T, N_KV_HEADS, HEAD_DIM)
    k_s = rope(rms_norm(k_s.reshape(B, T, N_KV_HEADS, HEAD_DIM), p['k_sel_norm']), pos)
    v_s = v_s.reshape(B, T, N_KV_HEADS, HEAD_DIM)
    k_w = rope(rms_norm(k_w.reshape(B, T, N_KV_HEADS, HEAD_DIM), p['k_win_norm']), pos)
    v_w = v_w.reshape(B, T, N_KV_HEADS, HEAD_DIM)
    kc_all = jnp.concatenate([pk_cmp, k_c], axis=1)
    vc_all = jnp.concatenate([pv_cmp, v_c], axis=1)
    ks_all = jnp.concatenate([pk_sel, k_s], axis=1)
    vs_all = jnp.concatenate([pv_sel, v_s], axis=1)
    k_cmp_tok = compress(kc_all, p['cmp_k_pos'], p['cmp_k_w1'], p['cmp_k_w2'])
    v_cmp_tok = compress(vc_all, p['cmp_v_pos'], p['cmp_v_w1'], p['cmp_v_w2'])
    cmp_end = jnp.arange(k_cmp_tok.shape[1]) * CMP_STRIDE + CMP_LEN - 1
    k_cmp_tok = rope(rms_norm(k_cmp_tok, p['k_cmp_norm']), cmp_end)
    kw_all = jnp.concatenate([win_k_prev, k_w], axis=1)
    vw_all = jnp.concatenate([win_v_prev, v_w], axis=1)
    win_pos0 = pos0 - win_k_prev.shape[1]
    gates = jax.nn.sigmoid(g_logit.reshape(B, T, N_HEADS, 3))
    o = nsa_attention(q, gates, pos0, k_cmp_tok, v_cmp_tok, cmp_end, ks_all, vs_all, kw_all, vw_all, win_pos0)
    keep = min(WINDOW, kw_all.shape[1])
    new_kw = kw_all[:, kw_all.shape[1] - keep:]
    new_vw = vw_all[:, vw_all.shape[1] - keep:]

    mixed = jnp.concatenate([rms_norm(c, p['out_norm_conv']), rms_norm(o, p['out_norm_attn'])], axis=-1) @ p['w_out']
    return mixed, (k_c, v_c, k_s, v_s, new_kw, new_vw, new_conv)


def layer(x, pos0, conv_prev, pk_cmp, pv_cmp, pk_sel, pv_sel, win_k_prev, win_v_prev, p):
    x = x + 0.5 * swiglu(rms_norm(x, p['ffn1_norm']), p['ffn1_w_in'], p['ffn1_w_out'])
    mixed, new_state = token_mixers(rms_norm(x, p['mix_norm']), pos0, conv_prev, pk_cmp, pv_cmp,
                                    pk_sel, pv_sel, win_k_prev, win_v_prev, p)
    x = x + mixed
    x = x + 0.5 * swiglu(rms_norm(x, p['ffn2_norm']), p['ffn2_w_in'], p['ffn2_w_out'])
    return rms_norm(x, p['final_norm']), new_state


def setup_inputs(seed: int = 0) -> dict:
    key = jax.random.key(seed)
    keys = iter(jax.random.split(key, 48))

    def nrm(shape, scale):
        return jax.random.normal(next(keys), shape, jnp.float32) * scale

    def gain(shape):
        return 1.0 + nrm(shape, 0.02)

    n_pages = PAST_LEN // PAGE_SIZE
    n_used = DEC_BATCH * n_pages
    n_pool = n_used + (n_used + 3) // 4
    win_buf = min(WINDOW, PAST_LEN)
    pool_shape = (DEPTH, n_pool, PAGE_SIZE, N_KV_HEADS, HEAD_DIM)
    win_shape = (DEPTH, DEC_BATCH, win_buf, N_KV_HEADS, HEAD_DIM)
    x_prompt = nrm((BATCH, SEQ, D_MODEL), 1.0)
    x_sample = nrm((DEC_BATCH, DEC_SEQ, D_MODEL), 1.0)
    cache_k_cmp = nrm(pool_shape, 1.0)
    cache_v_cmp = nrm(pool_shape, 1.0)
    cache_k_sel = nrm(pool_shape, 1.0)
    cache_v_sel = nrm(pool_shape, 1.0)
    state_k_win = nrm(win_shape, 1.0)
    state_v_win = nrm(win_shape, 1.0)
    state_conv = nrm((DEPTH, DEC_BATCH, CONV_W - 1, C_CONV), 0.5)
    page_table = jax.random.permutation(next(keys), n_pool)[:n_used].reshape(DEC_BATCH, n_pages).astype(jnp.int32)
    return {
        'x_prompt': x_prompt,
        'x_sample': x_sample,
        'cache_k_cmp': cache_k_cmp,
        'cache_v_cmp': cache_v_cmp,
        'cache_k_sel': cache_k_sel,
        'cache_v_sel': cache_v_sel,
        'state_k_win': state_k_win,
        'state_v_win': state_v_win,
        'state_conv': state_conv,
        'page_table': page_table,
        'ffn1_norm': gain((DEPTH, D_MODEL)),
        'ffn1_w_in': nrm((DEPTH, D_MODEL, 2 * D_FF), D_MODEL ** -0.5),
        'ffn1_w_out': nrm((DEPTH, D_FF, D_MODEL), D_FF ** -0.5),
        'mix_norm': gain((DEPTH, D_MODEL)),
        'w_in': nrm((DEPTH, D_MODEL, N_IN), D_MODEL ** -0.5),
        'conv_w': nrm((DEPTH, CONV_W, C_CONV), CONV_W ** -0.5),
        'conv_b': nrm((DEPTH, C_CONV), 0.01),
        'conv_ln_g': gain((DEPTH, C_CONV)),
        'conv_ln_b': nrm((DEPTH, C_CONV), 0.01),
        'q_norm': gain((DEPTH, HEAD_DIM)),
        'k_cmp_norm': gain((DEPTH, HEAD_DIM)),
        'k_sel_norm': gain((DEPTH, HEAD_DIM)),
        'k_win_norm': gain((DEPTH, HEAD_DIM)),
        'cmp_k_pos': nrm((DEPTH, CMP_LEN, HEAD_DIM), 0.1),
        'cmp_k_w1': nrm((DEPTH, CMP_LEN, HEAD_DIM, CMP_HID), (CMP_LEN * HEAD_DIM) ** -0.5),
        'cmp_k_w2': nrm((DEPTH, CMP_HID, HEAD_DIM), CMP_HID ** -0.5),
        'cmp_v_pos': nrm((DEPTH, CMP_LEN, HEAD_DIM), 0.1),
        'cmp_v_w1': nrm((DEPTH, CMP_LEN, HEAD_DIM, CMP_HID), (CMP_LEN * HEAD_DIM) ** -0.5),
        'cmp_v_w2': nrm((DEPTH, CMP_HID, HEAD_DIM), CMP_HID ** -0.5),
        'out_norm_conv': gain((DEPTH, C_CONV)),
        'out_norm_attn': gain((DEPTH, D_ATT)),
        'w_out': nrm((DEPTH, D_MIX, D_MODEL), D_MIX ** -0.5),
        'ffn2_norm': gain((DEPTH, D_MODEL)),
        'ffn2_w_in': nrm((DEPTH, D_MODEL, 2 * D_FF), D_MODEL ** -0.5),
        'ffn2_w_out': nrm((DEPTH, D_FF, D_MODEL), D_FF ** -0.5),
        'final_norm': gain((DEPTH, D_MODEL)),
    }


def reference(x_prompt, x_sample, cache_k_cmp, cache_v_cmp, cache_k_sel, cache_v_sel, state_k_win, state_v_win,
              state_conv, page_table, ffn1_norm, ffn1_w_in, ffn1_w_out, mix_norm, w_in, conv_w, conv_b, conv_ln_g,
              conv_ln_b, q_norm, k_cmp_norm, k_sel_norm, k_win_norm, cmp_k_pos, cmp_k_w1, cmp_k_w2, cmp_v_pos,
              cmp_v_w1, cmp_v_w2, out_norm_conv, out_norm_attn, w_out, ffn2_norm, ffn2_w_in, ffn2_w_out, final_norm):
    b_p = x_prompt.shape[0]
    b_s = x_sample.shape[0]
    past_len = page_table.shape[1] * cache_k_cmp.shape[2]
    yp, ys = x_prompt, x_sample
    new_p, new_s = [], []
    for l in range(DEPTH):
        p = {
            'ffn1_norm': ffn1_norm[l], 'ffn1_w_in': ffn1_w_in[l], 'ffn1_w_out': ffn1_w_out[l],
            'mix_norm': mix_norm[l], 'w_in': w_in[l], 'conv_w': conv_w[l], 'conv_b': conv_b[l],
            'conv_ln_g': conv_ln_g[l], 'conv_ln_b': conv_ln_b[l], 'q_norm': q_norm[l],
            'k_cmp_norm': k_cmp_norm[l], 'k_sel_norm': k_sel_norm[l], 'k_win_norm': k_win_norm[l],
            'cmp_k_pos': cmp_k_pos[l], 'cmp_k_w1': cmp_k_w1[l], 'cmp_k_w2': cmp_k_w2[l],
            'cmp_v_pos': cmp_v_pos[l], 'cmp_v_w1': cmp_v_w1[l], 'cmp_v_w2': cmp_v_w2[l],
            'out_norm_conv': out_norm_conv[l], 'out_norm_attn': out_norm_attn[l], 'w_out': w_out[l],
            'ffn2_norm': ffn2_norm[l], 'ffn2_w_in': ffn2_w_in[l], 'ffn2_w_out': ffn2_w_out[l],
            'final_norm': final_norm[l],
        }
        empty = jnp.zeros((b_p, 0, N_KV_HEADS, HEAD_DIM), x_prompt.dtype)
        conv0 = jnp.zeros((b_p, CONV_W - 1, C_CONV), x_prompt.dtype)
        yp, st_p = layer(yp, 0, conv0, empty, empty, empty, empty, empty, empty, p)
        new_p.append(st_p)
        past = [c[l][page_table].reshape(b_s, past_len, N_KV_HEADS, HEAD_DIM)
                for c in (cache_k_cmp, cache_v_cmp, cache_k_sel, cache_v_sel)]
        ys, st_s = layer(ys, past_len, state_conv[l], past[0], past[1], past[2], past[3],
                         state_k_win[l], state_v_win[l], p)
        new_s.append(st_s)
    p_k_cmp, p_v_cmp, p_k_sel, p_v_sel, p_k_win, p_v_win, p_conv = [jnp.stack(t) for t in zip(*new_p)]
    s_k_cmp, s_v_cmp, s_k_sel, s_v_sel, s_k_win, s_v_win, s_conv = [jnp.stack(t) for t in zip(*new_s)]
    return (yp, ys, p_k_cmp, p_v_cmp, p_k_sel, p_v_sel, p_k_win, p_v_win, p_conv,
            s_k_cmp, s_v_cmp, s_k_sel, s_v_sel, s_k_win, s_v_win, s_conv)
```

```python
import numpy as np
import ml_dtypes
import concourse.bass as bass
import concourse.mybir as mybir
from concourse.bass_utils import run_bass_kernel_spmd

F32 = mybir.dt.float32
BF16 = mybir.dt.bfloat16
I32 = mybir.dt.int32
U32 = mybir.dt.uint32
U8 = mybir.dt.uint8
AF = mybir.ActivationFunctionType
ALU = mybir.AluOpType
AX = mybir.AxisListType

D_MODEL = 1024
SEQ = 2048
NS = 4
TT = SEQ + NS
D_FF = 2816
NFC = D_FF // 128
HD = 64
N_HEADS = 8
PAST = 16384
PAGE = 128
NPAGES = PAST // PAGE
N_POOL = 5120
EPS = 1e-6
SCALE = HD ** -0.5
N_IN = 2328
CONV_W = 31
NCMP_P = 127
NCMP_S = 1023
NSEL_P = 32
ROPE_THETA = 500000.0
ESZ = {F32: 4, BF16: 2, I32: 4, U32: 4, U8: 1}

COLT = [(0, 512), (512, 512), (1024, 512), (1536, 512), (2048, NS)]


class Prod:
    def __init__(self, name, sem, inc):
        self.name, self.sem, self.inc, self.count = name, sem, inc, 0


class Space:
    def __init__(self, name):
        self.name = name
        self.segs = []

    def touch(self, lo, hi, write, deps):
        segs = self.segs
        out = []
        inside = []
        cur = lo
        for s in segs:
            slo, shi, w, rd = s
            if shi <= lo or slo >= hi:
                out.append(s)
                continue
            if slo < lo:
                out.append([slo, lo, w, dict(rd)])
            a, b = max(slo, lo), min(shi, hi)
            if a > cur:
                ns = [cur, a, None, {}]
                out.append(ns)
                inside.append(ns)
            ns = [a, b, w, dict(rd)]
            out.append(ns)
            inside.append(ns)
            cur = b
            if shi > hi:
                out.append([hi, shi, w, dict(rd)])
        if cur < hi:
            ns = [cur, hi, None, {}]
            out.append(ns)
            inside.append(ns)
        out.sort(key=lambda s: s[0])
        self.segs = out
        for s in inside:
            if s[2] is not None:
                p, t = s[2]
                if deps.get(p, 0) < t:
                    deps[p] = t
            if write:
                for p, t in s[3].items():
                    if deps.get(p, 0) < t:
                        deps[p] = t
        return inside

    def mark(self, lo, hi, write, who):
        inside = self.touch(lo, hi, write, {})
        if write:
            keep = [s for s in self.segs if s[1] <= lo or s[0] >= hi]
            keep.append([lo, hi, who, {}])
            keep.sort(key=lambda s: s[0])
            self.segs = keep
        else:
            p, t = who
            for s in inside:
                if s[3].get(p, 0) < t:
                    s[3][p] = t


class View:
    def __init__(self, ap, space, lo, hi):
        self.ap, self.space, self.lo, self.hi = ap, space, lo, hi


class Tile:
    def __init__(self, kb, name, shape, dtype, off, parts=128):
        self.kb, self.name, self.shape, self.dtype, self.off = kb, name, tuple(shape), dtype, off
        self.esz = ESZ[dtype]
        n = int(np.prod(shape))
        self.nbytes = n * self.esz
        ap = kb.arena[0:parts, off:off + self.nbytes].bitcast(dtype)
        if len(shape) == 2:
            ap = ap.rearrange("p (a b) -> p a b", a=shape[0])
        elif len(shape) == 3:
            ap = ap.rearrange("p (a b c) -> p a b c", a=shape[0], b=shape[1])
        elif len(shape) == 4:
            ap = ap.rearrange("p (a b c d) -> p a b c d", a=shape[0], b=shape[1], c=shape[2])
        self.full = ap
        st = []
        acc = 1
        for s in reversed(shape):
            st.append(acc)
            acc *= s
        self.strides = list(reversed(st))

    def __getitem__(self, idx):
        if not isinstance(idx, tuple):
            idx = (idx,)
        pidx = idx[0]
        fidx = list(idx[1:]) + [slice(None)] * (len(self.shape) - len(idx) + 1)
        lo = 0
        hi = 0
        for i, (ix, n, s) in enumerate(zip(fidx, self.shape, self.strides)):
            if isinstance(ix, int):
                a, b = ix, ix + 1
            else:
                a = 0 if ix.start is None else ix.start
                b = n if ix.stop is None else ix.stop
                step = 1 if ix.step is None else ix.step
                b = a + ((b - a - 1) // step) * step + 1
            assert 0 <= a < b <= n, (self.name, idx, self.shape)
            lo += a * s
            hi += (b - 1) * s
        hi += 1
        ap = self.full[(pidx,) + tuple(fidx)]
        return View(ap, self.kb.sb, self.off + lo * self.esz, self.off + hi * self.esz)

    def all(self):
        return self[:]


class KB:
    def __init__(self, nc):
        self.nc = nc
        self.sb = Space("sbuf")
        self.ps = Space("psum")
        self.dram = {}
        self.eng = {}
        for name, h in [("pe", nc.tensor), ("act", nc.scalar), ("dve", nc.vector), ("pool", nc.gpsimd), ("sp", nc.sync)]:
            sem = nc.semaphore("sem_" + name).__enter__()
            p = Prod(name, sem, 1)
            p.h = h
            p.waited = {}
            self.eng[name] = p
        self.dsem = {"sp": [], "pool": [], "act": []}
        for q, n in [("sp", 12), ("pool", 10), ("act", 2)]:
            for i in range(n):
                sem = nc.semaphore(f"dsem_{q}{i}").__enter__()
                self.dsem[q].append(Prod(f"d{q}{i}", sem, 16))
        self.drr = {"sp": 0, "pool": 0, "act": 0}
        self.arena = nc.sbuf_tensor("arena", [128, ARENA], U8).__enter__()
        self.psum = nc.psum_tensor("psum", [128, 8, 512], F32).__enter__()
        self.n_ops = 0

    def P(self, bank, a=0, b=512, p0=0, p1=128):
        return View(self.psum[p0:p1, bank, a:b], self.ps, bank * 2048, bank * 2048 + 2048)

    def Pbf(self, bank, a=0, b=1024, p0=0, p1=128):
        return View(self.psum[p0:p1, bank, :].bitcast(BF16)[:, a:b], self.ps, bank * 2048, bank * 2048 + 2048)

    def dview(self, name, ap):
        sp = self.dram.setdefault(name, Space(name))
        return View(ap, sp, 0, 1)

    def _sync(self, E, reads, writes, skip_self=False):
        deps = {}
        for v in reads:
            if v is not None and v.space is not None:
                v.space.touch(v.lo, v.hi, v.space is self.ps, deps)
        for v in writes:
            if v.space is not None:
                v.space.touch(v.lo, v.hi, True, deps)
        for P, t in deps.items():
            if P is E and skip_self:
                continue
            if E.waited.get(P, 0) < t:
                E.h.wait_ge(P.sem, t)
                E.waited[P] = t

    def _mark(self, who, reads, writes):
        for v in reads:
            if v is not None and v.space is not None:
                v.space.mark(v.lo, v.hi, v.space is self.ps, who)
        for v in writes:
            if v.space is not None:
                v.space.mark(v.lo, v.hi, True, who)

    def op(self, eng, fn, reads=(), writes=()):
        E = self.eng[eng]
        self._sync(E, reads, writes, skip_self=(eng == "pe"))
        inst = fn(E.h)
        E.count += 1
        inst.then_inc(E.sem, 1)
        self._mark((E, E.count), reads, writes)
        self.n_ops += 1
        return inst

    def dma(self, q, out, in_, fn=None):
        Q = self.eng[q]
        reads = [in_]
        writes = [out]
        self._sync(Q, reads, writes)
        lst = self.dsem[q]
        S = lst[self.drr[q] % len(lst)]
        self.drr[q] += 1
        if Q.waited.get(S, 0) < S.count:
            Q.h.wait_ge(S.sem, S.count)
            Q.waited[S] = S.count
        if fn is None:
            inst = Q.h.dma_start(out=out.ap, in_=in_.ap)
        else:
            inst = fn(Q.h)
        S.count += 16
        inst.then_inc(S.sem, 16)
        self._mark((S, S.count), reads, writes)
        self.n_ops += 1

    def finish(self):
        E = self.eng["sp"]
        for q in self.dsem:
            for S in self.dsem[q]:
                if S.count > 0 and E.waited.get(S, 0) < S.count:
                    E.h.wait_ge(S.sem, S.count)
                    E.waited[S] = S.count
        for name, P in self.eng.items():
            if name != "sp" and P.count > 0:
                E.h.wait_ge(P.sem, P.count)

    def mm(self, out, lhsT, rhs, start, stop, **kw):
        self.op("pe", lambda e: e.matmul(out.ap, lhsT=lhsT.ap, rhs=rhs.ap, start=start, stop=stop,
                                         skip_group_check=True, **kw), reads=[lhsT, rhs], writes=[out])

    def transpose(self, out, in_, ident):
        self.op("pe", lambda e: e.transpose(out=out.ap, in_=in_.ap, identity=ident.ap), reads=[in_, ident], writes=[out])

    def act(self, out, in_, func, scale=1.0, bias=0.0, accum=None):
        reads = [in_]
        kw = {}
        if isinstance(scale, View):
            reads.append(scale)
            kw["scale"] = scale.ap
        else:
            kw["scale"] = float(scale)
        if isinstance(bias, View):
            reads.append(bias)
            kw["bias"] = bias.ap
        else:
            kw["bias"] = float(bias)
        writes = [out]
        if accum is not None:
            writes.append(accum)
            kw["accum_out"] = accum.ap
        self.op("act", lambda e: e.activation(out=out.ap, in_=in_.ap, func=func, **kw), reads=reads, writes=writes)

    def tt(self, out, a, b, op, eng="dve"):
        self.op(eng, lambda e: e.tensor_tensor(out=out.ap, in0=a.ap, in1=b.ap, op=op), reads=[a, b], writes=[out])

    def ts(self, out, a, s1, op0, s2=None, op1=None, eng="dve", accum=None):
        reads = [a]
        k1 = s1.ap if isinstance(s1, View) else float(s1)
        if isinstance(s1, View):
            reads.append(s1)
        k2 = None
        if s2 is not None:
            k2 = s2.ap if isinstance(s2, View) else float(s2)
            if isinstance(s2, View):
                reads.append(s2)
        writes = [out]
        kw = {}
        if accum is not None:
            writes.append(accum)
            kw["accum_out"] = accum.ap
        if op1 is None:
            self.op(eng, lambda e: e.tensor_scalar(out=out.ap, in0=a.ap, scalar1=k1, scalar2=None, op0=op0, **kw),
                    reads=reads, writes=writes)
        else:
            self.op(eng, lambda e: e.tensor_scalar(out=out.ap, in0=a.ap, scalar1=k1, scalar2=k2, op0=op0, op1=op1, **kw),
                    reads=reads, writes=writes)

    def stt(self, out, a, s, b, op0, op1):
        reads = [a, b]
        k = s.ap if isinstance(s, View) else float(s)
        if isinstance(s, View):
            reads.append(s)
        self.op("dve", lambda e: e.scalar_tensor_tensor(out=out.ap, in0=a.ap, scalar=k, in1=b.ap, op0=op0, op1=op1),
                reads=reads, writes=[out])

    def copy(self, out, in_, eng="dve"):
        if eng == "act":
            self.op("act", lambda e: e.copy(out=out.ap, in_=in_.ap), reads=[in_], writes=[out])
        else:
            self.op(eng, lambda e: e.tensor_copy(out=out.ap, in_=in_.ap), reads=[in_], writes=[out])

    def memset(self, out, val, eng="dve"):
        self.op(eng, lambda e: e.memset(out.ap, val), reads=[], writes=[out])

    def recip(self, out, in_):
        self.op("dve", lambda e: e.reciprocal(out=out.ap, in_=in_.ap), reads=[in_], writes=[out])

    def reduce(self, out, in_, op=ALU.add, axis=AX.X):
        self.op("dve", lambda e: e.tensor_reduce(out=out.ap, in_=in_.ap, axis=axis, op=op), reads=[in_], writes=[out])


ARENA = 207 * 1024


NV_FFN1, NV_MIX, NV_FFN2, NV_FIN = 0, 8, 16, 24
NV_CONVW = 32
NV_CONVB = NV_CONVW + 124
NV_LNG = NV_CONVB + 4
NV_LNB = NV_LNG + 4
NV_ONC = NV_LNB + 4
NV_ONA = NV_ONC + 4
NV_QN = NV_ONA + 4
NV_TOT = NV_QN + 4

CB_ONES, CB_ID, CB_BLK, CB_PM = 0, 128, 256, 384
CB_E = 512
CB_CM = CB_E + 2048
CB_CMP = CB_CM + 8 * 512
CB_OVP = CB_CMP + 2048
CB_TOT = CB_OVP + 33


def _consts():
    cb = np.zeros((128, CB_TOT), np.float32)
    cb[:, CB_ONES:CB_ONES + 128] = 1.0
    cb[:, CB_ID:CB_ID + 128] = np.eye(128, dtype=np.float32)
    blk = np.zeros((128, 128), np.float32)
    blk[:64, :64] = 1
    blk[64:, 64:] = 1
    cb[:, CB_BLK:CB_BLK + 128] = blk
    pm = np.zeros((128, 128), np.float32)
    for base in (0, 64):
        for i in range(8):
            pm[base + i + 8, base + i] = -1.0
            pm[base + i, base + i + 8] = 1.0
    cb[:, CB_PM:CB_PM + 128] = pm
    k = np.arange(2048)
    for j in range(32):
        cb[j, CB_E:CB_E + 2048] = (k // 64 == j)
    kk = np.arange(128)[:, None]
    qq = np.arange(512)[None, :]
    for r in range(4):
        cb[:, CB_CM + r * 512:CB_CM + (r + 1) * 512] = (kk + 128 * r <= qq)
    for r in range(1, 5):
        cb[:, CB_CM + (3 + r) * 512:CB_CM + (4 + r) * 512] = (qq - kk + 128 * r < 512)
    n = np.arange(128)[:, None]
    q = np.arange(2048)[None, :]
    cb[:, CB_CMP:CB_CMP + 2048] = ((16 * n + 31 <= q) & (n < 127))
    cs = np.arange(127)[:, None] * 16
    ss = np.arange(32)[None, :] * 64
    ov = np.clip(np.minimum(cs + 32, ss + 64) - np.maximum(cs, ss), 0, None).astype(np.float32) / 32
    cb[:127, CB_OVP] = 1.0
    cb[:127, CB_OVP + 1:CB_OVP + 33] = ov
    return cb


def _rope_tab(pos):
    half = 8
    inv = (np.float32(ROPE_THETA) ** (-(np.arange(half, dtype=np.float32) * np.float32(2.0) / np.float32(16)))).astype(np.float32)
    ang = pos.astype(np.float32)[:, None] * inv[None, :]
    cos = np.cos(ang).astype(np.float32)
    sin = np.sin(ang).astype(np.float32)
    n = pos.shape[0]
    tab = np.zeros((128, 2, n), np.float32)
    tab[:, 0, :] = 1.0
    for base in (0, 64):
        for i in range(8):
            tab[base + i, 0] = cos[:, i]
            tab[base + i + 8, 0] = cos[:, i]
            tab[base + i, 1] = sin[:, i]
            tab[base + i + 8, 1] = sin[:, i]
    return tab


SC_SELC = 0
SC_BONS = SC_SELC + 64
SC_MK = SC_BONS + 256
SC_MSKR = SC_MK + 32
SC_MSKW = SC_MSKR + 1
SC_IND2 = SC_MSKW + 32
SC_INDB = SC_IND2 + 4
SC_IOTA = SC_INDB + 128
SC_H2 = SC_IOTA + 256
SC_TOT = SC_H2 + 2


def _sconst():
    sc = np.zeros((128, SC_TOT), np.float32)
    for bg in range(8):
        sc[0:4, SC_SELC + bg * 8 + bg] = 1.0
    sc[0:8, SC_BONS + 0] = 1e4
    sc[0:8, SC_BONS + 255] = 1e4
    sc[:, SC_MK:SC_MK + 32] = 1.0
    sc[127, SC_MK + 28:SC_MK + 32] = 0.0
    p = np.arange(128)
    r = p % 16
    b = (p // 16) % 4
    sc[:, SC_MSKR] = (r != 15)
    sc[:, SC_MSKW:SC_MSKW + 32] = 1.0
    sc[r == 0, SC_MSKW] = 0.0
    for bb in range(4):
        sc[:, SC_IND2 + bb] = (b == bb)
        sc[bb, SC_INDB:SC_INDB + 128] = (b == bb)
    sc[:, SC_IOTA:SC_IOTA + 256] = np.arange(256)[None, :]
    sc[:, SC_H2 + 1] = 2.0
    return sc


def _ovs():
    cs = np.arange(1024)[:, None] * 16
    ss = np.arange(257)[None, :] * 64
    ov = np.clip(np.minimum(cs + 32, ss + 64) - np.maximum(cs, ss), 0, None).astype(np.float32) / 32
    ov[1023] = 0.0
    return np.ascontiguousarray(ov.reshape(128, 8, 257))


def _bonus_prompt():
    q = np.arange(2048)
    cur = q // 64
    j = np.arange(32)[None, :]
    valid = j <= cur[:, None]
    forced = (j == 0) | (j == cur[:, None]) | (j == cur[:, None] - 1)
    b = np.where(valid, np.where(forced, 1e4, 0.0), -1e30).astype(np.float32)
    return np.ascontiguousarray(b.reshape(16, 128, 32).transpose(1, 0, 2))


class Alloc:
    def __init__(self, kb, base=0):
        self.kb, self.cur, self.peak = kb, base, base

    def __call__(self, name, shape, dtype, parts=128):
        self.cur = (self.cur + 31) // 32 * 32
        t = Tile(self.kb, name, shape, dtype, self.cur, parts)
        self.cur += t.nbytes
        self.peak = max(self.peak, self.cur)
        assert self.cur <= ARENA, (name, self.cur)
        return t


def build_program(debug=None):
    nc = bass.Bass("TRN2", target_bir_lowering=False)
    kb = KB(nc)
    dbg_outs = {}

    def din(name, shape, dtype=F32):
        return nc.dram_tensor(name, list(shape), dtype, kind="ExternalInput").ap()

    def dout(name, shape, dtype=F32):
        return nc.dram_tensor(name, list(shape), dtype, kind="ExternalOutput").ap()

    d_xT = din("xT", [128, 8, TT])
    d_up = [din("ffn1_up", [11, 128, 8, 2, 256]), din("ffn2_up", [11, 128, 8, 2, 256])]
    d_dn = [din("ffn1_dn", [4, 128, 22, 256]), din("ffn2_dn", [4, 128, 22, 256])]
    d_win = din("w_in_t", [5, 128, 8, 512])
    d_wout = din("w_out_t", [2, 128, 8, 512])
    d_vec = din("vec", [128, NV_TOT])
    d_cb = din("cb", [128, CB_TOT])
    d_idf = din("idf", [128, 128])
    d_rope = din("rope_tok", [128, 2, TT])
    d_ropec = din("rope_cmp", [128, 2, 1024])
    d_bonus = din("bonus_p", [128, 16, 32])
    d_w1a = [din("cmpk_w1a", [128, 32, 128]), din("cmpv_w1a", [128, 32, 128])]
    d_pea = [din("cmpk_pea", [128, 32]), din("cmpv_pea", [128, 32])]
    d_w2 = [din("cmpk_w2", [128, 64]), din("cmpv_w2", [128, 64])]

    o_y = dout("yT", [128, 8, TT])
    o_kv = dout("kvT", [128, 6, TT])
    o_pconv = dout("pconvT", [128, 4, 30])

    A = Alloc(kb)
    X = A("X", [8, TT], F32)
    H = A("H", [8, TT], BF16)
    VEC = A("VEC", [NV_TOT], F32)
    CB = A("CB", [512], BF16)
    IDF = A("IDF", [128], F32)
    WA = [A("WA0", [8, 512], BF16), A("WA1", [8, 512], BF16)]
    WB = [A("WB0", [22, 256], BF16), A("WB1", [22, 256], BF16)]
    SCR = A.cur

    ones = CB[:, CB_ONES:CB_ONES + 128]
    identb = CB[:, CB_ID:CB_ID + 128]
    blk = CB[:, CB_BLK:CB_BLK + 128]
    pmat = CB[:, CB_PM:CB_PM + 128]

    bank_rr = [0]

    reserved = set()

    def nb():
        while True:
            b = bank_rr[0] % 8
            bank_rr[0] += 1
            if b not in reserved:
                return b

    for k in range(8):
        kb.dma("sp", X[:, k, :], View(d_xT[:, k, :], None, 0, 0))
    kb.dma("sp", VEC[:, :], View(d_vec, None, 0, 0))
    kb.dma("sp", IDF[:, :], View(d_idf, None, 0, 0))
    kb.dma("pool", CB[:, 0:512], View(d_cb[:, 0:512], None, 0, 0))

    def rmsnorm(A2, src, dst_fn, gcol, ntile_list=COLT):
        SQ = A2("SQ", [8, 512], BF16)
        LNV = A2("LNV", [512], F32)
        RSTD = A2("RSTD", [512], F32)
        for (c0, w) in ntile_list:
            b = nb()
            for k in range(8):
                kb.act(SQ[:, k, 0:w], src[:, k, c0:c0 + w], AF.Square)
            for k in range(8):
                kb.mm(kb.P(b, 0, w), ones, SQ[:, k, 0:w], start=(k == 0), stop=(k == 7))
            kb.act(LNV[:, 0:w], kb.P(b, 0, w), AF.Ln, scale=1.0 / D_MODEL, bias=EPS)
            kb.act(RSTD[:, 0:w], LNV[:, 0:w], AF.Exp, scale=-0.5)
            for k in range(8):
                kb.stt(dst_fn(k, c0, w), src[:, k, c0:c0 + w], VEC[:, gcol + k:gcol + k + 1], RSTD[:, 0:w], ALU.mult, ALU.mult)

    def ffn(idx, gcol):
        A2 = Alloc(kb, SCR)
        G = A2("G", [NFC, 1028], BF16)
        SA = [A2("SA0", [512], BF16), A2("SA1", [512], BF16)]
        rmsnorm(A2, X, lambda k, c0, w: H[:, k, c0:c0 + w], gcol)
        halves = [[COLT[0], COLT[1], COLT[4]], [COLT[2], COLT[3]]]
        cnt_a = [0]
        for hi_, tiles in enumerate(halves):
            loc = {}
            o = 0
            for (c0, w) in tiles:
                loc[c0] = o
                o += w
            def load_up(g):
                slot = WA[g % 2]
                kb.dma("pool", slot[:, :, :], View(d_up[idx][g].rearrange("p a b c -> p a (b c)"), None, 0, 0))
            load_up(0)
            for g in range(11):
                if g + 1 < 11:
                    load_up(g + 1)
                slot = WA[g % 2]
                for pair in range(2):
                    i = g * 2 + pair
                    for (c0, w) in tiles:
                        ba, bb = nb(), nb()
                        for ab, bk in ((0, ba), (1, bb)):
                            for dc in range(8):
                                kb.mm(kb.P(bk, 0, w), slot[:, dc, ab * 256 + pair * 128: ab * 256 + pair * 128 + 128],
                                      H[:, dc, c0:c0 + w], start=(dc == 0), stop=(dc == 7))
                        sa = SA[cnt_a[0] % 2]
                        cnt_a[0] += 1
                        kb.act(sa[:, 0:w], kb.P(ba, 0, w), AF.Silu)
                        kb.tt(G[:, i, loc[c0]:loc[c0] + w], sa[:, 0:w], kb.P(bb, 0, w), ALU.mult)
            def load_dn(g):
                slot = WB[g % 2]
                kb.dma("pool", slot[:, :, :], View(d_dn[idx][g], None, 0, 0))
            load_dn(0)
            for g in range(4):
                if g + 1 < 4:
                    load_dn(g + 1)
                slot = WB[g % 2]
                for dch in range(2):
                    dk = g * 2 + dch
                    for (c0, w) in tiles:
                        b = nb()
                        for fc in range(NFC):
                            kb.mm(kb.P(b, 0, w), slot[:, fc, dch * 128:(dch + 1) * 128], G[:, fc, loc[c0]:loc[c0] + w],
                                  start=(fc == 0), stop=(fc == NFC - 1))
                        kb.stt(X[:, dk, c0:c0 + w], kb.P(b, 0, w), 0.5, X[:, dk, c0:c0 + w], ALU.mult, ALU.add)

    ffn(0, NV_FFN1)
    if debug == "ffn1":
        for k in range(8):
            kb.dma("sp", kb.dview("yT", o_y[:, k, :]), X[:, k, :])
        kb.finish()
        return nc


    A3 = Alloc(kb, SCR)
    CN = A3("CN", [4, TT], BF16)
    MSCR = A3.cur

    rmsnorm(Alloc(kb, MSCR), X, lambda k, c0, w: H[:, k, c0:c0 + w], NV_MIX)

    A4 = Alloc(kb, MSCR)
    C = A4("C", [4, TT], F32)
    U = A4("U", [30 + SEQ], BF16)
    UT = A4("UT", [30], F32)
    DG = A4("DG", [CONV_W, 128], BF16)
    SIG = A4("SIG", [512], F32)
    UB = A4("UB", [4, NS, 31], F32)
    TMPS = A4("TMPS", [NS, 31], F32)
    AW = Alloc(kb, WB[0].off)
    SQ2 = AW("SQ2", [8, 512], BF16)
    ST1 = AW("ST1", [512], F32)
    ST2 = AW("ST2", [512], F32)
    ST3 = AW("ST3", [512], F32)
    OST = [AW("OST%d" % i, [512], F32) for i in range(4)]
    assert AW.cur <= WB[1].off + WB[1].nbytes
    ost_rr = [0]

    def ost():
        t = OST[ost_rr[0] % 4]
        ost_rr[0] += 1
        return t

    def load_win(g, slot):
        kb.dma("pool", slot[:, :, :], View(d_win[g], None, 0, 0))

    load_win(0, WA[0])
    load_win(1, WA[1])
    kb.memset(U[:, 0:30], 0.0)
    d_sconv = din("sconvT", [128, 4, NS, 30])
    o_sconv = dout("sconvT_out", [128, 4, NS, 30])
    kb.dma("sp", UB[:, :, :, 0:30], View(d_sconv, None, 0, 0))
    for c in range(4):
        for (c0, w) in COLT:
            ba, bb = nb(), nb()
            for slot, bk in ((WA[0], ba), (WA[1], bb)):
                for dc in range(8):
                    kb.mm(kb.P(bk, 0, w), slot[:, dc, c * 128:(c + 1) * 128], H[:, dc, c0:c0 + w], start=(dc == 0), stop=(dc == 7))
            kb.act(SIG[:, 0:w], kb.P(bb, 0, w), AF.Exp, scale=-1.0)
            kb.ts(SIG[:, 0:w], SIG[:, 0:w], 1.0, ALU.add)
            kb.recip(SIG[:, 0:w], SIG[:, 0:w])
            if c0 < SEQ:
                kb.tt(U[:, 30 + c0:30 + c0 + w], SIG[:, 0:w], kb.P(ba, 0, w), ALU.mult)
                if c0 + w == SEQ:
                    kb.tt(UT[:, :], SIG[:, w - 30:w], kb.P(ba, w - 30, w), ALU.mult)
            else:
                kb.tt(UB[:, c, :, 30], SIG[:, 0:w], kb.P(ba, 0, w), ALU.mult)
        kb.dma("sp", kb.dview("pconv", o_pconv[:, c, :]), UT[:, :])
        wc = NV_CONVW + c * 31
        for j in range(CONV_W):
            kb.ts(DG[:, j, :], identb, VEC[:, wc + j:wc + j + 1], ALU.mult, eng=("pool" if j % 2 else "dve"))
        for (c0, w) in COLT[:4]:
            bcv = nb()
            for j in range(CONV_W):
                kb.mm(kb.P(bcv, 0, w), DG[:, j, :], U[:, c0 + j:c0 + j + w], start=(j == 0), stop=(j == CONV_W - 1))
            kb.ts(C[:, c, c0:c0 + w], kb.P(bcv, 0, w), VEC[:, NV_CONVB + c:NV_CONVB + c + 1], ALU.add)
        kb.tt(TMPS[:, :, :], UB[:, c, :, :], View(VEC.full[:, wc:wc + 31].unsqueeze(1).to_broadcast([128, NS, 31]), kb.sb, VEC.off, VEC.off + VEC.nbytes), ALU.mult)
        kb.reduce(C[:, c, SEQ:TT], TMPS[:, :, :])
        kb.ts(C[:, c, SEQ:TT], C[:, c, SEQ:TT], VEC[:, NV_CONVB + c:NV_CONVB + c + 1], ALU.add)
    kb.dma("sp", kb.dview("sconv", o_sconv), UB[:, :, :, 1:31])

    for (c0, w) in COLT:
        b1, b2 = nb(), nb()
        for c in range(4):
            kb.act(SQ2[:, c, 0:w], C[:, c, c0:c0 + w], AF.Copy)
            kb.act(SQ2[:, 4 + c, 0:w], C[:, c, c0:c0 + w], AF.Square)
        for c in range(4):
            kb.mm(kb.P(b1, 0, w), ones, SQ2[:, c, 0:w], start=(c == 0), stop=(c == 3))
        for c in range(4):
            kb.mm(kb.P(b2, 0, w), ones, SQ2[:, 4 + c, 0:w], start=(c == 0), stop=(c == 3))
        kb.ts(ST1[:, 0:w], kb.P(b1, 0, w), 1.0 / 512, ALU.mult)
        kb.tt(ST2[:, 0:w], ST1[:, 0:w], ST1[:, 0:w], ALU.mult)
        kb.stt(ST2[:, 0:w], kb.P(b2, 0, w), 1.0 / 512, ST2[:, 0:w], ALU.mult, ALU.subtract)
        kb.act(ST3[:, 0:w], ST2[:, 0:w], AF.Ln, bias=EPS)
        kb.act(ST3[:, 0:w], ST3[:, 0:w], AF.Exp, scale=-0.5)
        for c in range(4):
            kb.tt(C[:, c, c0:c0 + w], C[:, c, c0:c0 + w], ST1[:, 0:w], ALU.subtract)
            kb.tt(C[:, c, c0:c0 + w], C[:, c, c0:c0 + w], ST3[:, 0:w], ALU.mult)
    for c in range(4):
        kb.act(C[:, c, :], C[:, c, :], AF.Silu, scale=VEC[:, NV_LNG + c:NV_LNG + c + 1], bias=VEC[:, NV_LNB + c:NV_LNB + c + 1])
    for (c0, w) in COLT:
        b1 = nb()
        for c in range(4):
            kb.act(SQ2[:, c, 0:w], C[:, c, c0:c0 + w], AF.Square)
        for c in range(4):
            kb.mm(kb.P(b1, 0, w), ones, SQ2[:, c, 0:w], start=(c == 0), stop=(c == 3))
        kb.act(ST3[:, 0:w], kb.P(b1, 0, w), AF.Ln, scale=1.0 / 512, bias=EPS)
        kb.act(ST3[:, 0:w], ST3[:, 0:w], AF.Exp, scale=-0.5)
        for c in range(4):
            kb.stt(CN[:, c, c0:c0 + w], C[:, c, c0:c0 + w], VEC[:, NV_ONC + c:NV_ONC + c + 1], ST3[:, 0:w], ALU.mult, ALU.mult)

    if debug == "conv":
        o_dbg = dout("dbg", [128, 4, TT])
        for c in range(4):
            kb.copy(C[:, c, :], CN[:, c, :])
            kb.dma("sp", kb.dview("dbg", o_dbg[:, c, :]), C[:, c, :])
        kb.finish()
        return nc


    A5 = Alloc(kb, MSCR)
    QT = A5("QT", [4, TT], BF16)
    KST = A5("KST", [TT], BF16)
    KWT = A5("KWT", [TT], BF16)
    KCT = A5("KCT", [TT], BF16)
    VCT = A5("VCT", [TT], BF16)
    VAS = A5("VAS", [17, 2, 65], BF16)
    VAW = A5("VAW", [17, 2, 65], BF16)
    GT = A5("GT", [17, 24], F32)
    KCMPT = A5("KCMPT", [128], BF16)
    RC = A5("RC", [2, 97], BF16)
    M2END = A5.cur
    AW = Alloc(kb, WB[0].off)
    SQb = AW("SQb", [512], BF16)
    QNB = AW("QNB", [512], BF16)
    VFB = AW("VFB", [512], BF16)
    ST1 = AW("ST1", [512], F32)
    ST2 = AW("ST2", [512], F32)
    ST3 = AW("ST3", [512], F32)
    OST = [AW("OST%d" % i, [512], F32) for i in range(2)]
    ROPE = [AW("ROPE%d" % i, [2, 512], F32) for i in range(2)]
    assert AW.cur <= WB[1].off + WB[1].nbytes
    ost_rr = [0]

    def ost2():
        t = OST[ost_rr[0] % 2]
        ost_rr[0] += 1
        return t

    def norm_rope(ps, w, gcol, cosv, sinv, out_bf, out_f32):
        kb.act(SQb[:, 0:w], ps, AF.Square)
        b2 = nb()
        kb.mm(kb.P(b2, 0, w), blk, SQb[:, 0:w], start=True, stop=True)
        kb.act(ST3[:, 0:w], kb.P(b2, 0, w), AF.Ln, scale=1.0 / HD, bias=EPS)
        kb.act(ST3[:, 0:w], ST3[:, 0:w], AF.Exp, scale=-0.5)
        kb.stt(ST1[:, 0:w], ps, VEC[:, gcol:gcol + 1], ST3[:, 0:w], ALU.mult, ALU.mult)
        kb.act(QNB[:, 0:w], ST1[:, 0:w], AF.Copy)
        b3 = nb()
        kb.mm(kb.P(b3, 0, w), pmat, QNB[:, 0:w], start=True, stop=True)
        kb.tt(ST2[:, 0:w], kb.P(b3, 0, w), sinv, ALU.mult)
        kb.tt(ST1[:, 0:w], ST1[:, 0:w], cosv, ALU.mult)
        if out_f32 is not None:
            kb.tt(out_f32, ST1[:, 0:w], ST2[:, 0:w], ALU.add)
            kb.act(out_bf, out_f32, AF.Copy)
        else:
            kb.tt(out_bf, ST1[:, 0:w], ST2[:, 0:w], ALU.add)

    kb.memset(VAS[:, :, :, 64:65], 1.0)
    kb.memset(VAW[:, :, :, 64:65], 1.0)
    rope_rr = [0]
    KV_SLOT = {"kc": 0, "vc": 1, "ks": 2, "vs": 3, "kw": 4, "vw": 5}
    plan = [
        (2, [(0, "q", 0), (1, "q", 1), (2, "q", 2), (3, "q", 3)]),
        (3, [(0, "kc", None), (1, "vc", None), (2, "ks", None), (3, "vs", None)]),
        (4, [(0, "kw", None), (1, "vw", None), (2, "gate", None)]),
    ]
    items = []
    for gi, (grp, chunks) in enumerate(plan):
        for ti, (c0, w) in enumerate(COLT):
            for k_, (ci, kind, qi) in enumerate(chunks):
                items.append((gi, grp, ti, c0, w, ci, kind, qi, k_ == 0))
    loaded = set()
    state = {"rp": None}

    def m2_proj(it):
        gi, grp, ti, c0, w, ci, kind, qi, first_in_tile = it
        slot = WA[gi % 2]
        if gi not in loaded:
            loaded.add(gi)
            load_win(grp, slot)
        b = nb()
        mrows = 24 if kind == "gate" else 128
        for dc in range(8):
            kb.mm(kb.P(b, 0, w, 0, mrows), slot[:, dc, ci * 128:ci * 128 + mrows], H[:, dc, c0:c0 + w], start=(dc == 0), stop=(dc == 7))
        return b

    def m2_post(it, b):
        gi, grp, ti, c0, w, ci, kind, qi, first_in_tile = it
        if first_in_tile and grp in (2, 3, 4):
            rp_ = ROPE[rope_rr[0] % 2]
            rope_rr[0] += 1
            kb.dma("sp", rp_[:, :, 0:w], View(d_rope[:, :, c0:c0 + w], None, 0, 0))
            state["rp"] = rp_
        rp = state["rp"]
        ps = kb.P(b, 0, w)
        if kind == "q":
            norm_rope(ps, w, NV_QN + 0, rp[:, 0, 0:w], rp[:, 1, 0:w], QT[:, qi, c0:c0 + w], None)
        elif kind in ("ks", "kw"):
            o32 = ost2()
            dst = KST if kind == "ks" else KWT
            norm_rope(ps, w, NV_QN + (2 if kind == "ks" else 3), rp[:, 0, 0:w], rp[:, 1, 0:w], dst[:, c0:c0 + w], o32[:, 0:w])
            kb.dma("sp", kb.dview("kvT", o_kv[:, KV_SLOT[kind], c0:c0 + w]), o32[:, 0:w])
        elif kind in ("kc", "vc", "vs", "vw"):
            o32 = ost2()
            kb.copy(o32[:, 0:w], ps)
            kb.dma("sp", kb.dview("kvT", o_kv[:, KV_SLOT[kind], c0:c0 + w]), o32[:, 0:w])
            if kind == "kc":
                kb.act(KCT[:, c0:c0 + w], ps, AF.Copy)
            elif kind == "vc":
                kb.act(VCT[:, c0:c0 + w], ps, AF.Copy)
            else:
                VA = VAS if kind == "vs" else VAW
                kb.act(VFB[:, 0:w], ps, AF.Copy)
                nsub = (w + 127) // 128
                for sb_ in range(nsub):
                    ww = min(128, w - sb_ * 128)
                    bt = nb()
                    kb.transpose(kb.Pbf(bt, 0, 128, 0, ww), VFB[:, sb_ * 128:sb_ * 128 + ww], identb)
                    tix = c0 // 128 + sb_
                    kb.copy(VA[0:ww, tix, :, 0:64], View(kb.psum[0:ww, bt, :].bitcast(BF16)[:, 0:128].rearrange("p (g d) -> p g d", g=2), kb.ps, bt * 2048, bt * 2048 + 2048))
        else:
            kb.act(ST1[0:24, 0:w], kb.P(b, 0, w, 0, 24), AF.Exp, scale=-1.0)
            kb.ts(ST1[0:24, 0:w], ST1[0:24, 0:w], 1.0, ALU.add)
            kb.recip(ST1[0:24, 0:w], ST1[0:24, 0:w])
            nsub = (w + 127) // 128
            for sb_ in range(nsub):
                ww = min(128, w - sb_ * 128)
                bt = nb()
                kb.transpose(kb.P(bt, 0, 24, 0, ww), ST1[0:24, sb_ * 128:sb_ * 128 + ww], View(IDF.full[0:24, 0:24], kb.sb, IDF.off, IDF.off + IDF.nbytes))
                kb.copy(GT[0:ww, c0 // 128 + sb_, :], kb.P(bt, 0, 24, 0, ww))

    pend = None
    for it in items:
        b = m2_proj(it)
        reserved.add(b)
        if pend is not None:
            m2_post(*pend)
            reserved.discard(pend[1])
        pend = (it, b)
    m2_post(*pend)
    reserved.discard(pend[1])

    if debug == "proj":
        o_dbg = dout("dbg", [128, 7, TT])
        for i, t in enumerate([QT[:, 0, :], QT[:, 1, :], QT[:, 2, :], QT[:, 3, :], KST[:, :], KWT[:, :], KCT[:, :]]):
            for (c0, w) in COLT:
                o32 = ost2()
                kb.copy(o32[:, 0:w], View(t.ap[:, c0:c0 + w], t.space, t.lo, t.hi))
                kb.dma("sp", kb.dview("dbg", o_dbg[:, i, c0:c0 + w]), o32[:, 0:w])
        o_dbg2 = dout("dbg_va", [128, 17, 2, 65])
        o_dbg3 = dout("dbg_gt", [128, 17, 24])
        VAf = A5("VAf", [17, 2, 65], F32)
        kb.copy(VAf[:, :, :, :], VAS[:, :, :, :])
        kb.dma("sp", kb.dview("dbg2", o_dbg2), VAf[:, :, :, :])
        kb.dma("sp", kb.dview("dbg3", o_dbg3), GT[:, :, :])
        kb.finish()
        return nc


    W1A = [Tile(kb, "W1Ak", [32, 128], BF16, WA[0].off), Tile(kb, "W1Av", [32, 128], BF16, WA[1].off)]
    A6 = Alloc(kb, M2END)
    PEA = [A6("PEAk", [32], BF16), A6("PEAv", [32], BF16)]
    W2 = [A6("W2k", [64], BF16), A6("W2v", [64], BF16)]
    BIAS = [A6("BIASk", [1], F32), A6("BIASv", [1], F32)]
    NBIAS = [A6("NBIASk", [1], F32), A6("NBIASv", [1], F32)]
    EH = A6("EH", [128], F32)
    HB = A6("HB", [128], BF16)
    for kv in range(2):
        kb.dma("pool", W1A[kv][:, :, :], View(d_w1a[kv], None, 0, 0))
        kb.dma("pool", PEA[kv][:, :], View(d_pea[kv], None, 0, 0))
        kb.dma("pool", W2[kv][:, :], View(d_w2[kv], None, 0, 0))
    kb.dma("pool", RC[:, 0, 64:97], View(d_cb[:, CB_OVP:CB_OVP + 33], None, 0, 0))
    kb.dma("pool", RC[:, 1, 64:97], View(d_cb[:, CB_OVP:CB_OVP + 33], None, 0, 0))
    kb.dma("sp", ROPE[0][:, :, 0:127], View(d_ropec[:, :, 0:127], None, 0, 0))
    import os
    CMPDBG = int(os.environ.get("CMPDBG", "99"))
    for kv in range(2):
        if CMPDBG < 1:
            break
        b = nb()
        for s_ in range(32):
            kb.mm(kb.P(b, 0, 1), W1A[kv][0:64, s_, :], PEA[kv][0:64, s_:s_ + 1], start=(s_ == 0), stop=(s_ == 31))
        kb.copy(BIAS[kv][:, :], kb.P(b, 0, 1))
        kb.ts(NBIAS[kv][:, :], BIAS[kv][:, :], -1.0, ALU.mult)
    bk = nb()
    for kv in range(2):
        if CMPDBG < 2 + kv:
            break
        src = KCT if kv == 0 else VCT
        for g in range(2):
            b = nb()
            for s_ in range(32):
                kb.mm(kb.P(b, 0, 127), W1A[kv][g * 64:(g + 1) * 64, s_, :], src[g * 64:(g + 1) * 64, s_:s_ + 16 * 126 + 1:16],
                      start=(s_ == 0), stop=(s_ == 31))
            kb.act(EH[:, 0:127], kb.P(b, 0, 127), AF.Exp, scale=-1.0, bias=NBIAS[kv][:, 0:1])
            kb.ts(EH[:, 0:127], EH[:, 0:127], 1.0, ALU.add)
            kb.recip(EH[:, 0:127], EH[:, 0:127])
            kb.stt(HB[:, 0:127], kb.P(b, 0, 127), BIAS[kv][:, 0:1], EH[:, 0:127], ALU.add, ALU.mult)
            if kv == 0:
                kb.mm(kb.P(bk, 0, 127, g * 64, g * 64 + 64), W2[0][:, :], HB[:, 0:127], start=True, stop=True)
            else:
                b2 = nb()
                kb.mm(kb.P(b2, 0, 64, 0, 127), HB[:, 0:127], W2[1][:, :], start=True, stop=True)
                kb.copy(RC[0:127, g, 0:64], kb.P(b2, 0, 64, 0, 127))
        if kv == 0 and CMPDBG != 2:
            norm_rope(kb.P(bk, 0, 127), 127, NV_QN + 1, ROPE[0][:, 0, 0:127], ROPE[0][:, 1, 0:127], KCMPT[:, 0:127], None)

    if debug == "cmp":
        o_dbg = dout("dbg", [128, 128])
        o_dbg2 = dout("dbg2", [128, 2, 97])
        kb.memset(ST1[:, 0:128], 0.0)
        kb.copy(ST1[:, 0:127], KCMPT[:, 0:127])
        kb.dma("sp", kb.dview("dbg", o_dbg), ST1[:, 0:128])
        RCf = A6("RCf", [2, 97], F32)
        kb.memset(RCf[:, :, :], 0.0)
        kb.copy(RCf[0:127, :, :], RC[0:127, :, :])
        kb.dma("sp", kb.dview("dbg2", o_dbg2), RCf[:, :, :])
        kb.finish()
        return nc

    AWB = Alloc(kb, WB[0].off)
    CM = AWB("CM", [8, 512], BF16)
    CMPM = AWB("CMPM", [2048], BF16)
    EE = AWB("EE", [2048], BF16)
    ET = [AWB("ET%d" % i, [512], BF16) for i in range(3)]
    MSK = [AWB("MSK%d" % i, [512], BF16) for i in range(2)]
    ET.append(AWB("ET3", [512], BF16))
    assert AWB.cur <= WB[1].off + WB[1].nbytes
    AH = Alloc(kb, H.off)
    OTOK = AH("OTOK", [4, 512], F32)
    SELT = AH("SELT", [512], BF16)
    IMP = AH("IMP", [4, 32], F32)
    TMPO = AH("TMPO", [4, 64], F32)
    TMPI = AH("TMPI", [4, 32], F32)
    SCO = AH("SCO", [32], F32)
    SC2 = AH("SC2", [32], F32)
    M8 = AH("M8", [16], F32)
    SELF = AH("SELF", [32], F32)
    BON = AH("BON", [4, 32], F32)
    RD = AH("RD", [4], F32)
    WG = AH("WG", [4], F32)
    SS = AH("SS", [4], F32)
    SS2 = AH("SS2", [4], F32)
    ONB = AH("ONB", [512], BF16)
    assert AH.cur <= H.off + 4 * TT * 2
    kb.dma("pool", CM[:, :, :], View(d_cb[:, CB_CM:CB_CM + 4096].rearrange("p (a b) -> p a b", a=8), None, 0, 0))
    kb.dma("pool", CMPM[:, :], View(d_cb[:, CB_CMP:CB_CMP + 2048], None, 0, 0))
    kb.dma("pool", EE[:, :], View(d_cb[:, CB_E:CB_E + 2048], None, 0, 0))
    et_rr = [0]
    sc_rr = [0]
    msk_rr = [0]
    OB = [3, 4, 5, 6]

    def strided_ps(bank, start, step, n, width, rows=128):
        full = kb.psum[0:rows, bank, 0:n * step].rearrange("p (n s) -> p n s", s=step)[:, :, start:start + width]
        return View(full, kb.ps, bank * 2048, bank * 2048 + 2048)

    def strided_ps2(bank, start, step, n, rows=128):
        full = kb.psum[0:rows, bank, 0:n * step].rearrange("p (n s) -> p n s", s=step)[:, :, start]
        return View(full, kb.ps, bank * 2048, bank * 2048 + 2048)

    def evac_branch(bank, W, h, bi, qt0, nsub, rows, first, want_imp, imp_first, IMP):
        den = strided_ps2(bank, 64, W, nsub, rows)
        kb.ts(RD[0:rows, 0:nsub], den, 1e-30, ALU.max)
        kb.recip(RD[0:rows, 0:nsub], RD[0:rows, 0:nsub])
        kb.tt(WG[0:rows, 0:nsub], RD[0:rows, 0:nsub], GT[0:rows, qt0:qt0 + nsub, 3 * h + bi], ALU.mult)
        wgb = View(WG.full[0:rows, 0:nsub].unsqueeze(2).to_broadcast([rows, nsub, 64]), kb.sb, WG.off, WG.off + WG.nbytes)
        onum = strided_ps(bank, 0, W, nsub, 64, rows)
        dst = OTOK[0:rows, 0:nsub, h * 64:(h + 1) * 64]
        if first:
            kb.tt(dst, onum, wgb, ALU.mult)
        else:
            kb.tt(TMPO[0:rows, 0:nsub, :], onum, wgb, ALU.mult)
            kb.tt(dst, dst, TMPO[0:rows, 0:nsub, :], ALU.add)
        if want_imp:
            rdb = View(RD.full[0:rows, 0:nsub].unsqueeze(2).to_broadcast([rows, nsub, 32]), kb.sb, RD.off, RD.off + RD.nbytes)
            oimp = strided_ps(bank, 65, W, nsub, 32, rows)
            if imp_first:
                kb.tt(IMP[0:rows, 0:nsub, :], oimp, rdb, ALU.mult)
            else:
                kb.tt(TMPI[0:rows, 0:nsub, :], oimp, rdb, ALU.mult)
                kb.tt(IMP[0:rows, 0:nsub, :], IMP[0:rows, 0:nsub, :], TMPI[0:rows, 0:nsub, :], ALU.add)

    def out_norm_to_H(rows, nsub, col0):
        for sub in range(nsub):
            kb.act(ONB[0:rows, :], OTOK[0:rows, sub, :], AF.Square, accum=SS[0:rows, sub:sub + 1])
        kb.act(SS2[0:rows, 0:nsub], SS[0:rows, 0:nsub], AF.Ln, scale=1.0 / 512, bias=EPS)
        kb.act(SS2[0:rows, 0:nsub], SS2[0:rows, 0:nsub], AF.Exp, scale=-0.5)
        for sub in range(nsub):
            kb.ts(ONB[0:rows, :], OTOK[0:rows, sub, :], SS2[0:rows, sub:sub + 1], ALU.mult)
            for j in range(4):
                kb.transpose(kb.Pbf(7, j * 128, j * 128 + rows), ONB[0:rows, j * 128:(j + 1) * 128], View(identb.ap[0:rows, 0:rows], kb.sb, identb.lo, identb.hi))
            for j in range(4):
                kb.ts(H[:, 4 + j, col0 + sub * 128:col0 + sub * 128 + rows], kb.Pbf(7, j * 128, j * 128 + rows),
                      VEC[:, NV_ONA + j:NV_ONA + j + 1], ALU.mult)

    SELT2 = [SELT, AH("SELT1", [512], BF16)]
    IMP2 = [IMP, AH("IMP1", [4, 32], F32)]
    ET4 = ET
    assert AH.cur <= H.off + 4 * TT * 2

    def attend(Q):
        q0 = Q * 512
        qcols = slice(q0, q0 + 512)
        tasks = []
        if Q >= 2:
            kb.dma("sp", BON[:, :, :], View(d_bonus[:, Q * 4:Q * 4 + 4, :], None, 0, 0))

        def add(**kw):
            t = dict(pre=None, post=None)
            t.update(kw)
            tasks.append(t)

        def topk(g):
            imp, selt = IMP2[g], SELT2[g]
            for sub in range(4):
                kb.tt(SCO[:, :], imp[:, sub, :], BON[:, sub, :], ALU.add)
                kb.op("dve", lambda e: e.max(out=M8[:, 0:8].ap, in_=SCO[:, :].ap), reads=[SCO[:, :]], writes=[M8[:, 0:8]])
                kb.op("dve", lambda e: e.match_replace(out=SC2[:, :].ap, in_to_replace=M8[:, 0:8].ap, in_values=SCO[:, :].ap, imm_value=-3.0e38),
                      reads=[SCO[:, :], M8[:, 0:8]], writes=[SC2[:, :]])
                kb.op("dve", lambda e: e.max(out=M8[:, 8:16].ap, in_=SC2[:, :].ap), reads=[SC2[:, :]], writes=[M8[:, 8:16]])
                kb.ts(SELF[:, :], SCO[:, :], M8[:, 15:16], ALU.is_ge)
                kb.transpose(kb.P(7, sub * 128, (sub + 1) * 128, 0, 32), SELF[:, :], IDF[:, :])
            kb.copy(selt[0:32, :], kb.P(7, 0, 512, 0, 32))

        for g in range(2):
            gp = slice(g * 64, (g + 1) * 64)
            for hh in range(4):
                def qk(a, gp=gp, hh=hh):
                    kb.mm(kb.P(a, 0, 512, 0, 127), KCMPT[gp, 0:127], QT[gp, hh, qcols], start=True, stop=True)
                def ex(a, et):
                    kb.act(et[0:127, :], kb.P(a, 0, 512, 0, 127), AF.Exp, scale=SCALE)
                    kb.tt(et[0:127, :], et[0:127, :], CMPM[0:127, qcols], ALU.mult)
                def pv(et, g=g, hh=hh):
                    for sub in range(4):
                        kb.mm(kb.P(OB[hh], sub * 97, sub * 97 + 97), et[0:127, sub * 128:(sub + 1) * 128], RC[0:127, g, :],
                              start=(sub == 0), stop=True)
                def post(g=g, hh=hh):
                    evac_branch(OB[hh], 97, 4 * g + hh, 0, Q * 4, 4, 128, True, Q >= 2, hh == 0, IMP2[g])
                    if hh == 3 and Q >= 2:
                        topk(g)
                add(qk=qk, ex=ex, pv=pv, post=post)
        for g in range(2):
            gp = slice(g * 64, (g + 1) * 64)
            for bi, KT, VA in ((1, KST, VAS), (2, KWT, VAW)):
                kt_lo = 0 if bi == 1 else max(0, 4 * Q - 4)
                kts = list(range(kt_lo, 4 * Q + 4))
                started = [False] * 4
                for Kt in kts:
                    r = Kt - 4 * Q
                    pre = None
                    mask_box = [None]
                    if bi == 1:
                        if Q >= 2:
                            mk = MSK[msk_rr[0] % 2]
                            msk_rr[0] += 1
                            def pre(Kt=Kt, r=r, mk=mk, g=g):
                                kb.mm(kb.P(7), EE[0:32, Kt * 128:(Kt + 1) * 128], SELT2[g][0:32, :], start=True, stop=True)
                                if r >= 0:
                                    kb.tt(mk[:, :], kb.P(7), CM[:, r, :], ALU.mult)
                                else:
                                    kb.copy(mk[:, :], kb.P(7))
                            mask_box[0] = mk[:, :]
                        elif r >= 0:
                            mask_box[0] = CM[:, r, :]
                        subs = [s_ for s_ in range(4) if r <= s_]
                    else:
                        mask_box[0] = CM[:, r, :] if r >= 0 else CM[:, 3 - r, :]
                        subs = [s_ for s_ in range(4) if (r <= s_ and s_ - r < 5)]
                    for hh in range(4):
                        def qk(a, gp=gp, hh=hh, Kt=Kt, KT=KT):
                            kb.mm(kb.P(a), KT[gp, Kt * 128:(Kt + 1) * 128], QT[gp, hh, qcols], start=True, stop=True)
                        def ex(a, et, mask=mask_box[0]):
                            kb.act(et[:, :], kb.P(a), AF.Exp, scale=SCALE)
                            if mask is not None:
                                kb.tt(et[:, :], et[:, :], mask, ALU.mult)
                        first = not started[hh]
                        started[hh] = True
                        def pv(et, hh=hh, Kt=Kt, g=g, VA=VA, subs=subs, first=first):
                            f = first
                            for sub in subs:
                                kb.mm(kb.P(OB[hh], sub * 65, sub * 65 + 65), et[:, sub * 128:(sub + 1) * 128], VA[:, Kt, g, :],
                                      start=f, stop=True)
                                f = False
                        post = None
                        if Kt == kts[-1]:
                            def post(g=g, hh=hh, bi=bi):
                                evac_branch(OB[hh], 65, 4 * g + hh, bi, Q * 4, 4, 128, False, False, False, None)
                        add(qk=qk, ex=ex, pv=pv, post=post, pre=(pre if hh == 0 else None))
        n = len(tasks)
        for i in range(n + 2):
            if i < n:
                t = tasks[i]
                if t["pre"] is not None:
                    t["pre"]()
                t["qk"](i % 3)
            if 1 <= i <= n:
                tasks[i - 1]["ex"]((i - 1) % 3, ET4[(i - 1) % 4])
            if i >= 2:
                t = tasks[i - 2]
                t["pv"](ET4[(i - 2) % 4])
                if t["post"] is not None:
                    t["post"]()
        out_norm_to_H(128, 4, q0)

    for Q in range(4):
        attend(Q)
        if debug == "att0":
            break

    if debug in ("att0", "att"):
        o_dbg = dout("dbg", [128, 4, TT])
        for j in range(4):
            for (c0, w) in COLT[:4]:
                o32 = ost2() if False else None
            kb.copy(X[:, j, 0:SEQ], H[:, 4 + j, 0:SEQ])
            kb.dma("sp", kb.dview("dbg", o_dbg[:, j, 0:SEQ]), X[:, j, 0:SEQ])
        kb.finish()
        return nc


    d_pools = [din("pool_kc", [N_POOL * 4, 4096]), din("pool_vc", [N_POOL * 4, 4096]),
               din("pool_ks", [N_POOL * 4, 4096]), din("pool_vs", [N_POOL * 4, 4096])]
    d_wins = [din("win_k", [NS, 512, 128]), din("win_v", [NS, 512, 128])]
    d_ptT = din("ptT", [128, NS], I32)
    d_ptB = din("ptB", [128, 128], I32)
    d_sc = din("sconst", [128, SC_TOT])
    d_ovs = din("ovs", [128, 8, 257])
    o_swin = [dout("swin_k", [NS, 512, 128]), dout("swin_v", [NS, 512, 128])]
    sc_idx = nc.dram_tensor("sc_idx", [128], U32, kind="Internal").ap()
    sc_oc = nc.dram_tensor("sc_oc", [NS, 2, 4, 64], F32, kind="Internal").ap()

    def sb_view(tile, ap):
        return View(ap, kb.sb, tile.off, tile.off + tile.nbytes)

    def _sample_attention():
        AS = Alloc(kb, M2END + 1536)
        SCN = AS("SCN", [SC_TOT], F32)
        kb.dma("sp", SCN[:, :], View(d_sc, None, 0, 0))
        QTOK = AS("QTOK", [4, 2, 64], F32)
        KNS = AS("KNS", [2, 64], F32)
        KNW = AS("KNW", [2, 64], F32)
        PTI = AS("PTI", [NS], I32)
        PTF = AS("PTF", [NS], F32)
        IDXF = AS("IDXF", [NS, 4], F32)
        IDX = AS("IDX", [NS, 4], I32)
        QSF = AS("QSF", [4, NS], BF16)
        VNS = AS("VNS", [2, 64], BF16)
        VNW = AS("VNW", [2, 64], BF16)
        assert AS.cur <= ARENA, AS.cur
        kb.copy(QSF[:, :, :], QT[:, :, SEQ:TT])
        kb.copy(VNS[0:NS, :, :], VAS[0:NS, 16, :, 0:64])
        kb.copy(VNW[0:NS, :, :], VAW[0:NS, 16, :, 0:64])
        AL = Alloc(kb, MSCR)
        QB = AL("QB", [4, 64], F32)
        ETS = AL("ETS", [32], BF16)
        RDS = AL("RDS", [1], F32)
        LSEL = AL("LSEL", [8], F32)
        IMPR = AL("IMPR", [257], F32)
        OCS = AL("OCS", [64], F32)
        SCOS = AL("SCOS", [256], F32)
        SC2S = AL("SC2S", [256], F32)
        M8S = AL("M8S", [16], F32)
        IXS = AL("IXS", [16], U32)
        JI = AL("JI", [1], U32)
        JF = AL("JF", [6], F32)
        IDX2F = AL("IDX2F", [2], F32)
        IDX2 = AL("IDX2", [2], I32)
        PTBI = AL("PTBI", [128], I32)
        PTBF = AL("PTBF", [128], F32)
        OH = AL("OH", [256], F32)
        TAB = AL("TAB", [256], F32)
        SCD = AL("SCD", [4, 32], F32)
        ED = AL("ED", [4, 32], F32)
        PARTS = [AL("PART%d" % i, [4, 65], F32) for i in range(2)]
        NUM = AL("NUM", [8, 65], F32)
        SNE = AL("SNE", [8], F32)
        WG8 = AL("WG8", [8], F32)
        assert AL.cur <= MSCR + 16384, AL.cur
        sconst = lambda a, n, p0=0, p1=128: sb_view(SCN, SCN.full[p0:p1, a:a + n])

        def to_tok(src_view, dst_view):
            bt = nb()
            kb.transpose(kb.Pbf(bt, 0, 128, 0, NS), src_view, identb)
            kb.copy(dst_view, View(kb.psum[0:NS, bt, :].bitcast(BF16)[:, 0:128].rearrange("p (g d) -> p g d", g=2), kb.ps, bt * 2048, bt * 2048 + 2048))
        for c in range(4):
            to_tok(QT[:, c, SEQ:TT], QTOK[0:NS, c, :, :])
        to_tok(KST[:, SEQ:TT], KNS[0:NS, :, :])
        to_tok(KWT[:, SEQ:TT], KNW[0:NS, :, :])

        for kv, slot in ((0, 4), (1, 5)):
            kb.dma("sp", kb.dview("swin%d" % kv, o_swin[kv][:, 0:511, :]), View(d_wins[kv][:, 1:512, :], None, 0, 0))
            kb.dma("sp", kb.dview("swin%d" % kv, o_swin[kv][:, 511, :].rearrange("b p -> p b")), kb.dview("kvT", o_kv[:, slot, SEQ:TT]),
                   fn=lambda e, kv=kv, slot=slot: e.dma_start(out=o_swin[kv][:, 511, :].rearrange("b p -> p b"), in_=o_kv[:, slot, SEQ:TT],
                                                             allow_slow_non_contiguous=True))

        AG = Alloc(kb, WA[0].off)
        GA = [AG("GA0", [4096], F32), AG("GA1", [4096], F32)]
        HBS = AG("HBS", [128], BF16)
        EHS = AG("EHS", [128], F32)
        SQb_ = AG("SQbs", [128], BF16)
        QNB_ = AG("QNBs", [128], BF16)
        S1_ = AG("S1s", [128], F32)
        S2_ = AG("S2s", [128], F32)
        S3_ = AG("S3s", [128], F32)
        assert AG.cur <= WB[1].off + WB[1].nbytes, AG.cur
        AX_ = Alloc(kb, H.off)
        XT = [AX_("XT%d" % i, [16, 128], BF16) for i in range(4)]
        assert AX_.cur <= H.off + 4 * TT * 2
        AC = Alloc(kb, MSCR)
        W1S = AC("W1S", [32, 128], BF16)
        ROPC = AC("ROPC", [2, 1024], F32)
        OVS = AC("OVS", [8, 257], BF16)
        KCS = [AC("KCS%d" % b_, [1024], BF16) for b_ in range(NS)]
        VCS = [[AC("VCS%d%d" % (b_, g), [8, 65], BF16) for g in range(2)] for b_ in range(NS)]
        assert AC.cur <= M2END, (AC.cur, M2END)
        assert OVS.off >= MSCR + 16384
        AL2 = Alloc(kb, OVS.off)
        OCT = AL2("OCT", [8, 64], F32)
        TMPQ = AL2("TMPQ", [8, 64], F32)
        assert AL2.cur <= OVS.off + OVS.nbytes
        kb.dma("sp", ROPC[:, :, :], View(d_ropec, None, 0, 0))
        kb.dma("pool", OVS[:, :, :], View(d_ovs, None, 0, 0))
        kb.dma("sp", PTI[:, :], View(d_ptT, None, 0, 0))
        kb.copy(PTF[:, :], PTI[:, :])
        for q4 in range(4):
            kb.ts(IDXF[:, :, q4], PTF[:, :], 4.0, ALU.mult, float(q4), ALU.add)
        kb.copy(IDX[:, :, :], IDXF[:, :, :])
        for b_ in range(NS):
            kb.memset(KCS[b_][:, 1016:1024], 0.0)
            for g in range(2):
                kb.memset(VCS[b_][g][:, :, :], 0.0)
                kb.memset(VCS[b_][g][:, :, 64:65], 1.0)

        def norm_rope_s(ps, w, gcol, cosv, sinv, out_bf):
            kb.act(SQb_[:, 0:w], ps, AF.Square)
            b2 = nb()
            kb.mm(kb.P(b2, 0, w), blk, SQb_[:, 0:w], start=True, stop=True)
            kb.act(S3_[:, 0:w], kb.P(b2, 0, w), AF.Ln, scale=1.0 / HD, bias=EPS)
            kb.act(S3_[:, 0:w], S3_[:, 0:w], AF.Exp, scale=-0.5)
            kb.stt(S1_[:, 0:w], ps, VEC[:, gcol:gcol + 1], S3_[:, 0:w], ALU.mult, ALU.mult)
            kb.act(QNB_[:, 0:w], S1_[:, 0:w], AF.Copy)
            b3 = nb()
            kb.mm(kb.P(b3, 0, w), pmat, QNB_[:, 0:w], start=True, stop=True)
            kb.tt(S2_[:, 0:w], kb.P(b3, 0, w), sinv, ALU.mult)
            kb.tt(S1_[:, 0:w], S1_[:, 0:w], cosv, ALU.mult)
            kb.tt(out_bf, S1_[:, 0:w], S2_[:, 0:w], ALU.add)

        ga_rr = [0]
        xt_slot = lambda ci: 0 if ci == 0 else 1 + (ci - 1) % 3
        for kv in range(2):
            kb.dma("pool", W1S[:, :, :], View(d_w1a[kv], None, 0, 0))
            for b_ in range(NS):
                def do_ci(ci):
                    w = 128 if ci < 7 else 127
                    bkk = nb() if kv == 0 else None
                    if bkk is not None:
                        reserved.add(bkk)
                    for g in range(2):
                        bh = nb()
                        gp = slice(g * 64, (g + 1) * 64)
                        for sp_ in range(32):
                            s16 = sp_ % 16
                            if sp_ < 16:
                                rhs = XT[xt_slot(ci)][gp, s16, 0:w]
                            elif ci < 7:
                                rhs = XT[xt_slot(ci + 1)][gp, s16, 0:w]
                            else:
                                rhs = XT[0][gp, s16, 1:128]
                            kb.mm(kb.P(bh, 0, w), W1S[gp, sp_, :], rhs, start=(sp_ == 0), stop=(sp_ == 31))
                        kb.act(EHS[:, 0:w], kb.P(bh, 0, w), AF.Exp, scale=-1.0, bias=NBIAS[kv][:, 0:1])
                        kb.ts(EHS[:, 0:w], EHS[:, 0:w], 1.0, ALU.add)
                        kb.recip(EHS[:, 0:w], EHS[:, 0:w])
                        kb.stt(HBS[:, 0:w], kb.P(bh, 0, w), BIAS[kv][:, 0:1], EHS[:, 0:w], ALU.add, ALU.mult)
                        if kv == 0:
                            kb.mm(kb.P(bkk, 0, w, g * 64, g * 64 + 64), W2[0][:, :], HBS[:, 0:w], start=True, stop=True)
                        else:
                            b2 = nb()
                            kb.mm(kb.P(b2, 0, 64, 0, w), HBS[:, 0:w], W2[1][:, :], start=True, stop=True)
                            kb.copy(VCS[b_][g][0:w, ci, 0:64], kb.P(b2, 0, 64, 0, w))
                    if kv == 0:
                        hi_ = ci + 8 * (w - 1) + 1
                        norm_rope_s(kb.P(bkk, 0, w), w, NV_QN + 1, ROPC[:, 0, ci:hi_:8], ROPC[:, 1, ci:hi_:8], KCS[b_][:, ci:hi_:8])
                        reserved.discard(bkk)

                for q4 in range(4):
                    ga = GA[ga_rr[0] % 2]
                    ga_rr[0] += 1
                    idxv = IDX[:, b_, q4:q4 + 1]
                    kb.dma("pool", ga[:, :], idxv, fn=lambda e, ga=ga, idxv=idxv, kv=kv: e.indirect_dma_start(
                        out=ga[:, :].ap, out_offset=None, in_=d_pools[kv], in_offset=bass.IndirectOffsetOnAxis(ap=idxv.ap, axis=0)))
                    for cl in range(2):
                        ci = 2 * q4 + cl
                        xt = XT[xt_slot(ci)]
                        for s4 in range(4):
                            bt = nb()
                            for t4 in range(4):
                                t0 = cl * 16 + s4 * 4 + t4
                                kb.transpose(kb.P(bt, t4 * 128, (t4 + 1) * 128), ga[:, t0 * 128:(t0 + 1) * 128], IDF[:, :])
                            kb.copy(sb_view(xt, xt.full[:, s4 * 4:s4 * 4 + 4, :]),
                                    View(kb.psum[:, bt, :].rearrange("p (s n) -> p s n", s=4), kb.ps, bt * 2048, bt * 2048 + 2048),
                                    eng=("act" if s4 % 2 else "dve"))
                        if ci >= 1:
                            do_ci(ci - 1)
                do_ci(7)

        reserved.add(7)
        for b_ in range(NS):
            for g in range(2):
                gp = slice(g * 64, (g + 1) * 64)
                bgi = g * NS + b_
                a = nb()
                for ci in range(8):
                    kb.mm(kb.P(a, ci * 4, ci * 4 + 4), KCS[b_][gp, ci:1024:8], QSF[gp, :, b_], start=(ci == 0), stop=True)
                kb.act(ETS[:, :], kb.P(a, 0, 32), AF.Exp, scale=SCALE)
                kb.tt(ETS[:, :], ETS[:, :], sconst(SC_MK, 32), ALU.mult)
                o1, o2 = nb(), nb()
                for ci in range(8):
                    kb.mm(kb.P(o1, 0, 65, 0, 4), ETS[:, ci * 4:ci * 4 + 4], VCS[b_][g][:, ci, :], start=(ci == 0), stop=(ci == 7))
                for ci in range(8):
                    kb.mm(kb.P(o2, 0, 257, 0, 4), ETS[:, ci * 4:ci * 4 + 4], OVS[:, ci, :], start=(ci == 0), stop=(ci == 7))
                kb.ts(RDS[0:4, :], kb.P(o1, 64, 65, 0, 4), 1e-30, ALU.max)
                kb.recip(RDS[0:4, :], RDS[0:4, :])
                kb.ts(OCS[0:4, :], kb.P(o1, 0, 64, 0, 4), RDS[0:4, 0:1], ALU.mult)
                kb.dma("sp", kb.dview("sc_oc", sc_oc[b_, g]), OCS[0:4, :])
                kb.copy(IMPR[0:4, :], kb.P(o2, 0, 257, 0, 4))
                kb.ts(LSEL[0:4, :], sconst(SC_SELC + bgi * 8, 8, 0, 4), RDS[0:4, 0:1], ALU.mult)
                kb.mm(kb.P(7, 0, 257, 0, 8), LSEL[0:4, :], IMPR[0:4, :], start=(b_ == 0 and g == 0), stop=(b_ == NS - 1 and g == 1))
        kb.tt(SCOS[0:8, :], kb.P(7, 0, 256, 0, 8), sconst(SC_BONS, 256, 0, 8), ALU.add)
        reserved.discard(7)
        kb.op("dve", lambda e: e.max(out=M8S[0:8, 0:8].ap, in_=SCOS[0:8, :].ap), reads=[SCOS[0:8, :]], writes=[M8S[0:8, 0:8]])
        kb.op("dve", lambda e: e.max_index(out=IXS[0:8, 0:8].ap, in_max=M8S[0:8, 0:8].ap, in_values=SCOS[0:8, :].ap),
              reads=[SCOS[0:8, :], M8S[0:8, 0:8]], writes=[IXS[0:8, 0:8]])
        kb.op("dve", lambda e: e.match_replace(out=SC2S[0:8, :].ap, in_to_replace=M8S[0:8, 0:8].ap, in_values=SCOS[0:8, :].ap, imm_value=-3.0e38),
              reads=[SCOS[0:8, :], M8S[0:8, 0:8]], writes=[SC2S[0:8, :]])
        kb.op("dve", lambda e: e.max(out=M8S[0:8, 8:16].ap, in_=SC2S[0:8, :].ap), reads=[SC2S[0:8, :]], writes=[M8S[0:8, 8:16]])
        kb.op("dve", lambda e: e.max_index(out=IXS[0:8, 8:16].ap, in_max=M8S[0:8, 8:16].ap, in_values=SC2S[0:8, :].ap),
              reads=[SC2S[0:8, :], M8S[0:8, 8:16]], writes=[IXS[0:8, 8:16]])
        kb.dma("sp", kb.dview("sc_idx", sc_idx.rearrange("(a r) -> a r", r=16)), IXS[0:8, :])
        kb.dma("sp", JI[:, :], kb.dview("sc_idx", sc_idx.rearrange("(p o) -> p o", o=1)))
        kb.dma("sp", PTBI[:, :], View(d_ptB, None, 0, 0))
        kb.copy(PTBF[:, :], PTBI[:, :])
        kb.copy(JF[:, 0:1], JI[:, :])
        ptb3 = sb_view(PTBF, PTBF.full[:, :].unsqueeze(2).to_broadcast([128, 128, 2]))
        tab3 = sb_view(TAB, TAB.full[:, :].rearrange("p (a h) -> p a h", h=2))
        kb.ts(tab3, ptb3, 4.0, ALU.mult)
        h2b = sb_view(SCN, SCN.full[:, SC_H2:SC_H2 + 2].unsqueeze(1).to_broadcast([128, 128, 2]))
        kb.tt(tab3, tab3, h2b, ALU.add)
        kb.ts(OH[:, :], sconst(SC_IOTA, 256), JF[:, 0:1], ALU.is_equal)
        kb.tt(OH[:, :], OH[:, :], TAB[:, :], ALU.mult)
        kb.reduce(JF[:, 5:6], OH[:, :])
        for pc in range(2):
            kb.ts(IDX2F[:, pc:pc + 1], JF[:, 5:6], float(pc), ALU.add)
        kb.copy(IDX2[:, :], IDX2F[:, :])

        if debug == "samp":
            o_d1 = dout("d_kcs", [128, 1024])
            o_d2 = dout("d_ixs", [8, 16], U32)
            o_d3 = dout("d_imp", [8, 256])
            o_d4 = dout("d_idx2", [128, 2], I32)
            kb.copy(GA[0][:, 0:1024], KCS[0][:, :])
            kb.dma("sp", kb.dview("d1", o_d1), GA[0][:, 0:1024])
            kb.dma("sp", kb.dview("d2", o_d2), IXS[0:8, :])
            kb.dma("sp", kb.dview("d3", o_d3), SCOS[0:8, :])
            kb.dma("sp", kb.dview("d4", o_d4), IDX2[:, :])
        bq = nb()
        for g in range(2):
            kb.mm(kb.P(bq, 0, 256, g * 64, g * 64 + 64), sconst(SC_INDB + g * 64, 64, 0, NS), QTOK[0:NS, :, g, :], start=True, stop=True)
        kb.copy(QB[:, :, :], View(kb.psum[:, bq, 0:256].rearrange("p (h d) -> p h d", h=4), kb.ps, bq * 2048, bq * 2048 + 2048))
        GW = [Tile(kb, "GWk", [32, 64], F32, GA[0].off), Tile(kb, "GWv", [32, 64], F32, GA[1].off)]
        TMPD = Tile(kb, "TMPD", [32, 64], F32, KCS[0].off)
        assert TMPD.nbytes <= 4 * KCS[0].nbytes

        def dve_attend(kview_fn, vview_fn, halves, part, mask_fn):
            for gp_, g in halves:
                kv_ = kview_fn(gp_, g)
                for hh in range(4):
                    qb = sb_view(QB, QB.full[gp_, hh, :].unsqueeze(1).to_broadcast([gp_.stop - gp_.start, 32, 64]))
                    kb.tt(TMPD[gp_, :, :], kv_, qb, ALU.mult)
                    kb.reduce(SCD[gp_, hh, :], TMPD[gp_, :, :])
            kb.act(ED[:, :, :], SCD[:, :, :], AF.Exp, scale=SCALE)
            mask_fn()
            kb.reduce(sb_view(part, part.full[:, :, 64]), ED[:, :, :])
            for gp_, g in halves:
                vv_ = vview_fn(gp_, g)
                n = gp_.stop - gp_.start
                for hh in range(4):
                    eb = sb_view(ED, ED.full[gp_, hh, :].unsqueeze(1).to_broadcast([n, 64, 32]))
                    kb.tt(sb_view(TMPD, TMPD.full[gp_, :, :].rearrange("p t d -> p d t")), vv_, eb, ALU.mult)
                    kb.reduce(part[gp_, hh, 0:64], sb_view(TMPD, TMPD.full[gp_, :, :].rearrange("p t d -> p d t")))

        def finish_branch(part, knew, va_new, bi):
            for g in range(2):
                bn = nb()
                kb.mm(kb.P(bn, 0, 260, 0, NS), sconst(SC_IND2, NS, g * 64, g * 64 + 64), sb_view(part, part.full[g * 64:g * 64 + 64, :, :]),
                      start=True, stop=True)
                kb.copy(NUM[0:NS, g * 4:g * 4 + 4, :], View(kb.psum[0:NS, bn, 0:260].rearrange("p (h w) -> p h w", h=4), kb.ps, bn * 2048, bn * 2048 + 2048))
            knb = sb_view(knew, knew.full[0:NS, :, :].unsqueeze(1).to_broadcast([NS, 4, 2, 64]))
            kb.tt(sb_view(TMPQ, TMPQ.full[0:NS, :, :].rearrange("p (c g) d -> p c g d", c=4)), QTOK[0:NS, :, :, :], knb, ALU.mult)
            kb.reduce(SNE[0:NS, :], TMPQ[0:NS, :, :])
            kb.act(SNE[0:NS, :], SNE[0:NS, :], AF.Exp, scale=SCALE)
            en = sb_view(SNE, SNE.full[0:NS, :].rearrange("p (c g) -> p g c", c=4))
            num3 = sb_view(NUM, NUM.full[0:NS, :, 64].rearrange("p (g c) -> p g c", g=2))
            kb.tt(num3, num3, en, ALU.add)
            vnb = sb_view(va_new, va_new.full[0:NS, :, :].unsqueeze(2).to_broadcast([NS, 2, 4, 64]))
            enb = sb_view(SNE, SNE.full[0:NS, :].rearrange("p (c g) -> p g c", c=4).unsqueeze(3).to_broadcast([NS, 2, 4, 64]))
            t4 = sb_view(TMPQ, TMPQ.full[0:NS, :, :].rearrange("p (g c) d -> p g c d", g=2))
            kb.tt(t4, vnb, enb, ALU.mult)
            kb.tt(NUM[0:NS, :, 0:64], NUM[0:NS, :, 0:64], TMPQ[0:NS, :, :], ALU.add)
            kb.recip(WG8[0:NS, :], sb_view(NUM, NUM.full[0:NS, :, 64]))
            kb.tt(WG8[0:NS, :], WG8[0:NS, :], sb_view(GT, GT.full[0:NS, 16, bi:24:3]), ALU.mult)
            wgb = sb_view(WG8, WG8.full[0:NS, :].unsqueeze(2).to_broadcast([NS, 8, 64]))
            kb.tt(TMPQ[0:NS, :, :], NUM[0:NS, :, 0:64], wgb, ALU.mult)
            o3 = sb_view(OTOK, OTOK.full[0:NS, 0, :].rearrange("p (h d) -> p h d", h=8))
            kb.tt(o3, o3, TMPQ[0:NS, :, :], ALU.add)

        kb.dma("sp", OCT[0:NS, :, :], kb.dview("sc_oc", sc_oc.rearrange("b g h d -> b (g h) d")))
        g0b = sb_view(GT, GT.full[0:NS, 16, 0:24:3].unsqueeze(2).to_broadcast([NS, 8, 64]))
        kb.tt(sb_view(OTOK, OTOK.full[0:NS, 0, :].rearrange("p (h d) -> p h d", h=8)), OCT[0:NS, :, :], g0b, ALU.mult)

        dbg_n = [0]
        def dump_otok():
            if debug == "samp":
                o_ = dout("d_otok%d" % dbg_n[0], [NS, 512])
                dbg_n[0] += 1
                kb.dma("sp", kb.dview("dotok%d" % dbg_n[0], o_), OTOK[0:NS, 0, :])
        dump_otok()
        all_halves = [(slice(0, 64), 0), (slice(64, 128), 1)]
        for pc in range(2):
            idxv = IDX2[:, pc:pc + 1]
            for t_, pool_i in ((GA[0], 2), (GA[1], 3)):
                kb.dma("pool", t_[:, :], idxv, fn=lambda e, t_=t_, idxv=idxv, pool_i=pool_i: e.indirect_dma_start(
                    out=t_[:, :].ap, out_offset=None, in_=d_pools[pool_i], in_offset=bass.IndirectOffsetOnAxis(ap=idxv.ap, axis=0)))
            kfn = lambda gp_, g: sb_view(GA[0], GA[0].full[gp_, :].rearrange("p (t g d) -> p t g d", g=2, d=64)[:, :, g, :])
            vfn = lambda gp_, g: sb_view(GA[1], GA[1].full[gp_, :].rearrange("p (t g d) -> p d g t", g=2, d=64)[:, :, g, :])
            mfn = lambda: kb.ts(ED[:, :, :], ED[:, :, :], sconst(SC_MSKR, 1), ALU.mult)
            dve_attend(kfn, vfn, all_halves, PARTS[pc], mfn)
        kb.tt(PARTS[0][:, :, :], PARTS[0][:, :, :], PARTS[1][:, :, :], ALU.add)
        finish_branch(PARTS[0], KNS, VNS, 1)
        dump_otok()

        for kv in range(2):
            for g in range(2):
                src = d_wins[kv].rearrange("b (c t) (g d) -> g (b c) t d", c=16, g=2)[g]
                kb.dma("sp", GW[kv][g * 64:(g + 1) * 64, :, :], View(src, None, 0, 0))
        kfn = lambda gp_, g: GW[0][gp_, :, :]
        vfn = lambda gp_, g: sb_view(GW[1], GW[1].full[gp_, :, :].rearrange("p t d -> p d t"))
        def mfn_w():
            mb = sb_view(SCN, SCN.full[:, SC_MSKW:SC_MSKW + 32].unsqueeze(1).to_broadcast([128, 4, 32]))
            kb.tt(ED[:, :, :], ED[:, :, :], mb, ALU.mult)
        dve_attend(kfn, vfn, [(slice(0, 128), 0)], PARTS[1], mfn_w)
        finish_branch(PARTS[1], KNW, VNW, 2)
        dump_otok()
        out_norm_to_H(NS, 1, SEQ)

    def mixer_out():
        for half in range(2):
            kb.dma("pool", WA[half][:, :, :], View(d_wout[half], None, 0, 0))
        for half in range(2):
            for dch in range(4):
                dk = half * 4 + dch
                for (c0, w) in COLT:
                    b = nb()
                    for kc in range(8):
                        rhs = CN[:, kc, c0:c0 + w] if kc < 4 else H[:, kc, c0:c0 + w]
                        kb.mm(kb.P(b, 0, w), WA[half][:, kc, dch * 128:(dch + 1) * 128], rhs, start=(kc == 0), stop=(kc == 7))
                    kb.tt(X[:, dk, c0:c0 + w], X[:, dk, c0:c0 + w], kb.P(b, 0, w), ALU.add)

    _sample_attention()
    mixer_out()
    ffn(1, NV_FFN2)
    A7 = Alloc(kb, SCR)
    rmsnorm(A7, X, lambda k, c0, w: X[:, k, c0:c0 + w], NV_FIN)
    for k in range(8):
        kb.dma("sp", kb.dview("yT", o_y[:, k, :]), X[:, k, :])
    kb.finish()
    return nc


def _tile_up(w):
    w = w.reshape(8, 128, 2, 11, 256)
    return np.ascontiguousarray(w.transpose(3, 1, 0, 2, 4))


def _tile_dn(w):
    w = w.reshape(22, 128, 4, 256)
    return np.ascontiguousarray(w.transpose(2, 1, 0, 3))


def _fm(v, nchunk):
    return np.ascontiguousarray(v.reshape(nchunk, 128).T)


def prep_shared(inp):
    f = lambda k: np.asarray(inp[k], dtype=np.float32)[0]
    sh = {}
    sh["ffn1_up"] = _tile_up(f("ffn1_w_in"))
    sh["ffn2_up"] = _tile_up(f("ffn2_w_in"))
    sh["ffn1_dn"] = _tile_dn(f("ffn1_w_out"))
    sh["ffn2_dn"] = _tile_dn(f("ffn2_w_out"))
    w_in = f("w_in")
    cols = list(range(1024))
    for c in range(4):
        cols += list(range(1024 + c * 64, 1024 + c * 64 + 64)) + list(range(1024 + (4 + c) * 64, 1024 + (4 + c) * 64 + 64))
    cols += list(range(1536, N_IN))
    wp = np.zeros((1024, 2560), np.float32)
    wp[:, :N_IN] = w_in[:, cols]
    sh["w_in_t"] = np.ascontiguousarray(wp.reshape(8, 128, 5, 512).transpose(2, 1, 0, 3))
    sh["w_out_t"] = np.ascontiguousarray(f("w_out").reshape(8, 128, 2, 512).transpose(2, 1, 0, 3))
    vec = np.zeros((128, NV_TOT), np.float32)
    vec[:, NV_FFN1:NV_FFN1 + 8] = _fm(f("ffn1_norm"), 8)
    vec[:, NV_MIX:NV_MIX + 8] = _fm(f("mix_norm"), 8)
    vec[:, NV_FFN2:NV_FFN2 + 8] = _fm(f("ffn2_norm"), 8)
    vec[:, NV_FIN:NV_FIN + 8] = _fm(f("final_norm"), 8)
    cw = f("conv_w")
    vec[:, NV_CONVW:NV_CONVW + 124] = cw.reshape(31, 4, 128).transpose(2, 1, 0).reshape(128, 124)
    vec[:, NV_CONVB:NV_CONVB + 4] = _fm(f("conv_b"), 4)
    vec[:, NV_LNG:NV_LNG + 4] = _fm(f("conv_ln_g"), 4)
    vec[:, NV_LNB:NV_LNB + 4] = _fm(f("conv_ln_b"), 4)
    vec[:, NV_ONC:NV_ONC + 4] = _fm(f("out_norm_conv"), 4)
    vec[:, NV_ONA:NV_ONA + 4] = _fm(f("out_norm_attn"), 4)
    for i, k in enumerate(["q_norm", "k_cmp_norm", "k_sel_norm", "k_win_norm"]):
        vec[:, NV_QN + i] = np.tile(f(k), 2)
    sh["vec"] = vec
    sh["cb"] = _consts()
    sh["idf"] = np.eye(128, dtype=np.float32)
    pos = np.concatenate([np.arange(SEQ), np.full(NS, PAST)]).astype(np.float32)
    sh["rope_tok"] = _rope_tab(pos)
    sh["rope_cmp"] = _rope_tab((np.arange(1024) * 16 + 31).astype(np.float32))
    sh["bonus_p"] = _bonus_prompt()
    for nm, key in (("cmpk", "cmp_k"), ("cmpv", "cmp_v")):
        w1 = f(key + "_w1")
        a = np.ascontiguousarray(w1.transpose(1, 0, 2))
        sh[nm + "_w1a"] = np.concatenate([a, a], axis=0)
        pe = np.ascontiguousarray(f(key + "_pos").T)
        sh[nm + "_pea"] = np.concatenate([pe, pe], axis=0)
        sh[nm + "_w2"] = f(key + "_w2")
    sh["sconst"] = _sconst()
    sh["ovs"] = _ovs()
    for nm, key in (("pool_kc", "cache_k_cmp"), ("pool_vc", "cache_v_cmp"), ("pool_ks", "cache_k_sel"), ("pool_vs", "cache_v_sel")):
        sh[nm] = np.asarray(inp[key], dtype=np.float32).reshape(N_POOL * 4, 4096)
    return sh


def prep_core(inp, c):
    xp = np.asarray(inp["x_prompt"], dtype=np.float32)[c]
    xs = np.asarray(inp["x_sample"], dtype=np.float32)[NS * c:NS * c + NS, 0]
    x = np.concatenate([xp, xs], axis=0)
    m = {"xT": np.ascontiguousarray(x.T.reshape(8, 128, TT).transpose(1, 0, 2))}
    sc = np.asarray(inp["state_conv"], dtype=np.float32)[0, NS * c:NS * c + NS]
    m["sconvT"] = np.ascontiguousarray(sc.reshape(NS, 30, 4, 128).transpose(3, 2, 0, 1))
    pt = np.asarray(inp["page_table"], dtype=np.int32)[NS * c:NS * c + NS]
    m["ptT"] = np.ascontiguousarray(pt.T)
    p = np.arange(128)
    m["ptB"] = np.ascontiguousarray(pt[(p // 16) % 4])
    m["win_k"] = np.asarray(inp["state_k_win"], dtype=np.float32)[0, NS * c:NS * c + NS].reshape(NS, 512, 128)
    m["win_v"] = np.asarray(inp["state_v_win"], dtype=np.float32)[0, NS * c:NS * c + NS].reshape(NS, 512, 128)
    return m


def assemble(results):
    n = len(results)
    yp, ys = [], []
    kv = [[] for _ in range(6)]
    skv = [[] for _ in range(4)]
    pw = [[], []]
    sw = [[], []]
    pconv, sconv = [], []
    for r in results:
        yT = np.asarray(r["yT"])
        y = yT.transpose(2, 1, 0).reshape(TT, D_MODEL)
        yp.append(y[:SEQ])
        ys.append(y[SEQ:])
        kvT = np.asarray(r["kvT"])
        for i in range(6):
            t = kvT[:, i, :].T
            if i < 4:
                kv[i].append(t[:SEQ].reshape(SEQ, 2, HD))
                skv[i].append(t[SEQ:].reshape(NS, 1, 2, HD))
            else:
                pw[i - 4].append(t[SEQ - 512:SEQ].reshape(512, 2, HD))
        for i in range(2):
            sw[i].append(np.asarray(r["swin_k" if i == 0 else "swin_v"]).reshape(NS, 512, 2, HD))
        pconv.append(np.asarray(r["pconvT"]).transpose(2, 1, 0).reshape(30, 512))
        sconv.append(np.asarray(r["sconvT_out"]).transpose(2, 3, 1, 0).reshape(NS, 30, 512))
    f32 = lambda a: np.ascontiguousarray(a, dtype=np.float32)
    outs = [f32(np.stack(yp)), f32(np.concatenate(ys)[:, None, :])]
    outs += [f32(np.stack(kv[i])[None]) for i in range(4)]
    outs += [f32(np.stack(pw[i])[None]) for i in range(2)]
    outs += [f32(np.stack(pconv)[None])]
    outs += [f32(np.concatenate(skv[i])[None]) for i in range(4)]
    outs += [f32(np.concatenate(sw[i])[None]) for i in range(2)]
    outs += [f32(np.concatenate(sconv)[None])]
    return tuple(outs)


def kernel(**inputs):
    n = 8
    sh = prep_shared(inputs)
    in_maps = []
    for c in range(n):
        m = dict(sh)
        m.update(prep_core(inputs, c))
        in_maps.append(m)
    nc = build_program()
    res = run_bass_kernel_spmd(nc, in_maps, core_ids=list(range(n)))
    return assemble(res.results)
```

```python
import numpy as np
import ml_dtypes
import concourse.bass as bass
import concourse.mybir as mybir
from concourse.bass_utils import run_bass_kernel_spmd

F32 = mybir.dt.float32
BF16 = mybir.dt.bfloat16
I32 = mybir.dt.int32
U32 = mybir.dt.uint32
U8 = mybir.dt.uint8
AF = mybir.ActivationFunctionType
ALU = mybir.AluOpType
AX = mybir.AxisListType

D_MODEL = 1024
SEQ = 2048
NS = 4
TT = SEQ + NS
D_FF = 2816
NFC = D_FF // 128
HD = 64
N_HEADS = 8
PAST = 16384
PAGE = 128
NPAGES = PAST // PAGE
N_POOL = 5120
EPS = 1e-6
SCALE = HD ** -0.5
N_IN = 2328
CONV_W = 31
NCMP_P = 127
NCMP_S = 1023
NSEL_P = 32
ROPE_THETA = 500000.0
ESZ = {F32: 4, BF16: 2, I32: 4, U32: 4, U8: 1}

COLT = [(0, 512), (512, 512), (1024, 512), (1536, 512), (2048, NS)]


class Prod:
    def __init__(self, name, sem, inc):
        self.name, self.sem, self.inc, self.count = name, sem, inc, 0


class Space:
    def __init__(self, name):
        self.name = name
        self.segs = []

    def touch(self, lo, hi, write, deps):
        segs = self.segs
        out = []
        inside = []
        cur = lo
        for s in segs:
            slo, shi, w, rd = s
            if shi <= lo or slo >= hi:
                out.append(s)
                continue
            if slo < lo:
                out.append([slo, lo, w, dict(rd)])
            a, b = max(slo, lo), min(shi, hi)
            if a > cur:
                ns = [cur, a, None, {}]
                out.append(ns)
                inside.append(ns)
            ns = [a, b, w, dict(rd)]
            out.append(ns)
            inside.append(ns)
            cur = b
            if shi > hi:
                out.append([hi, shi, w, dict(rd)])
        if cur < hi:
            ns = [cur, hi, None, {}]
            out.append(ns)
            inside.append(ns)
        out.sort(key=lambda s: s[0])
        self.segs = out
        for s in inside:
            if s[2] is not None:
                p, t = s[2]
                if deps.get(p, 0) < t:
                    deps[p] = t
            if write:
                for p, t in s[3].items():
                    if deps.get(p, 0) < t:
                        deps[p] = t
        return inside

    def mark(self, lo, hi, write, who):
        inside = self.touch(lo, hi, write, {})
        if write:
            keep = [s for s in self.segs if s[1] <= lo or s[0] >= hi]
            keep.append([lo, hi, who, {}])
            keep.sort(key=lambda s: s[0])
            self.segs = keep
        else:
            p, t = who
            for s in inside:
                if s[3].get(p, 0) < t:
                    s[3][p] = t


class View:
    def __init__(self, ap, space, lo, hi):
        self.ap, self.space, self.lo, self.hi = ap, space, lo, hi


class Tile:
    def __init__(self, kb, name, shape, dtype, off, parts=128):
        self.kb, self.name, self.shape, self.dtype, self.off = kb, name, tuple(shape), dtype, off
        self.esz = ESZ[dtype]
        n = int(np.prod(shape))
        self.nbytes = n * self.esz
        ap = kb.arena[0:parts, off:off + self.nbytes].bitcast(dtype)
        if len(shape) == 2:
            ap = ap.rearrange("p (a b) -> p a b", a=shape[0])
        elif len(shape) == 3:
            ap = ap.rearrange("p (a b c) -> p a b c", a=shape[0], b=shape[1])
        elif len(shape) == 4:
            ap = ap.rearrange("p (a b c d) -> p a b c d", a=shape[0], b=shape[1], c=shape[2])
        self.full = ap
        st = []
        acc = 1
        for s in reversed(shape):
            st.append(acc)
            acc *= s
        self.strides = list(reversed(st))

    def __getitem__(self, idx):
        if not isinstance(idx, tuple):
            idx = (idx,)
        pidx = idx[0]
        fidx = list(idx[1:]) + [slice(None)] * (len(self.shape) - len(idx) + 1)
        lo = 0
        hi = 0
        for i, (ix, n, s) in enumerate(zip(fidx, self.shape, self.strides)):
            if isinstance(ix, int):
                a, b = ix, ix + 1
            else:
                a = 0 if ix.start is None else ix.start
                b = n if ix.stop is None else ix.stop
                step = 1 if ix.step is None else ix.step
                b = a + ((b - a - 1) // step) * step + 1
            assert 0 <= a < b <= n, (self.name, idx, self.shape)
            lo += a * s
            hi += (b - 1) * s
        hi += 1
        ap = self.full[(pidx,) + tuple(fidx)]
        return View(ap, self.kb.sb, self.off + lo * self.esz, self.off + hi * self.esz)

    def all(self):
        return self[:]


class KB:
    def __init__(self, nc):
        self.nc = nc
        self.sb = Space("sbuf")
        self.ps = Space("psum")
        self.dram = {}
        self.eng = {}
        for name, h in [("pe", nc.tensor), ("act", nc.scalar), ("dve", nc.vector), ("pool", nc.gpsimd), ("sp", nc.sync)]:
            sem = nc.semaphore("sem_" + name).__enter__()
            p = Prod(name, sem, 1)
            p.h = h
            p.waited = {}
            self.eng[name] = p
        self.dsem = {"sp": [], "pool": [], "act": []}
        for q, n in [("sp", 12), ("pool", 10), ("act", 2)]:
            for i in range(n):
                sem = nc.semaphore(f"dsem_{q}{i}").__enter__()
                self.dsem[q].append(Prod(f"d{q}{i}", sem, 16))
        self.drr = {"sp": 0, "pool": 0, "act": 0}
        self.arena = nc.sbuf_tensor("arena", [128, ARENA], U8).__enter__()
        self.psum = nc.psum_tensor("psum", [128, 8, 512], F32).__enter__()
        self.n_ops = 0

    def P(self, bank, a=0, b=512, p0=0, p1=128):
        return View(self.psum[p0:p1, bank, a:b], self.ps, bank * 2048, bank * 2048 + 2048)

    def Pbf(self, bank, a=0, b=1024, p0=0, p1=128):
        return View(self.psum[p0:p1, bank, :].bitcast(BF16)[:, a:b], self.ps, bank * 2048, bank * 2048 + 2048)

    def dview(self, name, ap):
        sp = self.dram.setdefault(name, Space(name))
        return View(ap, sp, 0, 1)

    def _sync(self, E, reads, writes, skip_self=False):
        deps = {}
        for v in reads:
            if v is not None and v.space is not None:
                v.space.touch(v.lo, v.hi, v.space is self.ps, deps)
        for v in writes:
            if v.space is not None:
                v.space.touch(v.lo, v.hi, True, deps)
        for P, t in deps.items():
            if P is E and skip_self:
                continue
            if E.waited.get(P, 0) < t:
                E.h.wait_ge(P.sem, t)
                E.waited[P] = t

    def _mark(self, who, reads, writes):
        for v in reads:
            if v is not None and v.space is not None:
                v.space.mark(v.lo, v.hi, v.space is self.ps, who)
        for v in writes:
            if v.space is not None:
                v.space.mark(v.lo, v.hi, True, who)

    def op(self, eng, fn, reads=(), writes=()):
        E = self.eng[eng]
        self._sync(E, reads, writes, skip_self=(eng == "pe"))
        inst = fn(E.h)
        E.count += 1
        inst.then_inc(E.sem, 1)
        self._mark((E, E.count), reads, writes)
        self.n_ops += 1
        return inst

    def dma(self, q, out, in_, fn=None):
        Q = self.eng[q]
        reads = [in_]
        writes = [out]
        self._sync(Q, reads, writes)
        lst = self.dsem[q]
        S = lst[self.drr[q] % len(lst)]
        self.drr[q] += 1
        if Q.waited.get(S, 0) < S.count:
            Q.h.wait_ge(S.sem, S.count)
            Q.waited[S] = S.count
        if fn is None:
            inst = Q.h.dma_start(out=out.ap, in_=in_.ap)
        else:
            inst = fn(Q.h)
        S.count += 16
        inst.then_inc(S.sem, 16)
        self._mark((S, S.count), reads, writes)
        self.n_ops += 1

    def finish(self):
        E = self.eng["sp"]
        for q in self.dsem:
            for S in self.dsem[q]:
                if S.count > 0 and E.waited.get(S, 0) < S.count:
                    E.h.wait_ge(S.sem, S.count)
                    E.waited[S] = S.count
        for name, P in self.eng.items():
            if name != "sp" and P.count > 0:
                E.h.wait_ge(P.sem, P.count)

    def mm(self, out, lhsT, rhs, start, stop, **kw):
        self.op("pe", lambda e: e.matmul(out.ap, lhsT=lhsT.ap, rhs=rhs.ap, start=start, stop=stop,
                                         skip_group_check=True, **kw), reads=[lhsT, rhs], writes=[out])

    def transpose(self, out, in_, ident):
        self.op("pe", lambda e: e.transpose(out=out.ap, in_=in_.ap, identity=ident.ap), reads=[in_, ident], writes=[out])

    def act(self, out, in_, func, scale=1.0, bias=0.0, accum=None):
        reads = [in_]
        kw = {}
        if isinstance(scale, View):
            reads.append(scale)
            kw["scale"] = scale.ap
        else:
            kw["scale"] = float(scale)
        if isinstance(bias, View):
            reads.append(bias)
            kw["bias"] = bias.ap
        else:
            kw["bias"] = float(bias)
        writes = [out]
        if accum is not None:
            writes.append(accum)
            kw["accum_out"] = accum.ap
        self.op("act", lambda e: e.activation(out=out.ap, in_=in_.ap, func=func, **kw), reads=reads, writes=writes)

    def tt(self, out, a, b, op, eng="dve"):
        self.op(eng, lambda e: e.tensor_tensor(out=out.ap, in0=a.ap, in1=b.ap, op=op), reads=[a, b], writes=[out])

    def ts(self, out, a, s1, op0, s2=None, op1=None, eng="dve", accum=None):
        reads = [a]
        k1 = s1.ap if isinstance(s1, View) else float(s1)
        if isinstance(s1, View):
            reads.append(s1)
        k2 = None
        if s2 is not None:
            k2 = s2.ap if isinstance(s2, View) else float(s2)
            if isinstance(s2, View):
                reads.append(s2)
        writes = [out]
        kw = {}
        if accum is not None:
            writes.append(accum)
            kw["accum_out"] = accum.ap
        if op1 is None:
            self.op(eng, lambda e: e.tensor_scalar(out=out.ap, in0=a.ap, scalar1=k1, scalar2=None, op0=op0, **kw),
                    reads=reads, writes=writes)
        else:
            self.op(eng, lambda e: e.tensor_scalar(out=out.ap, in0=a.ap, scalar1=k1, scalar2=k2, op0=op0, op1=op1, **kw),
                    reads=reads, writes=writes)

    def stt(self, out, a, s, b, op0, op1):
        reads = [a, b]
        k = s.ap if isinstance(s, View) else float(s)
        if isinstance(s, View):
            reads.append(s)
        self.op("dve", lambda e: e.scalar_tensor_tensor(out=out.ap, in0=a.ap, scalar=k, in1=b.ap, op0=op0, op1=op1),
                reads=reads, writes=[out])

    def copy(self, out, in_, eng="dve"):
        if eng == "act":
            self.op("act", lambda e: e.copy(out=out.ap, in_=in_.ap), reads=[in_], writes=[out])
        else:
            self.op(eng, lambda e: e.tensor_copy(out=out.ap, in_=in_.ap), reads=[in_], writes=[out])

    def memset(self, out, val, eng="dve"):
        self.op(eng, lambda e: e.memset(out.ap, val), reads=[], writes=[out])

    def recip(self, out, in_):
        self.op("dve", lambda e: e.reciprocal(out=out.ap, in_=in_.ap), reads=[in_], writes=[out])

    def reduce(self, out, in_, op=ALU.add, axis=AX.X):
        self.op("dve", lambda e: e.tensor_reduce(out=out.ap, in_=in_.ap, axis=axis, op=op), reads=[in_], writes=[out])


ARENA = 207 * 1024


NV_FFN1, NV_MIX, NV_FFN2, NV_FIN = 0, 8, 16, 24
NV_CONVW = 32
NV_CONVB = NV_CONVW + 124
NV_LNG = NV_CONVB + 4
NV_LNB = NV_LNG + 4
NV_ONC = NV_LNB + 4
NV_ONA = NV_ONC + 4
NV_QN = NV_ONA + 4
NV_TOT = NV_QN + 4

CB_ONES, CB_ID, CB_BLK, CB_PM = 0, 128, 256, 384
CB_E = 512
CB_CM = CB_E + 2048
CB_CMP = CB_CM + 8 * 512
CB_OVP = CB_CMP + 2048
CB_TOT = CB_OVP + 33


def _consts():
    cb = np.zeros((128, CB_TOT), np.float32)
    cb[:, CB_ONES:CB_ONES + 128] = 1.0
    cb[:, CB_ID:CB_ID + 128] = np.eye(128, dtype=np.float32)
    blk = np.zeros((128, 128), np.float32)
    blk[:64, :64] = 1
    blk[64:, 64:] = 1
    cb[:, CB_BLK:CB_BLK + 128] = blk
    pm = np.zeros((128, 128), np.float32)
    for base in (0, 64):
        for i in range(8):
            pm[base + i + 8, base + i] = -1.0
            pm[base + i, base + i + 8] = 1.0
    cb[:, CB_PM:CB_PM + 128] = pm
    k = np.arange(2048)
    for j in range(32):
        cb[j, CB_E:CB_E + 2048] = (k // 64 == j)
    kk = np.arange(128)[:, None]
    qq = np.arange(512)[None, :]
    for r in range(4):
        cb[:, CB_CM + r * 512:CB_CM + (r + 1) * 512] = (kk + 128 * r <= qq)
    for r in range(1, 5):
        cb[:, CB_CM + (3 + r) * 512:CB_CM + (4 + r) * 512] = (qq - kk + 128 * r < 512)
    n = np.arange(128)[:, None]
    q = np.arange(2048)[None, :]
    cb[:, CB_CMP:CB_CMP + 2048] = ((16 * n + 31 <= q) & (n < 127))
    cs = np.arange(127)[:, None] * 16
    ss = np.arange(32)[None, :] * 64
    ov = np.clip(np.minimum(cs + 32, ss + 64) - np.maximum(cs, ss), 0, None).astype(np.float32) / 32
    cb[:127, CB_OVP] = 1.0
    cb[:127, CB_OVP + 1:CB_OVP + 33] = ov
    return cb


def _rope_tab(pos):
    half = 8
    inv = (np.float32(ROPE_THETA) ** (-(np.arange(half, dtype=np.float32) * np.float32(2.0) / np.float32(16)))).astype(np.float32)
    ang = pos.astype(np.float32)[:, None] * inv[None, :]
    cos = np.cos(ang).astype(np.float32)
    sin = np.sin(ang).astype(np.float32)
    n = pos.shape[0]
    tab = np.zeros((128, 2, n), np.float32)
    tab[:, 0, :] = 1.0
    for base in (0, 64):
        for i in range(8):
            tab[base + i, 0] = cos[:, i]
            tab[base + i + 8, 0] = cos[:, i]
            tab[base + i, 1] = sin[:, i]
            tab[base + i + 8, 1] = sin[:, i]
    return tab


SC_SELC = 0
SC_BONS = SC_SELC + 64
SC_MK = SC_BONS + 256
SC_MSKR = SC_MK + 32
SC_MSKW = SC_MSKR + 1
SC_IND2 = SC_MSKW + 32
SC_INDB = SC_IND2 + 4
SC_IOTA = SC_INDB + 128
SC_H2 = SC_IOTA + 256
SC_TOT = SC_H2 + 2


def _sconst():
    sc = np.zeros((128, SC_TOT), np.float32)
    for bg in range(8):
        sc[0:4, SC_SELC + bg * 8 + bg] = 1.0
    sc[0:8, SC_BONS + 0] = 1e4
    sc[0:8, SC_BONS + 255] = 1e4
    sc[:, SC_MK:SC_MK + 32] = 1.0
    sc[127, SC_MK + 28:SC_MK + 32] = 0.0
    p = np.arange(128)
    r = p % 16
    b = (p // 16) % 4
    sc[:, SC_MSKR] = (r != 15)
    sc[:, SC_MSKW:SC_MSKW + 32] = 1.0
    sc[r == 0, SC_MSKW] = 0.0
    for bb in range(4):
        sc[:, SC_IND2 + bb] = (b == bb)
        sc[bb, SC_INDB:SC_INDB + 128] = (b == bb)
    sc[:, SC_IOTA:SC_IOTA + 256] = np.arange(256)[None, :]
    sc[:, SC_H2 + 1] = 2.0
    return sc


def _ovs():
    cs = np.arange(1024)[:, None] * 16
    ss = np.arange(257)[None, :] * 64
    ov = np.clip(np.minimum(cs + 32, ss + 64) - np.maximum(cs, ss), 0, None).astype(np.float32) / 32
    ov[1023] = 0.0
    return np.ascontiguousarray(ov.reshape(128, 8, 257))


def _bonus_prompt():
    q = np.arange(2048)
    cur = q // 64
    j = np.arange(32)[None, :]
    valid = j <= cur[:, None]
    forced = (j == 0) | (j == cur[:, None]) | (j == cur[:, None] - 1)
    b = np.where(valid, np.where(forced, 1e4, 0.0), -1e30).astype(np.float32)
    return np.ascontiguousarray(b.reshape(16, 128, 32).transpose(1, 0, 2))


class Alloc:
    def __init__(self, kb, base=0):
        self.kb, self.cur, self.peak = kb, base, base

    def __call__(self, name, shape, dtype, parts=128):
        self.cur = (self.cur + 31) // 32 * 32
        t = Tile(self.kb, name, shape, dtype, self.cur, parts)
        self.cur += t.nbytes
        self.peak = max(self.peak, self.cur)
        assert self.cur <= ARENA, (name, self.cur)
        return t


def build_program(debug=None):
    nc = bass.Bass("TRN2", target_bir_lowering=False)
    kb = KB(nc)
    dbg_outs = {}

    def din(name, shape, dtype=F32):
        return nc.dram_tensor(name, list(shape), dtype, kind="ExternalInput").ap()

    def dout(name, shape, dtype=F32):
        return nc.dram_tensor(name, list(shape), dtype, kind="ExternalOutput").ap()

    d_xT = din("xT", [128, 8, TT])
    d_up = [din("ffn1_up", [11, 128, 8, 2, 256]), din("ffn2_up", [11, 128, 8, 2, 256])]
    d_dn = [din("ffn1_dn", [4, 128, 22, 256]), din("ffn2_dn", [4, 128, 22, 256])]
    d_win = din("w_in_t", [5, 128, 8, 512])
    d_wout = din("w_out_t", [2, 128, 8, 512])
    d_vec = din("vec", [128, NV_TOT])
    d_cb = din("cb", [128, CB_TOT])
    d_idf = din("idf", [128, 128])
    d_rope = din("rope_tok", [128, 2, TT])
    d_ropec = din("rope_cmp", [128, 2, 1024])
    d_bonus = din("bonus_p", [128, 16, 32])
    d_w1a = [din("cmpk_w1a", [128, 32, 128]), din("cmpv_w1a", [128, 32, 128])]
    d_pea = [din("cmpk_pea", [128, 32]), din("cmpv_pea", [128, 32])]
    d_w2 = [din("cmpk_w2", [128, 64]), din("cmpv_w2", [128, 64])]

    o_y = dout("yT", [128, 8, TT])
    o_kv = dout("kvT", [128, 6, TT])
    o_pconv = dout("pconvT", [128, 4, 30])

    A = Alloc(kb)
    X = A("X", [8, TT], F32)
    H = A("H", [8, TT], BF16)
    VEC = A("VEC", [NV_TOT], F32)
    CB = A("CB", [512], BF16)
    IDF = A("IDF", [128], F32)
    WA = [A("WA0", [8, 512], BF16), A("WA1", [8, 512], BF16)]
    WB = [A("WB0", [22, 256], BF16), A("WB1", [22, 256], BF16)]
    SCR = A.cur

    ones = CB[:, CB_ONES:CB_ONES + 128]
    identb = CB[:, CB_ID:CB_ID + 128]
    blk = CB[:, CB_BLK:CB_BLK + 128]
    pmat = CB[:, CB_PM:CB_PM + 128]

    bank_rr = [0]

    reserved = set()

    def nb():
        while True:
            b = bank_rr[0] % 8
            bank_rr[0] += 1
            if b not in reserved:
                return b

    for k in range(8):
        kb.dma("sp", X[:, k, :], View(d_xT[:, k, :], None, 0, 0))
    kb.dma("sp", VEC[:, :], View(d_vec, None, 0, 0))
    kb.dma("sp", IDF[:, :], View(d_idf, None, 0, 0))
    kb.dma("pool", CB[:, 0:512], View(d_cb[:, 0:512], None, 0, 0))

    def rmsnorm(A2, src, dst_fn, gcol, ntile_list=COLT):
        SQ = A2("SQ", [8, 512], BF16)
        LNV = A2("LNV", [512], F32)
        RSTD = A2("RSTD", [512], F32)
        for (c0, w) in ntile_list:
            b = nb()
            for k in range(8):
                kb.act(SQ[:, k, 0:w], src[:, k, c0:c0 + w], AF.Square)
            for k in range(8):
                kb.mm(kb.P(b, 0, w), ones, SQ[:, k, 0:w], start=(k == 0), stop=(k == 7))
            kb.act(LNV[:, 0:w], kb.P(b, 0, w), AF.Ln, scale=1.0 / D_MODEL, bias=EPS)
            kb.act(RSTD[:, 0:w], LNV[:, 0:w], AF.Exp, scale=-0.5)
            for k in range(8):
                kb.stt(dst_fn(k, c0, w), src[:, k, c0:c0 + w], VEC[:, gcol + k:gcol + k + 1], RSTD[:, 0:w], ALU.mult, ALU.mult)

    def ffn(idx, gcol):
        A2 = Alloc(kb, SCR)
        G = A2("G", [NFC, 1028], BF16)
        SA = [A2("SA0", [512], BF16), A2("SA1", [512], BF16)]
        rmsnorm(A2, X, lambda k, c0, w: H[:, k, c0:c0 + w], gcol)
        halves = [[COLT[0], COLT[1], COLT[4]], [COLT[2], COLT[3]]]
        cnt_a = [0]
        for hi_, tiles in enumerate(halves):
            loc = {}
            o = 0
            for (c0, w) in tiles:
                loc[c0] = o
                o += w
            def load_up(g):
                slot = WA[g % 2]
                kb.dma("pool", slot[:, :, :], View(d_up[idx][g].rearrange("p a b c -> p a (b c)"), None, 0, 0))
            load_up(0)
            for g in range(11):
                if g + 1 < 11:
                    load_up(g + 1)
                slot = WA[g % 2]
                for pair in range(2):
                    i = g * 2 + pair
                    for (c0, w) in tiles:
                        ba, bb = nb(), nb()
                        for ab, bk in ((0, ba), (1, bb)):
                            for dc in range(8):
                                kb.mm(kb.P(bk, 0, w), slot[:, dc, ab * 256 + pair * 128: ab * 256 + pair * 128 + 128],
                                      H[:, dc, c0:c0 + w], start=(dc == 0), stop=(dc == 7))
                        sa = SA[cnt_a[0] % 2]
                        cnt_a[0] += 1
                        kb.act(sa[:, 0:w], kb.P(ba, 0, w), AF.Silu)
                        kb.tt(G[:, i, loc[c0]:loc[c0] + w], sa[:, 0:w], kb.P(bb, 0, w), ALU.mult)
            def load_dn(g):
                slot = WB[g % 2]
                kb.dma("pool", slot[:, :, :], View(d_dn[idx][g], None, 0, 0))
            load_dn(0)
            for g in range(4):
                if g + 1 < 4:
                    load_dn(g + 1)
                slot = WB[g % 2]
                for dch in range(2):
                    dk = g * 2 + dch
                    for (c0, w) in tiles:
                        b = nb()
                        for fc in range(NFC):
                            kb.mm(kb.P(b, 0, w), slot[:, fc, dch * 128:(dch + 1) * 128], G[:, fc, loc[c0]:loc[c0] + w],
                                  start=(fc == 0), stop=(fc == NFC - 1))
                        kb.stt(X[:, dk, c0:c0 + w], kb.P(b, 0, w), 0.5, X[:, dk, c0:c0 + w], ALU.mult, ALU.add)

    ffn(0, NV_FFN1)
    if debug == "ffn1":
        for k in range(8):
            kb.dma("sp", kb.dview("yT", o_y[:, k, :]), X[:, k, :])
        kb.finish()
        return nc


    A3 = Alloc(kb, SCR)
    CN = A3("CN", [4, TT], BF16)
    MSCR = A3.cur

    rmsnorm(Alloc(kb, MSCR), X, lambda k, c0, w: H[:, k, c0:c0 + w], NV_MIX)

    A4 = Alloc(kb, MSCR)
    C = A4("C", [4, TT], F32)
    U = A4("U", [30 + SEQ], BF16)
    UT = A4("UT", [30], F32)
    DG = A4("DG", [CONV_W, 128], BF16)
    SIG = A4("SIG", [512], F32)
    UB = A4("UB", [4, NS, 31], F32)
    TMPS = A4("TMPS", [NS, 31], F32)
    AW = Alloc(kb, WB[0].off)
    SQ2 = AW("SQ2", [8, 512], BF16)
    ST1 = AW("ST1", [512], F32)
    ST2 = AW("ST2", [512], F32)
    ST3 = AW("ST3", [512], F32)
    OST = [AW("OST%d" % i, [512], F32) for i in range(4)]
    assert AW.cur <= WB[1].off + WB[1].nbytes
    ost_rr = [0]

    def ost():
        t = OST[ost_rr[0] % 4]
        ost_rr[0] += 1
        return t

    def load_win(g, slot):
        kb.dma("pool", slot[:, :, :], View(d_win[g], None, 0, 0))

    load_win(0, WA[0])
    load_win(1, WA[1])
    kb.memset(U[:, 0:30], 0.0)
    d_sconv = din("sconvT", [128, 4, NS, 30])
    o_sconv = dout("sconvT_out", [128, 4, NS, 30])
    kb.dma("sp", UB[:, :, :, 0:30], View(d_sconv, None, 0, 0))
    for c in range(4):
        for (c0, w) in COLT:
            ba, bb = nb(), nb()
            for slot, bk in ((WA[0], ba), (WA[1], bb)):
                for dc in range(8):
                    kb.mm(kb.P(bk, 0, w), slot[:, dc, c * 128:(c + 1) * 128], H[:, dc, c0:c0 + w], start=(dc == 0), stop=(dc == 7))
            kb.act(SIG[:, 0:w], kb.P(bb, 0, w), AF.Exp, scale=-1.0)
            kb.ts(SIG[:, 0:w], SIG[:, 0:w], 1.0, ALU.add)
            kb.recip(SIG[:, 0:w], SIG[:, 0:w])
            if c0 < SEQ:
                kb.tt(U[:, 30 + c0:30 + c0 + w], SIG[:, 0:w], kb.P(ba, 0, w), ALU.mult)
                if c0 + w == SEQ:
                    kb.tt(UT[:, :], SIG[:, w - 30:w], kb.P(ba, w - 30, w), ALU.mult)
            else:
                kb.tt(UB[:, c, :, 30], SIG[:, 0:w], kb.P(ba, 0, w), ALU.mult)
        kb.dma("sp", kb.dview("pconv", o_pconv[:, c, :]), UT[:, :])
        wc = NV_CONVW + c * 31
        for j in range(CONV_W):
            kb.ts(DG[:, j, :], identb, VEC[:, wc + j:wc + j + 1], ALU.mult, eng=("pool" if j % 2 else "dve"))
        for (c0, w) in COLT[:4]:
            bcv = nb()
            for j in range(CONV_W):
                kb.mm(kb.P(bcv, 0, w), DG[:, j, :], U[:, c0 + j:c0 + j + w], start=(j == 0), stop=(j == CONV_W - 1))
            kb.ts(C[:, c, c0:c0 + w], kb.P(bcv, 0, w), VEC[:, NV_CONVB + c:NV_CONVB + c + 1], ALU.add)
        kb.tt(TMPS[:, :, :], UB[:, c, :, :], View(VEC.full[:, wc:wc + 31].unsqueeze(1).to_broadcast([128, NS, 31]), kb.sb, VEC.off, VEC.off + VEC.nbytes), ALU.mult)
        kb.reduce(C[:, c, SEQ:TT], TMPS[:, :, :])
        kb.ts(C[:, c, SEQ:TT], C[:, c, SEQ:TT], VEC[:, NV_CONVB + c:NV_CONVB + c + 1], ALU.add)
    kb.dma("sp", kb.dview("sconv", o_sconv), UB[:, :, :, 1:31])

    for (c0, w) in COLT:
        b1, b2 = nb(), nb()
        for c in range(4):
            kb.act(SQ2[:, c, 0:w], C[:, c, c0:c0 + w], AF.Copy)
            kb.act(SQ2[:, 4 + c, 0:w], C[:, c, c0:c0 + w], AF.Square)
        for c in range(4):
            kb.mm(kb.P(b1, 0, w), ones, SQ2[:, c, 0:w], start=(c == 0), stop=(c == 3))
        for c in range(4):
            kb.mm(kb.P(b2, 0, w), ones, SQ2[:, 4 + c, 0:w], start=(c == 0), stop=(c == 3))
        kb.ts(ST1[:, 0:w], kb.P(b1, 0, w), 1.0 / 512, ALU.mult)
        kb.tt(ST2[:, 0:w], ST1[:, 0:w], ST1[:, 0:w], ALU.mult)
        kb.stt(ST2[:, 0:w], kb.P(b2, 0, w), 1.0 / 512, ST2[:, 0:w], ALU.mult, ALU.subtract)
        kb.act(ST3[:, 0:w], ST2[:, 0:w], AF.Ln, bias=EPS)
        kb.act(ST3[:, 0:w], ST3[:, 0:w], AF.Exp, scale=-0.5)
        for c in range(4):
            kb.tt(C[:, c, c0:c0 + w], C[:, c, c0:c0 + w], ST1[:, 0:w], ALU.subtract)
            kb.tt(C[:, c, c0:c0 + w], C[:, c, c0:c0 + w], ST3[:, 0:w], ALU.mult)
    for c in range(4):
        kb.act(C[:, c, :], C[:, c, :], AF.Silu, scale=VEC[:, NV_LNG + c:NV_LNG + c + 1], bias=VEC[:, NV_LNB + c:NV_LNB + c + 1])
    for (c0, w) in COLT:
        b1 = nb()
        for c in range(4):
            kb.act(SQ2[:, c, 0:w], C[:, c, c0:c0 + w], AF.Square)
        for c in range(4):
            kb.mm(kb.P(b1, 0, w), ones, SQ2[:, c, 0:w], start=(c == 0), stop=(c == 3))
        kb.act(ST3[:, 0:w], kb.P(b1, 0, w), AF.Ln, scale=1.0 / 512, bias=EPS)
        kb.act(ST3[:, 0:w], ST3[:, 0:w], AF.Exp, scale=-0.5)
        for c in range(4):
            kb.stt(CN[:, c, c0:c0 + w], C[:, c, c0:c0 + w], VEC[:, NV_ONC + c:NV_ONC + c + 1], ST3[:, 0:w], ALU.mult, ALU.mult)

    if debug == "conv":
        o_dbg = dout("dbg", [128, 4, TT])
        for c in range(4):
            kb.copy(C[:, c, :], CN[:, c, :])
            kb.dma("sp", kb.dview("dbg", o_dbg[:, c, :]), C[:, c, :])
        kb.finish()
        return nc


    A5 = Alloc(kb, MSCR)
    QT = A5("QT", [4, TT], BF16)
    KST = A5("KST", [TT], BF16)
    KWT = A5("KWT", [TT], BF16)
    KCT = A5("KCT", [TT], BF16)
    VCT = A5("VCT", [TT], BF16)
    VAS = A5("VAS", [17, 2, 65], BF16)
    VAW = A5("VAW", [17, 2, 65], BF16)
    GT = A5("GT", [17, 24], F32)
    KCMPT = A5("KCMPT", [128], BF16)
    RC = A5("RC", [2, 97], BF16)
    M2END = A5.cur
    AW = Alloc(kb, WB[0].off)
    SQb = AW("SQb", [512], BF16)
    QNB = AW("QNB", [512], BF16)
    VFB = AW("VFB", [512], BF16)
    ST1 = AW("ST1", [512], F32)
    ST2 = AW("ST2", [512], F32)
    ST3 = AW("ST3", [512], F32)
    OST = [AW("OST%d" % i, [512], F32) for i in range(2)]
    ROPE = [AW("ROPE%d" % i, [2, 512], F32) for i in range(2)]
    assert AW.cur <= WB[1].off + WB[1].nbytes
    ost_rr = [0]

    def ost2():
        t = OST[ost_rr[0] % 2]
        ost_rr[0] += 1
        return t

    def norm_rope(ps, w, gcol, cosv, sinv, out_bf, out_f32):
        kb.act(SQb[:, 0:w], ps, AF.Square)
        b2 = nb()
        kb.mm(kb.P(b2, 0, w), blk, SQb[:, 0:w], start=True, stop=True)
        kb.act(ST3[:, 0:w], kb.P(b2, 0, w), AF.Ln, scale=1.0 / HD, bias=EPS)
        kb.act(ST3[:, 0:w], ST3[:, 0:w], AF.Exp, scale=-0.5)
        kb.stt(ST1[:, 0:w], ps, VEC[:, gcol:gcol + 1], ST3[:, 0:w], ALU.mult, ALU.mult)
        kb.act(QNB[:, 0:w], ST1[:, 0:w], AF.Copy)
        b3 = nb()
        kb.mm(kb.P(b3, 0, w), pmat, QNB[:, 0:w], start=True, stop=True)
        kb.tt(ST2[:, 0:w], kb.P(b3, 0, w), sinv, ALU.mult)
        kb.tt(ST1[:, 0:w], ST1[:, 0:w], cosv, ALU.mult)
        if out_f32 is not None:
            kb.tt(out_f32, ST1[:, 0:w], ST2[:, 0:w], ALU.add)
            kb.act(out_bf, out_f32, AF.Copy)
        else:
            kb.tt(out_bf, ST1[:, 0:w], ST2[:, 0:w], ALU.add)

    kb.memset(VAS[:, :, :, 64:65], 1.0)
    kb.memset(VAW[:, :, :, 64:65], 1.0)
    rope_rr = [0]
    KV_SLOT = {"kc": 0, "vc": 1, "ks": 2, "vs": 3, "kw": 4, "vw": 5}
    plan = [
        (2, [(0, "q", 0), (1, "q", 1), (2, "q", 2), (3, "q", 3)]),
        (3, [(0, "kc", None), (1, "vc", None), (2, "ks", None), (3, "vs", None)]),
        (4, [(0, "kw", None), (1, "vw", None), (2, "gate", None)]),
    ]
    items = []
    for gi, (grp, chunks) in enumerate(plan):
        for ti, (c0, w) in enumerate(COLT):
            for k_, (ci, kind, qi) in enumerate(chunks):
                items.append((gi, grp, ti, c0, w, ci, kind, qi, k_ == 0))
    loaded = set()
    state = {"rp": None}

    def m2_proj(it):
        gi, grp, ti, c0, w, ci, kind, qi, first_in_tile = it
        slot = WA[gi % 2]
        if gi not in loaded:
            loaded.add(gi)
            load_win(grp, slot)
        b = nb()
        mrows = 24 if kind == "gate" else 128
        for dc in range(8):
            kb.mm(kb.P(b, 0, w, 0, mrows), slot[:, dc, ci * 128:ci * 128 + mrows], H[:, dc, c0:c0 + w], start=(dc == 0), stop=(dc == 7))
        return b

    def m2_post(it, b):
        gi, grp, ti, c0, w, ci, kind, qi, first_in_tile = it
        if first_in_tile and grp in (2, 3, 4):
            rp_ = ROPE[rope_rr[0] % 2]
            rope_rr[0] += 1
            kb.dma("sp", rp_[:, :, 0:w], View(d_rope[:, :, c0:c0 + w], None, 0, 0))
            state["rp"] = rp_
        rp = state["rp"]
        ps = kb.P(b, 0, w)
        if kind == "q":
            norm_rope(ps, w, NV_QN + 0, rp[:, 0, 0:w], rp[:, 1, 0:w], QT[:, qi, c0:c0 + w], None)
        elif kind in ("ks", "kw"):
            o32 = ost2()
            dst = KST if kind == "ks" else KWT
            norm_rope(ps, w, NV_QN + (2 if kind == "ks" else 3), rp[:, 0, 0:w], rp[:, 1, 0:w], dst[:, c0:c0 + w], o32[:, 0:w])
            kb.dma("sp", kb.dview("kvT", o_kv[:, KV_SLOT[kind], c0:c0 + w]), o32[:, 0:w])
        elif kind in ("kc", "vc", "vs", "vw"):
            o32 = ost2()
            kb.copy(o32[:, 0:w], ps)
            kb.dma("sp", kb.dview("kvT", o_kv[:, KV_SLOT[kind], c0:c0 + w]), o32[:, 0:w])
            if kind == "kc":
                kb.act(KCT[:, c0:c0 + w], ps, AF.Copy)
            elif kind == "vc":
                kb.act(VCT[:, c0:c0 + w], ps, AF.Copy)
            else:
                VA = VAS if kind == "vs" else VAW
                kb.act(VFB[:, 0:w], ps, AF.Copy)
                nsub = (w + 127) // 128
                for sb_ in range(nsub):
                    ww = min(128, w - sb_ * 128)
                    bt = nb()
                    kb.transpose(kb.Pbf(bt, 0, 128, 0, ww), VFB[:, sb_ * 128:sb_ * 128 + ww], identb)
                    tix = c0 // 128 + sb_
                    kb.copy(VA[0:ww, tix, :, 0:64], View(kb.psum[0:ww, bt, :].bitcast(BF16)[:, 0:128].rearrange("p (g d) -> p g d", g=2), kb.ps, bt * 2048, bt * 2048 + 2048))
        else:
            kb.act(ST1[0:24, 0:w], kb.P(b, 0, w, 0, 24), AF.Exp, scale=-1.0)
            kb.ts(ST1[0:24, 0:w], ST1[0:24, 0:w], 1.0, ALU.add)
            kb.recip(ST1[0:24, 0:w], ST1[0:24, 0:w])
            nsub = (w + 127) // 128
            for sb_ in range(nsub):
                ww = min(128, w - sb_ * 128)
                bt = nb()
                kb.transpose(kb.P(bt, 0, 24, 0, ww), ST1[0:24, sb_ * 128:sb_ * 128 + ww], View(IDF.full[0:24, 0:24], kb.sb, IDF.off, IDF.off + IDF.nbytes))
                kb.copy(GT[0:ww, c0 // 128 + sb_, :], kb.P(bt, 0, 24, 0, ww))

    pend = None
    for it in items:
        b = m2_proj(it)
        reserved.add(b)
        if pend is not None:
            m2_post(*pend)
            reserved.discard(pend[1])
        pend = (it, b)
    m2_post(*pend)
    reserved.discard(pend[1])

    if debug == "proj":
        o_dbg = dout("dbg", [128, 7, TT])
        for i, t in enumerate([QT[:, 0, :], QT[:, 1, :], QT[:, 2, :], QT[:, 3, :], KST[:, :], KWT[:, :], KCT[:, :]]):
            for (c0, w) in COLT:
                o32 = ost2()
                kb.copy(o32[:, 0:w], View(t.ap[:, c0:c0 + w], t.space, t.lo, t.hi))
                kb.dma("sp", kb.dview("dbg", o_dbg[:, i, c0:c0 + w]), o32[:, 0:w])
        o_dbg2 = dout("dbg_va", [128, 17, 2, 65])
        o_dbg3 = dout("dbg_gt", [128, 17, 24])
        VAf = A5("VAf", [17, 2, 65], F32)
        kb.copy(VAf[:, :, :, :], VAS[:, :, :, :])
        kb.dma("sp", kb.dview("dbg2", o_dbg2), VAf[:, :, :, :])
        kb.dma("sp", kb.dview("dbg3", o_dbg3), GT[:, :, :])
        kb.finish()
        return nc


    W1A = [Tile(kb, "W1Ak", [32, 128], BF16, WA[0].off), Tile(kb, "W1Av", [32, 128], BF16, WA[1].off)]
    A6 = Alloc(kb, M2END)
    PEA = [A6("PEAk", [32], BF16), A6("PEAv", [32], BF16)]
    W2 = [A6("W2k", [64], BF16), A6("W2v", [64], BF16)]
    BIAS = [A6("BIASk", [1], F32), A6("BIASv", [1], F32)]
    NBIAS = [A6("NBIASk", [1], F32), A6("NBIASv", [1], F32)]
    EH = A6("EH", [128], F32)
    HB = A6("HB", [128], BF16)
    for kv in range(2):
        kb.dma("pool", W1A[kv][:, :, :], View(d_w1a[kv], None, 0, 0))
        kb.dma("pool", PEA[kv][:, :], View(d_pea[kv], None, 0, 0))
        kb.dma("pool", W2[kv][:, :], View(d_w2[kv], None, 0, 0))
    kb.dma("pool", RC[:, 0, 64:97], View(d_cb[:, CB_OVP:CB_OVP + 33], None, 0, 0))
    kb.dma("pool", RC[:, 1, 64:97], View(d_cb[:, CB_OVP:CB_OVP + 33], None, 0, 0))
    kb.dma("sp", ROPE[0][:, :, 0:127], View(d_ropec[:, :, 0:127], None, 0, 0))
    import os
    CMPDBG = int(os.environ.get("CMPDBG", "99"))
    for kv in range(2):
        if CMPDBG < 1:
            break
        b = nb()
        for s_ in range(32):
            kb.mm(kb.P(b, 0, 1), W1A[kv][0:64, s_, :], PEA[kv][0:64, s_:s_ + 1], start=(s_ == 0), stop=(s_ == 31))
        kb.copy(BIAS[kv][:, :], kb.P(b, 0, 1))
        kb.ts(NBIAS[kv][:, :], BIAS[kv][:, :], -1.0, ALU.mult)
    bk = nb()
    for kv in range(2):
        if CMPDBG < 2 + kv:
            break
        src = KCT if kv == 0 else VCT
        for g in range(2):
            b = nb()
            for s_ in range(32):
                kb.mm(kb.P(b, 0, 127), W1A[kv][g * 64:(g + 1) * 64, s_, :], src[g * 64:(g + 1) * 64, s_:s_ + 16 * 126 + 1:16],
                      start=(s_ == 0), stop=(s_ == 31))
            kb.act(EH[:, 0:127], kb.P(b, 0, 127), AF.Exp, scale=-1.0, bias=NBIAS[kv][:, 0:1])
            kb.ts(EH[:, 0:127], EH[:, 0:127], 1.0, ALU.add)
            kb.recip(EH[:, 0:127], EH[:, 0:127])
            kb.stt(HB[:, 0:127], kb.P(b, 0, 127), BIAS[kv][:, 0:1], EH[:, 0:127], ALU.add, ALU.mult)
            if kv == 0:
                kb.mm(kb.P(bk, 0, 127, g * 64, g * 64 + 64), W2[0][:, :], HB[:, 0:127], start=True, stop=True)
            else:
                b2 = nb()
                kb.mm(kb.P(b2, 0, 64, 0, 127), HB[:, 0:127], W2[1][:, :], start=True, stop=True)
                kb.copy(RC[0:127, g, 0:64], kb.P(b2, 0, 64, 0, 127))
        if kv == 0 and CMPDBG != 2:
            norm_rope(kb.P(bk, 0, 127), 127, NV_QN + 1, ROPE[0][:, 0, 0:127], ROPE[0][:, 1, 0:127], KCMPT[:, 0:127], None)

    if debug == "cmp":
        o_dbg = dout("dbg", [128, 128])
        o_dbg2 = dout("dbg2", [128, 2, 97])
        kb.memset(ST1[:, 0:128], 0.0)
        kb.copy(ST1[:, 0:127], KCMPT[:, 0:127])
        kb.dma("sp", kb.dview("dbg", o_dbg), ST1[:, 0:128])
        RCf = A6("RCf", [2, 97], F32)
        kb.memset(RCf[:, :, :], 0.0)
        kb.copy(RCf[0:127, :, :], RC[0:127, :, :])
        kb.dma("sp", kb.dview("dbg2", o_dbg2), RCf[:, :, :])
        kb.finish()
        return nc

    AWB = Alloc(kb, WB[0].off)
    CM = AWB("CM", [8, 512], BF16)
    CMPM = AWB("CMPM", [2048], BF16)
    EE = AWB("EE", [2048], BF16)
    ET = [AWB("ET%d" % i, [512], BF16) for i in range(3)]
    MSK = [AWB("MSK%d" % i, [512], BF16) for i in range(2)]
    ET.append(AWB("ET3", [512], BF16))
    assert AWB.cur <= WB[1].off + WB[1].nbytes
    AH = Alloc(kb, H.off)
    OTOK = AH("OTOK", [4, 512], F32)
    SELT = AH("SELT", [512], BF16)
    IMP = AH("IMP", [4, 32], F32)
    TMPO = AH("TMPO", [4, 64], F32)
    TMPI = AH("TMPI", [4, 32], F32)
    SCO = AH("SCO", [32], F32)
    SC2 = AH("SC2", [32], F32)
    M8 = AH("M8", [16], F32)
    SELF = AH("SELF", [32], F32)
    BON = AH("BON", [4, 32], F32)
    RD = AH("RD", [4], F32)
    WG = AH("WG", [4], F32)
    SS = AH("SS", [4], F32)
    SS2 = AH("SS2", [4], F32)
    ONB = AH("ONB", [512], BF16)
    assert AH.cur <= H.off + 4 * TT * 2
    kb.dma("pool", CM[:, :, :], View(d_cb[:, CB_CM:CB_CM + 4096].rearrange("p (a b) -> p a b", a=8), None, 0, 0))
    kb.dma("pool", CMPM[:, :], View(d_cb[:, CB_CMP:CB_CMP + 2048], None, 0, 0))
    kb.dma("pool", EE[:, :], View(d_cb[:, CB_E:CB_E + 2048], None, 0, 0))
    et_rr = [0]
    sc_rr = [0]
    msk_rr = [0]
    OB = [3, 4, 5, 6]

    def strided_ps(bank, start, step, n, width, rows=128):
        full = kb.psum[0:rows, bank, 0:n * step].rearrange("p (n s) -> p n s", s=step)[:, :, start:start + width]
        return View(full, kb.ps, bank * 2048, bank * 2048 + 2048)

    def strided_ps2(bank, start, step, n, rows=128):
        full = kb.psum[0:rows, bank, 0:n * step].rearrange("p (n s) -> p n s", s=step)[:, :, start]
        return View(full, kb.ps, bank * 2048, bank * 2048 + 2048)

    def evac_branch(bank, W, h, bi, qt0, nsub, rows, first, want_imp, imp_first, IMP):
        den = strided_ps2(bank, 64, W, nsub, rows)
        kb.ts(RD[0:rows, 0:nsub], den, 1e-30, ALU.max)
        kb.recip(RD[0:rows, 0:nsub], RD[0:rows, 0:nsub])
        kb.tt(WG[0:rows, 0:nsub], RD[0:rows, 0:nsub], GT[0:rows, qt0:qt0 + nsub, 3 * h + bi], ALU.mult)
        wgb = View(WG.full[0:rows, 0:nsub].unsqueeze(2).to_broadcast([rows, nsub, 64]), kb.sb, WG.off, WG.off + WG.nbytes)
        onum = strided_ps(bank, 0, W, nsub, 64, rows)
        dst = OTOK[0:rows, 0:nsub, h * 64:(h + 1) * 64]
        if first:
            kb.tt(dst, onum, wgb, ALU.mult)
        else:
            kb.tt(TMPO[0:rows, 0:nsub, :], onum, wgb, ALU.mult)
            kb.tt(dst, dst, TMPO[0:rows, 0:nsub, :], ALU.add)
        if want_imp:
            rdb = View(RD.full[0:rows, 0:nsub].unsqueeze(2).to_broadcast([rows, nsub, 32]), kb.sb, RD.off, RD.off + RD.nbytes)
            oimp = strided_ps(bank, 65, W, nsub, 32, rows)
            if imp_first:
                kb.tt(IMP[0:rows, 0:nsub, :], oimp, rdb, ALU.mult)
            else:
                kb.tt(TMPI[0:rows, 0:nsub, :], oimp, rdb, ALU.mult)
                kb.tt(IMP[0:rows, 0:nsub, :], IMP[0:rows, 0:nsub, :], TMPI[0:rows, 0:nsub, :], ALU.add)

    def out_norm_to_H(rows, nsub, col0):
        for sub in range(nsub):
            kb.act(ONB[0:rows, :], OTOK[0:rows, sub, :], AF.Square, accum=SS[0:rows, sub:sub + 1])
        kb.act(SS2[0:rows, 0:nsub], SS[0:rows, 0:nsub], AF.Ln, scale=1.0 / 512, bias=EPS)
        kb.act(SS2[0:rows, 0:nsub], SS2[0:rows, 0:nsub], AF.Exp, scale=-0.5)
        for sub in range(nsub):
            kb.ts(ONB[0:rows, :], OTOK[0:rows, sub, :], SS2[0:rows, sub:sub + 1], ALU.mult)
            for j in range(4):
                kb.transpose(kb.Pbf(7, j * 128, j * 128 + rows), ONB[0:rows, j * 128:(j + 1) * 128], View(identb.ap[0:rows, 0:rows], kb.sb, identb.lo, identb.hi))
            for j in range(4):
                kb.ts(H[:, 4 + j, col0 + sub * 128:col0 + sub * 128 + rows], kb.Pbf(7, j * 128, j * 128 + rows),
                      VEC[:, NV_ONA + j:NV_ONA + j + 1], ALU.mult)

    SELT2 = [SELT, AH("SELT1", [512], BF16)]
    IMP2 = [IMP, AH("IMP1", [4, 32], F32)]
    ET4 = ET
    assert AH.cur <= H.off + 4 * TT * 2

    def attend(Q):
        q0 = Q * 512
        qcols = slice(q0, q0 + 512)
        tasks = []
        if Q >= 2:
            kb.dma("sp", BON[:, :, :], View(d_bonus[:, Q * 4:Q * 4 + 4, :], None, 0, 0))

        def add(**kw):
            t = dict(pre=None, post=None)
            t.update(kw)
            tasks.append(t)

        def topk(g):
            imp, selt = IMP2[g], SELT2[g]
            for sub in range(4):
                kb.tt(SCO[:, :], imp[:, sub, :], BON[:, sub, :], ALU.add)
                kb.op("dve", lambda e: e.max(out=M8[:, 0:8].ap, in_=SCO[:, :].ap), reads=[SCO[:, :]], writes=[M8[:, 0:8]])
                kb.op("dve", lambda e: e.match_replace(out=SC2[:, :].ap, in_to_replace=M8[:, 0:8].ap, in_values=SCO[:, :].ap, imm_value=-3.0e38),
                      reads=[SCO[:, :], M8[:, 0:8]], writes=[SC2[:, :]])
                kb.op("dve", lambda e: e.max(out=M8[:, 8:16].ap, in_=SC2[:, :].ap), reads=[SC2[:, :]], writes=[M8[:, 8:16]])
                kb.ts(SELF[:, :], SCO[:, :], M8[:, 15:16], ALU.is_ge)
                kb.transpose(kb.P(7, sub * 128, (sub + 1) * 128, 0, 32), SELF[:, :], IDF[:, :])
            kb.copy(selt[0:32, :], kb.P(7, 0, 512, 0, 32))

        for g in range(2):
            gp = slice(g * 64, (g + 1) * 64)
            for hh in range(4):
                def qk(a, gp=gp, hh=hh):
                    kb.mm(kb.P(a, 0, 512, 0, 127), KCMPT[gp, 0:127], QT[gp, hh, qcols], start=True, stop=True)
                def ex(a, et):
                    kb.act(et[0:127, :], kb.P(a, 0, 512, 0, 127), AF.Exp, scale=SCALE)
                    kb.tt(et[0:127, :], et[0:127, :], CMPM[0:127, qcols], ALU.mult)
                def pv(et, g=g, hh=hh):
                    for sub in range(4):
                        kb.mm(kb.P(OB[hh], sub * 97, sub * 97 + 97), et[0:127, sub * 128:(sub + 1) * 128], RC[0:127, g, :],
                              start=(sub == 0), stop=True)
                def post(g=g, hh=hh):
                    evac_branch(OB[hh], 97, 4 * g + hh, 0, Q * 4, 4, 128, True, Q >= 2, hh == 0, IMP2[g])
                    if hh == 3 and Q >= 2:
                        topk(g)
                add(qk=qk, ex=ex, pv=pv, post=post)
        for g in range(2):
            gp = slice(g * 64, (g + 1) * 64)
            for bi, KT, VA in ((1, KST, VAS), (2, KWT, VAW)):
                kt_lo = 0 if bi == 1 else max(0, 4 * Q - 4)
                kts = list(range(kt_lo, 4 * Q + 4))
                started = [False] * 4
                for Kt in kts:
                    r = Kt - 4 * Q
                    pre = None
                    mask_box = [None]
                    if bi == 1:
                        if Q >= 2:
                            mk = MSK[msk_rr[0] % 2]
                            msk_rr[0] += 1
                            def pre(Kt=Kt, r=r, mk=mk, g=g):
                                kb.mm(kb.P(7), EE[0:32, Kt * 128:(Kt + 1) * 128], SELT2[g][0:32, :], start=True, stop=True)
                                if r >= 0:
                                    kb.tt(mk[:, :], kb.P(7), CM[:, r, :], ALU.mult)
                                else:
                                    kb.copy(mk[:, :], kb.P(7))
                            mask_box[0] = mk[:, :]
                        elif r >= 0:
                            mask_box[0] = CM[:, r, :]
                        subs = [s_ for s_ in range(4) if r <= s_]
                    else:
                        mask_box[0] = CM[:, r, :] if r >= 0 else CM[:, 3 - r, :]
                        subs = [s_ for s_ in range(4) if (r <= s_ and s_ - r < 5)]
                    for hh in range(4):
                        def qk(a, gp=gp, hh=hh, Kt=Kt, KT=KT):
                            kb.mm(kb.P(a), KT[gp, Kt * 128:(Kt + 1) * 128], QT[gp, hh, qcols], start=True, stop=True)
                        def ex(a, et, mask=mask_box[0]):
                            kb.act(et[:, :], kb.P(a), AF.Exp, scale=SCALE)
                            if mask is not None:
                                kb.tt(et[:, :], et[:, :], mask, ALU.mult)
                        first = not started[hh]
                        started[hh] = True
                        def pv(et, hh=hh, Kt=Kt, g=g, VA=VA, subs=subs, first=first):
                            f = first
                            for sub in subs:
                                kb.mm(kb.P(OB[hh], sub * 65, sub * 65 + 65), et[:, sub * 128:(sub + 1) * 128], VA[:, Kt, g, :],
                                      start=f, stop=True)
                                f = False
                        post = None
                        if Kt == kts[-1]:
                            def post(g=g, hh=hh, bi=bi):
                                evac_branch(OB[hh], 65, 4 * g + hh, bi, Q * 4, 4, 128, False, False, False, None)
                        add(qk=qk, ex=ex, pv=pv, post=post, pre=(pre if hh == 0 else None))
        n = len(tasks)
        for i in range(n + 2):
            if i < n:
                t = tasks[i]
                if t["pre"] is not None:
                    t["pre"]()
                t["qk"](i % 3)
            if 1 <= i <= n:
                tasks[i - 1]["ex"]((i - 1) % 3, ET4[(i - 1) % 4])
            if i >= 2:
                t = tasks[i - 2]
                t["pv"](ET4[(i - 2) % 4])
                if t["post"] is not None:
                    t["post"]()
        out_norm_to_H(128, 4, q0)

    for Q in range(4):
        attend(Q)
        if debug == "att0":
            break

    if debug in ("att0", "att"):
        o_dbg = dout("dbg", [128, 4, TT])
        for j in range(4):
            for (c0, w) in COLT[:4]:
                o32 = ost2() if False else None
            kb.copy(X[:, j, 0:SEQ], H[:, 4 + j, 0:SEQ])
            kb.dma("sp", kb.dview("dbg", o_dbg[:, j, 0:SEQ]), X[:, j, 0:SEQ])
        kb.finish()
        return nc


    d_pools = [din("pool_kc", [N_POOL * 4, 4096]), din("pool_vc", [N_POOL * 4, 4096]),
               din("pool_ks", [N_POOL * 4, 4096]), din("pool_vs", [N_POOL * 4, 4096])]
    d_wins = [din("win_k", [NS, 512, 128]), din("win_v", [NS, 512, 128])]
    d_ptT = din("ptT", [128, NS], I32)
    d_ptB = din("ptB", [128, 128], I32)
    d_sc = din("sconst", [128, SC_TOT])
    d_ovs = din("ovs", [128, 8, 257])
    o_swin = [dout("swin_k", [NS, 512, 128]), dout("swin_v", [NS, 512, 128])]
    sc_idx = nc.dram_tensor("sc_idx", [128], U32, kind="Internal").ap()
    sc_oc = nc.dram_tensor("sc_oc", [NS, 2, 4, 64], F32, kind="Internal").ap()

    def sb_view(tile, ap):
        return View(ap, kb.sb, tile.off, tile.off + tile.nbytes)

    def _sample_attention():
        AS = Alloc(kb, M2END + 1536)
        SCN = AS("SCN", [SC_TOT], F32)
        kb.dma("sp", SCN[:, :], View(d_sc, None, 0, 0))
        QTOK = AS("QTOK", [4, 2, 64], F32)
        KNS = AS("KNS", [2, 64], F32)
        KNW = AS("KNW", [2, 64], F32)
        PTI = AS("PTI", [NS], I32)
        PTF = AS("PTF", [NS], F32)
        IDXF = AS("IDXF", [NS, 4], F32)
        IDX = AS("IDX", [NS, 4], I32)
        QSF = AS("QSF", [4, NS], BF16)
        VNS = AS("VNS", [2, 64], BF16)
        VNW = AS("VNW", [2, 64], BF16)
        assert AS.cur <= ARENA, AS.cur
        kb.copy(QSF[:, :, :], QT[:, :, SEQ:TT])
        kb.copy(VNS[0:NS, :, :], VAS[0:NS, 16, :, 0:64])
        kb.copy(VNW[0:NS, :, :], VAW[0:NS, 16, :, 0:64])
        AL = Alloc(kb, MSCR)
        QB = AL("QB", [4, 64], F32)
        ETS = AL("ETS", [32], BF16)
        RDS = AL("RDS", [1], F32)
        LSEL = AL("LSEL", [8], F32)
        IMPR = AL("IMPR", [257], F32)
        OCS = AL("OCS", [64], F32)
        SCOS = AL("SCOS", [256], F32)
        SC2S = AL("SC2S", [256], F32)
        M8S = AL("M8S", [16], F32)
        IXS = AL("IXS", [16], U32)
        JI = AL("JI", [1], U32)
        JF = AL("JF", [6], F32)
        IDX2F = AL("IDX2F", [2], F32)
        IDX2 = AL("IDX2", [2], I32)
        PTBI = AL("PTBI", [128], I32)
        PTBF = AL("PTBF", [128], F32)
        OH = AL("OH", [256], F32)
        TAB = AL("TAB", [256], F32)
        SCD = AL("SCD", [4, 32], F32)
        ED = AL("ED", [4, 32], F32)
        PARTS = [AL("PART%d" % i, [4, 65], F32) for i in range(2)]
        NUM = AL("NUM", [8, 65], F32)
        SNE = AL("SNE", [8], F32)
        WG8 = AL("WG8", [8], F32)
        assert AL.cur <= MSCR + 16384, AL.cur
        sconst = lambda a, n, p0=0, p1=128: sb_view(SCN, SCN.full[p0:p1, a:a + n])

        def to_tok(src_view, dst_view):
            bt = nb()
            kb.transpose(kb.Pbf(bt, 0, 128, 0, NS), src_view, identb)
            kb.copy(dst_view, View(kb.psum[0:NS, bt, :].bitcast(BF16)[:, 0:128].rearrange("p (g d) -> p g d", g=2), kb.ps, bt * 2048, bt * 2048 + 2048))
        for c in range(4):
            to_tok(QT[:, c, SEQ:TT], QTOK[0:NS, c, :, :])
        to_tok(KST[:, SEQ:TT], KNS[0:NS, :, :])
        to_tok(KWT[:, SEQ:TT], KNW[0:NS, :, :])

        for kv, slot in ((0, 4), (1, 5)):
            kb.dma("sp", kb.dview("swin%d" % kv, o_swin[kv][:, 0:511, :]), View(d_wins[kv][:, 1:512, :], None, 0, 0))
            kb.dma("sp", kb.dview("swin%d" % kv, o_swin[kv][:, 511, :].rearrange("b p -> p b")), kb.dview("kvT", o_kv[:, slot, SEQ:TT]),
                   fn=lambda e, kv=kv, slot=slot: e.dma_start(out=o_swin[kv][:, 511, :].rearrange("b p -> p b"), in_=o_kv[:, slot, SEQ:TT],
                                                             allow_slow_non_contiguous=True))

        AG = Alloc(kb, WA[0].off)
        GA = [AG("GA0", [4096], F32), AG("GA1", [4096], F32)]
        HBS = AG("HBS", [128], BF16)
        EHS = AG("EHS", [128], F32)
        SQb_ = AG("SQbs", [128], BF16)
        QNB_ = AG("QNBs", [128], BF16)
        S1_ = AG("S1s", [128], F32)
        S2_ = AG("S2s", [128], F32)
        S3_ = AG("S3s", [128], F32)
        AX_ = Alloc(kb, H.off)
        XT = [AX_("XT%d" % i, [16, 128], BF16) for i in range(4)]
        assert AX_.cur <= H.off + 4 * TT * 2
        AC = Alloc(kb, MSCR)
        W1S = AC("W1S", [32, 128], BF16)
        ROPC = AC("ROPC", [2, 1024], F32)
        OVS = AC("OVS", [8, 257], BF16)
        KCS = [AC("KCS%d" % b_, [1024], BF16) for b_ in range(NS)]
        VCS = [[AC("VCS%d%d" % (b_, g), [8, 65], BF16) for g in range(2)] for b_ in range(NS)]
        assert AC.cur <= M2END, (AC.cur, M2END)
        assert OVS.off >= MSCR + 16384
        AL2 = Alloc(kb, OVS.off)
        OCT = AL2("OCT", [8, 64], F32)
        TMPQ = AL2("TMPQ", [8, 64], F32)
        assert AL2.cur <= OVS.off + OVS.nbytes
        kb.dma("sp", ROPC[:, :, :], View(d_ropec, None, 0, 0))
        kb.dma("pool", OVS[:, :, :], View(d_ovs, None, 0, 0))
        kb.dma("sp", PTI[:, :], View(d_ptT, None, 0, 0))
        kb.copy(PTF[:, :], PTI[:, :])
        for q4 in range(4):
            kb.ts(IDXF[:, :, q4], PTF[:, :], 4.0, ALU.mult, float(q4), ALU.add)
        kb.copy(IDX[:, :, :], IDXF[:, :, :])
        for b_ in range(NS):
            kb.memset(KCS[b_][:, 1016:1024], 0.0)
            for g in range(2):
                kb.memset(VCS[b_][g][:, :, :], 0.0)
                kb.memset(VCS[b_][g][:, :, 64:65], 1.0)

        HB2 = [[AG("HBS%d%d" % (i, g), [128], BF16) for g in range(2)] for i in range(2)]
        S1B = [S1_, AG("S1b", [128], F32)]
        QNB2 = [QNB_, AG("QNBb", [128], BF16)]
        assert AG.cur <= WB[1].off + WB[1].nbytes, AG.cur

        ga_rr = [0]
        xt_slot = lambda ci: 0 if ci == 0 else 1 + (ci - 1) % 3
        for kv in range(2):
            kb.dma("pool", W1S[:, :, :], View(d_w1a[kv], None, 0, 0))
            for b_ in range(NS):
                wof = lambda ci: 128 if ci < 7 else 127

                def part1(ci):
                    w = wof(ci)
                    for g in range(2):
                        bh = nb()
                        gp = slice(g * 64, (g + 1) * 64)
                        for sp_ in range(32):
                            s16 = sp_ % 16
                            if sp_ < 16:
                                rhs = XT[xt_slot(ci)][gp, s16, 0:w]
                            elif ci < 7:
                                rhs = XT[xt_slot(ci + 1)][gp, s16, 0:w]
                            else:
                                rhs = XT[0][gp, s16, 1:128]
                            kb.mm(kb.P(bh, 0, w), W1S[gp, sp_, :], rhs, start=(sp_ == 0), stop=(sp_ == 31))
                        hb = HB2[ci % 2][g]
                        kb.act(EHS[:, 0:w], kb.P(bh, 0, w), AF.Exp, scale=-1.0, bias=NBIAS[kv][:, 0:1])
                        kb.ts(EHS[:, 0:w], EHS[:, 0:w], 1.0, ALU.add)
                        kb.recip(EHS[:, 0:w], EHS[:, 0:w])
                        kb.stt(hb[:, 0:w], kb.P(bh, 0, w), BIAS[kv][:, 0:1], EHS[:, 0:w], ALU.add, ALU.mult)

                def part2(ci):
                    w = wof(ci)
                    if kv == 0:
                        bkk = nb()
                        for g in range(2):
                            kb.mm(kb.P(bkk, 0, w, g * 64, g * 64 + 64), W2[0][:, :], HB2[ci % 2][g][:, 0:w], start=True, stop=True)
                        ps = kb.P(bkk, 0, w)
                        kb.act(SQb_[:, 0:w], ps, AF.Square)
                        b2 = nb()
                        kb.mm(kb.P(b2, 0, w), blk, SQb_[:, 0:w], start=True, stop=True)
                        kb.act(S3_[:, 0:w], kb.P(b2, 0, w), AF.Ln, scale=1.0 / HD, bias=EPS)
                        kb.act(S3_[:, 0:w], S3_[:, 0:w], AF.Exp, scale=-0.5)
                        s1 = S1B[ci % 2]
                        kb.stt(s1[:, 0:w], ps, VEC[:, NV_QN + 1:NV_QN + 2], S3_[:, 0:w], ALU.mult, ALU.mult)
                        kb.act(QNB2[ci % 2][:, 0:w], s1[:, 0:w], AF.Copy)
                    else:
                        for g in range(2):
                            b2 = nb()
                            kb.mm(kb.P(b2, 0, 64, 0, w), HB2[ci % 2][g][:, 0:w], W2[1][:, :], start=True, stop=True)
                            kb.copy(VCS[b_][g][0:w, ci, 0:64], kb.P(b2, 0, 64, 0, w))

                def part3(ci):
                    if kv != 0:
                        return
                    w = wof(ci)
                    hi_ = ci + 8 * (w - 1) + 1
                    s1 = S1B[ci % 2]
                    b3 = nb()
                    kb.mm(kb.P(b3, 0, w), pmat, QNB2[ci % 2][:, 0:w], start=True, stop=True)
                    kb.tt(S2_[:, 0:w], kb.P(b3, 0, w), ROPC[:, 1, ci:hi_:8], ALU.mult)
                    kb.tt(s1[:, 0:w], s1[:, 0:w], ROPC[:, 0, ci:hi_:8], ALU.mult)
                    kb.tt(KCS[b_][:, ci:hi_:8], s1[:, 0:w], S2_[:, 0:w], ALU.add)

                def step(k):
                    if 0 <= k - 4 <= 7:
                        part3(k - 4)
                    if 0 <= k - 3 <= 7:
                        part2(k - 3)
                    if 0 <= k - 2 <= 7:
                        part1(k - 2)

                for q4 in range(4):
                    ga = GA[ga_rr[0] % 2]
                    ga_rr[0] += 1
                    idxv = IDX[:, b_, q4:q4 + 1]
                    kb.dma("pool", ga[:, :], idxv, fn=lambda e, ga=ga, idxv=idxv, kv=kv: e.indirect_dma_start(
                        out=ga[:, :].ap, out_offset=None, in_=d_pools[kv], in_offset=bass.IndirectOffsetOnAxis(ap=idxv.ap, axis=0)))
                    for cl in range(2):
                        ci = 2 * q4 + cl
                        xt = XT[xt_slot(ci)]
                        for s4 in range(4):
                            bt = nb()
                            for t4 in range(4):
                                t0 = cl * 16 + s4 * 4 + t4
                                kb.transpose(kb.P(bt, t4 * 128, (t4 + 1) * 128), ga[:, t0 * 128:(t0 + 1) * 128], IDF[:, :])
                            kb.copy(sb_view(xt, xt.full[:, s4 * 4:s4 * 4 + 4, :]),
                                    View(kb.psum[:, bt, :].rearrange("p (s n) -> p s n", s=4), kb.ps, bt * 2048, bt * 2048 + 2048),
                                    eng=("act" if s4 % 2 else "dve"))
                        step(ci + 1)
                for k in range(9, 13):
                    step(k)

        reserved.add(7)
        for b_ in range(NS):
            for g in range(2):
                gp = slice(g * 64, (g + 1) * 64)
                bgi = g * NS + b_
                a = nb()
                for ci in range(8):
                    kb.mm(kb.P(a, ci * 4, ci * 4 + 4), KCS[b_][gp, ci:1024:8], QSF[gp, :, b_], start=(ci == 0), stop=True)
                kb.act(ETS[:, :], kb.P(a, 0, 32), AF.Exp, scale=SCALE)
                kb.tt(ETS[:, :], ETS[:, :], sconst(SC_MK, 32), ALU.mult)
                o1, o2 = nb(), nb()
                for ci in range(8):
                    kb.mm(kb.P(o1, 0, 65, 0, 4), ETS[:, ci * 4:ci * 4 + 4], VCS[b_][g][:, ci, :], start=(ci == 0), stop=(ci == 7))
                for ci in range(8):
                    kb.mm(kb.P(o2, 0, 257, 0, 4), ETS[:, ci * 4:ci * 4 + 4], OVS[:, ci, :], start=(ci == 0), stop=(ci == 7))
                kb.ts(RDS[0:4, :], kb.P(o1, 64, 65, 0, 4), 1e-30, ALU.max)
                kb.recip(RDS[0:4, :], RDS[0:4, :])
                kb.ts(OCS[0:4, :], kb.P(o1, 0, 64, 0, 4), RDS[0:4, 0:1], ALU.mult)
                kb.dma("sp", kb.dview("sc_oc", sc_oc[b_, g]), OCS[0:4, :])
                kb.copy(IMPR[0:4, :], kb.P(o2, 0, 257, 0, 4))
                kb.ts(LSEL[0:4, :], sconst(SC_SELC + bgi * 8, 8, 0, 4), RDS[0:4, 0:1], ALU.mult)
                kb.mm(kb.P(7, 0, 257, 0, 8), LSEL[0:4, :], IMPR[0:4, :], start=(b_ == 0 and g == 0), stop=(b_ == NS - 1 and g == 1))
        kb.tt(SCOS[0:8, :], kb.P(7, 0, 256, 0, 8), sconst(SC_BONS, 256, 0, 8), ALU.add)
        reserved.discard(7)
        kb.op("dve", lambda e: e.max(out=M8S[0:8, 0:8].ap, in_=SCOS[0:8, :].ap), reads=[SCOS[0:8, :]], writes=[M8S[0:8, 0:8]])
        kb.op("dve", lambda e: e.max_index(out=IXS[0:8, 0:8].ap, in_max=M8S[0:8, 0:8].ap, in_values=SCOS[0:8, :].ap),
              reads=[SCOS[0:8, :], M8S[0:8, 0:8]], writes=[IXS[0:8, 0:8]])
        kb.op("dve", lambda e: e.match_replace(out=SC2S[0:8, :].ap, in_to_replace=M8S[0:8, 0:8].ap, in_values=SCOS[0:8, :].ap, imm_value=-3.0e38),
              reads=[SCOS[0:8, :], M8S[0:8, 0:8]], writes=[SC2S[0:8, :]])
        kb.op("dve", lambda e: e.max(out=M8S[0:8, 8:16].ap, in_=SC2S[0:8, :].ap), reads=[SC2S[0:8, :]], writes=[M8S[0:8, 8:16]])
        kb.op("dve", lambda e: e.max_index(out=IXS[0:8, 8:16].ap, in_max=M8S[0:8, 8:16].ap, in_values=SC2S[0:8, :].ap),
              reads=[SC2S[0:8, :], M8S[0:8, 8:16]], writes=[IXS[0:8, 8:16]])
        kb.dma("sp", kb.dview("sc_idx", sc_idx.rearrange("(a r) -> a r", r=16)), IXS[0:8, :])
        kb.dma("sp", JI[:, :], kb.dview("sc_idx", sc_idx.rearrange("(p o) -> p o", o=1)))
        kb.dma("sp", PTBI[:, :], View(d_ptB, None, 0, 0))
        kb.copy(PTBF[:, :], PTBI[:, :])
        kb.copy(JF[:, 0:1], JI[:, :])
        ptb3 = sb_view(PTBF, PTBF.full[:, :].unsqueeze(2).to_broadcast([128, 128, 2]))
        tab3 = sb_view(TAB, TAB.full[:, :].rearrange("p (a h) -> p a h", h=2))
        kb.ts(tab3, ptb3, 4.0, ALU.mult)
        h2b = sb_view(SCN, SCN.full[:, SC_H2:SC_H2 + 2].unsqueeze(1).to_broadcast([128, 128, 2]))
        kb.tt(tab3, tab3, h2b, ALU.add)
        kb.ts(OH[:, :], sconst(SC_IOTA, 256), JF[:, 0:1], ALU.is_equal)
        kb.tt(OH[:, :], OH[:, :], TAB[:, :], ALU.mult)
        kb.reduce(JF[:, 5:6], OH[:, :])
        for pc in range(2):
            kb.ts(IDX2F[:, pc:pc + 1], JF[:, 5:6], float(pc), ALU.add)
        kb.copy(IDX2[:, :], IDX2F[:, :])

        if debug == "samp":
            o_d1 = dout("d_kcs", [128, 1024])
            o_d2 = dout("d_ixs", [8, 16], U32)
            o_d3 = dout("d_imp", [8, 256])
            o_d4 = dout("d_idx2", [128, 2], I32)
            kb.copy(GA[0][:, 0:1024], KCS[0][:, :])
            kb.dma("sp", kb.dview("d1", o_d1), GA[0][:, 0:1024])
            kb.dma("sp", kb.dview("d2", o_d2), IXS[0:8, :])
            kb.dma("sp", kb.dview("d3", o_d3), SCOS[0:8, :])
            kb.dma("sp", kb.dview("d4", o_d4), IDX2[:, :])
        bq = nb()
        for g in range(2):
            kb.mm(kb.P(bq, 0, 256, g * 64, g * 64 + 64), sconst(SC_INDB + g * 64, 64, 0, NS), QTOK[0:NS, :, g, :], start=True, stop=True)
        kb.copy(QB[:, :, :], View(kb.psum[:, bq, 0:256].rearrange("p (h d) -> p h d", h=4), kb.ps, bq * 2048, bq * 2048 + 2048))
        GW = [Tile(kb, "GWk", [32, 64], F32, GA[0].off), Tile(kb, "GWv", [32, 64], F32, GA[1].off)]
        TMPD = Tile(kb, "TMPD", [32, 64], F32, KCS[0].off)
        assert TMPD.nbytes <= 4 * KCS[0].nbytes

        def dve_attend(kview_fn, vview_fn, halves, part, mask_fn):
            for gp_, g in halves:
                kv_ = kview_fn(gp_, g)
                for hh in range(4):
                    qb = sb_view(QB, QB.full[gp_, hh, :].unsqueeze(1).to_broadcast([gp_.stop - gp_.start, 32, 64]))
                    kb.tt(TMPD[gp_, :, :], kv_, qb, ALU.mult)
                    kb.reduce(SCD[gp_, hh, :], TMPD[gp_, :, :])
            kb.act(ED[:, :, :], SCD[:, :, :], AF.Exp, scale=SCALE)
            mask_fn()
            kb.reduce(sb_view(part, part.full[:, :, 64]), ED[:, :, :])
            for gp_, g in halves:
                vv_ = vview_fn(gp_, g)
                n = gp_.stop - gp_.start
                for hh in range(4):
                    eb = sb_view(ED, ED.full[gp_, hh, :].unsqueeze(1).to_broadcast([n, 64, 32]))
                    kb.tt(sb_view(TMPD, TMPD.full[gp_, :, :].rearrange("p t d -> p d t")), vv_, eb, ALU.mult)
                    kb.reduce(part[gp_, hh, 0:64], sb_view(TMPD, TMPD.full[gp_, :, :].rearrange("p t d -> p d t")))

        def finish_branch(part, knew, va_new, bi):
            for g in range(2):
                bn = nb()
                kb.mm(kb.P(bn, 0, 260, 0, NS), sconst(SC_IND2, NS, g * 64, g * 64 + 64), sb_view(part, part.full[g * 64:g * 64 + 64, :, :]),
                      start=True, stop=True)
                kb.copy(NUM[0:NS, g * 4:g * 4 + 4, :], View(kb.psum[0:NS, bn, 0:260].rearrange("p (h w) -> p h w", h=4), kb.ps, bn * 2048, bn * 2048 + 2048))
            knb = sb_view(knew, knew.full[0:NS, :, :].unsqueeze(1).to_broadcast([NS, 4, 2, 64]))
            kb.tt(sb_view(TMPQ, TMPQ.full[0:NS, :, :].rearrange("p (c g) d -> p c g d", c=4)), QTOK[0:NS, :, :, :], knb, ALU.mult)
            kb.reduce(SNE[0:NS, :], TMPQ[0:NS, :, :])
            kb.act(SNE[0:NS, :], SNE[0:NS, :], AF.Exp, scale=SCALE)
            en = sb_view(SNE, SNE.full[0:NS, :].rearrange("p (c g) -> p g c", c=4))
            num3 = sb_view(NUM, NUM.full[0:NS, :, 64].rearrange("p (g c) -> p g c", g=2))
            kb.tt(num3, num3, en, ALU.add)
            vnb = sb_view(va_new, va_new.full[0:NS, :, :].unsqueeze(2).to_broadcast([NS, 2, 4, 64]))
            enb = sb_view(SNE, SNE.full[0:NS, :].rearrange("p (c g) -> p g c", c=4).unsqueeze(3).to_broadcast([NS, 2, 4, 64]))
            t4 = sb_view(TMPQ, TMPQ.full[0:NS, :, :].rearrange("p (g c) d -> p g c d", g=2))
            kb.tt(t4, vnb, enb, ALU.mult)
            kb.tt(NUM[0:NS, :, 0:64], NUM[0:NS, :, 0:64], TMPQ[0:NS, :, :], ALU.add)
            kb.recip(WG8[0:NS, :], sb_view(NUM, NUM.full[0:NS, :, 64]))
            kb.tt(WG8[0:NS, :], WG8[0:NS, :], sb_view(GT, GT.full[0:NS, 16, bi:24:3]), ALU.mult)
            wgb = sb_view(WG8, WG8.full[0:NS, :].unsqueeze(2).to_broadcast([NS, 8, 64]))
            kb.tt(TMPQ[0:NS, :, :], NUM[0:NS, :, 0:64], wgb, ALU.mult)
            o3 = sb_view(OTOK, OTOK.full[0:NS, 0, :].rearrange("p (h d) -> p h d", h=8))
            kb.tt(o3, o3, TMPQ[0:NS, :, :], ALU.add)

        kb.dma("sp", OCT[0:NS, :, :], kb.dview("sc_oc", sc_oc.rearrange("b g h d -> b (g h) d")))
        g0b = sb_view(GT, GT.full[0:NS, 16, 0:24:3].unsqueeze(2).to_broadcast([NS, 8, 64]))
        kb.tt(sb_view(OTOK, OTOK.full[0:NS, 0, :].rearrange("p (h d) -> p h d", h=8)), OCT[0:NS, :, :], g0b, ALU.mult)

        dbg_n = [0]
        def dump_otok():
            if debug == "samp":
                o_ = dout("d_otok%d" % dbg_n[0], [NS, 512])
                dbg_n[0] += 1
                kb.dma("sp", kb.dview("dotok%d" % dbg_n[0], o_), OTOK[0:NS, 0, :])
        dump_otok()
        all_halves = [(slice(0, 64), 0), (slice(64, 128), 1)]
        for pc in range(2):
            idxv = IDX2[:, pc:pc + 1]
            for t_, pool_i in ((GA[0], 2), (GA[1], 3)):
                kb.dma("pool", t_[:, :], idxv, fn=lambda e, t_=t_, idxv=idxv, pool_i=pool_i: e.indirect_dma_start(
                    out=t_[:, :].ap, out_offset=None, in_=d_pools[pool_i], in_offset=bass.IndirectOffsetOnAxis(ap=idxv.ap, axis=0)))
            kfn = lambda gp_, g: sb_view(GA[0], GA[0].full[gp_, :].rearrange("p (t g d) -> p t g d", g=2, d=64)[:, :, g, :])
            vfn = lambda gp_, g: sb_view(GA[1], GA[1].full[gp_, :].rearrange("p (t g d) -> p d g t", g=2, d=64)[:, :, g, :])
            mfn = lambda: kb.ts(ED[:, :, :], ED[:, :, :], sconst(SC_MSKR, 1), ALU.mult)
            dve_attend(kfn, vfn, all_halves, PARTS[pc], mfn)
        kb.tt(PARTS[0][:, :, :], PARTS[0][:, :, :], PARTS[1][:, :, :], ALU.add)
        finish_branch(PARTS[0], KNS, VNS, 1)
        dump_otok()

        for kv in range(2):
            for g in range(2):
                src = d_wins[kv].rearrange("b (c t) (g d) -> g (b c) t d", c=16, g=2)[g]
                kb.dma("sp", GW[kv][g * 64:(g + 1) * 64, :, :], View(src, None, 0, 0))
        kfn = lambda gp_, g: GW[0][gp_, :, :]
        vfn = lambda gp_, g: sb_view(GW[1], GW[1].full[gp_, :, :].rearrange("p t d -> p d t"))
        def mfn_w():
            mb = sb_view(SCN, SCN.full[:, SC_MSKW:SC_MSKW + 32].unsqueeze(1).to_broadcast([128, 4, 32]))
            kb.tt(ED[:, :, :], ED[:, :, :], mb, ALU.mult)
        dve_attend(kfn, vfn, [(slice(0, 128), 0)], PARTS[1], mfn_w)
        finish_branch(PARTS[1], KNW, VNW, 2)
        dump_otok()
        out_norm_to_H(NS, 1, SEQ)

    def mixer_out():
        for half in range(2):
            kb.dma("pool", WA[half][:, :, :], View(d_wout[half], None, 0, 0))
        for half in range(2):
            for dch in range(4):
                dk = half * 4 + dch
                for (c0, w) in COLT:
                    b = nb()
                    for kc in range(8):
                        rhs = CN[:, kc, c0:c0 + w] if kc < 4 else H[:, kc, c0:c0 + w]
                        kb.mm(kb.P(b, 0, w), WA[half][:, kc, dch * 128:(dch + 1) * 128], rhs, start=(kc == 0), stop=(kc == 7))
                    kb.tt(X[:, dk, c0:c0 + w], X[:, dk, c0:c0 + w], kb.P(b, 0, w), ALU.add)

    _sample_attention()
    mixer_out()
    ffn(1, NV_FFN2)
    A7 = Alloc(kb, SCR)
    rmsnorm(A7, X, lambda k, c0, w: X[:, k, c0:c0 + w], NV_FIN)
    for k in range(8):
        kb.dma("sp", kb.dview("yT", o_y[:, k, :]), X[:, k, :])
    kb.finish()
    return nc


def _tile_up(w):
    w = w.reshape(8, 128, 2, 11, 256)
    return np.ascontiguousarray(w.transpose(3, 1, 0, 2, 4))


def _tile_dn(w):
    w = w.reshape(22, 128, 4, 256)
    return np.ascontiguousarray(w.transpose(2, 1, 0, 3))


def _fm(v, nchunk):
    return np.ascontiguousarray(v.reshape(nchunk, 128).T)


def prep_shared(inp):
    f = lambda k: np.asarray(inp[k], dtype=np.float32)[0]
    sh = {}
    sh["ffn1_up"] = _tile_up(f("ffn1_w_in"))
    sh["ffn2_up"] = _tile_up(f("ffn2_w_in"))
    sh["ffn1_dn"] = _tile_dn(f("ffn1_w_out"))
    sh["ffn2_dn"] = _tile_dn(f("ffn2_w_out"))
    w_in = f("w_in")
    cols = list(range(1024))
    for c in range(4):
        cols += list(range(1024 + c * 64, 1024 + c * 64 + 64)) + list(range(1024 + (4 + c) * 64, 1024 + (4 + c) * 64 + 64))
    cols += list(range(1536, N_IN))
    wp = np.zeros((1024, 2560), np.float32)
    wp[:, :N_IN] = w_in[:, cols]
    sh["w_in_t"] = np.ascontiguousarray(wp.reshape(8, 128, 5, 512).transpose(2, 1, 0, 3))
    sh["w_out_t"] = np.ascontiguousarray(f("w_out").reshape(8, 128, 2, 512).transpose(2, 1, 0, 3))
    vec = np.zeros((128, NV_TOT), np.float32)
    vec[:, NV_FFN1:NV_FFN1 + 8] = _fm(f("ffn1_norm"), 8)
    vec[:, NV_MIX:NV_MIX + 8] = _fm(f("mix_norm"), 8)
    vec[:, NV_FFN2:NV_FFN2 + 8] = _fm(f("ffn2_norm"), 8)
    vec[:, NV_FIN:NV_FIN + 8] = _fm(f("final_norm"), 8)
    cw = f("conv_w")
    vec[:, NV_CONVW:NV_CONVW + 124] = cw.reshape(31, 4, 128).transpose(2, 1, 0).reshape(128, 124)
    vec[:, NV_CONVB:NV_CONVB + 4] = _fm(f("conv_b"), 4)
    vec[:, NV_LNG:NV_LNG + 4] = _fm(f("conv_ln_g"), 4)
    vec[:, NV_LNB:NV_LNB + 4] = _fm(f("conv_ln_b"), 4)
    vec[:, NV_ONC:NV_ONC + 4] = _fm(f("out_norm_conv"), 4)
    vec[:, NV_ONA:NV_ONA + 4] = _fm(f("out_norm_attn"), 4)
    for i, k in enumerate(["q_norm", "k_cmp_norm", "k_sel_norm", "k_win_norm"]):
        vec[:, NV_QN + i] = np.tile(f(k), 2)
    sh["vec"] = vec
    sh["cb"] = _consts()
    sh["idf"] = np.eye(128, dtype=np.float32)
    pos = np.concatenate([np.arange(SEQ), np.full(NS, PAST)]).astype(np.float32)
    sh["rope_tok"] = _rope_tab(pos)
    sh["rope_cmp"] = _rope_tab((np.arange(1024) * 16 + 31).astype(np.float32))
    sh["bonus_p"] = _bonus_prompt()
    for nm, key in (("cmpk", "cmp_k"), ("cmpv", "cmp_v")):
        w1 = f(key + "_w1")
        a = np.ascontiguousarray(w1.transpose(1, 0, 2))
        sh[nm + "_w1a"] = np.concatenate([a, a], axis=0)
        pe = np.ascontiguousarray(f(key + "_pos").T)
        sh[nm + "_pea"] = np.concatenate([pe, pe], axis=0)
        sh[nm + "_w2"] = f(key + "_w2")
    sh["sconst"] = _sconst()
    sh["ovs"] = _ovs()
    for nm, key in (("pool_kc", "cache_k_cmp"), ("pool_vc", "cache_v_cmp"), ("pool_ks", "cache_k_sel"), ("pool_vs", "cache_v_sel")):
        sh[nm] = np.asarray(inp[key], dtype=np.float32).reshape(N_POOL * 4, 4096)
    return sh


def prep_core(inp, c):
    xp = np.asarray(inp["x_prompt"], dtype=np.float32)[c]
    xs = np.asarray(inp["x_sample"], dtype=np.float32)[NS * c:NS * c + NS, 0]
    x = np.concatenate([xp, xs], axis=0)
    m = {"xT": np.ascontiguousarray(x.T.reshape(8, 128, TT).transpose(1, 0, 2))}
    sc = np.asarray(inp["state_conv"], dtype=np.float32)[0, NS * c:NS * c + NS]
    m["sconvT"] = np.ascontiguousarray(sc.reshape(NS, 30, 4, 128).transpose(3, 2, 0, 1))
    pt = np.asarray(inp["page_table"], dtype=np.int32)[NS * c:NS * c + NS]
    m["ptT"] = np.ascontiguousarray(pt.T)
    p = np.arange(128)
    m["ptB"] = np.ascontiguousarray(pt[(p // 16) % 4])
    m["win_k"] = np.asarray(inp["state_k_win"], dtype=np.float32)[0, NS * c:NS * c + NS].reshape(NS, 512, 128)
    m["win_v"] = np.asarray(inp["state_v_win"], dtype=np.float32)[0, NS * c:NS * c + NS].reshape(NS, 512, 128)
    return m


def assemble(results):
    n = len(results)
    yp, ys = [], []
    kv = [[] for _ in range(6)]
    skv = [[] for _ in range(4)]
    pw = [[], []]
    sw = [[], []]
    pconv, sconv = [], []
    for r in results:
        yT = np.asarray(r["yT"])
        y = yT.transpose(2, 1, 0).reshape(TT, D_MODEL)
        yp.append(y[:SEQ])
        ys.append(y[SEQ:])
        kvT = np.asarray(r["kvT"])
        for i in range(6):
            t = kvT[:, i, :].T
            if i < 4:
                kv[i].append(t[:SEQ].reshape(SEQ, 2, HD))
                skv[i].append(t[SEQ:].reshape(NS, 1, 2, HD))
            else:
                pw[i - 4].append(t[SEQ - 512:SEQ].reshape(512, 2, HD))
        for i in range(2):
            sw[i].append(np.asarray(r["swin_k" if i == 0 else "swin_v"]).reshape(NS, 512, 2, HD))
        pconv.append(np.asarray(r["pconvT"]).transpose(2, 1, 0).reshape(30, 512))
        sconv.append(np.asarray(r["sconvT_out"]).transpose(2, 3, 1, 0).reshape(NS, 30, 512))
    f32 = lambda a: np.ascontiguousarray(a, dtype=np.float32)
    outs = [f32(np.stack(yp)), f32(np.concatenate(ys)[:, None, :])]
    outs += [f32(np.stack(kv[i])[None]) for i in range(4)]
    outs += [f32(np.stack(pw[i])[None]) for i in range(2)]
    outs += [f32(np.stack(pconv)[None])]
    outs += [f32(np.concatenate(skv[i])[None]) for i in range(4)]
    outs += [f32(np.concatenate(sw[i])[None]) for i in range(2)]
    outs += [f32(np.concatenate(sconv)[None])]
    return tuple(outs)


def kernel(**inputs):
    n = 8
    sh = prep_shared(inputs)
    in_maps = []
    for c in range(n):
        m = dict(sh)
        m.update(prep_core(inputs, c))
        in_maps.append(m)
    nc = build_program()
    res = run_bass_kernel_spmd(nc, in_maps, core_ids=list(range(n)))
    return assemble(res.results)
```

```python
import numpy as np
import ml_dtypes
import concourse.bass as bass
import concourse.mybir as mybir
from concourse.bass_utils import run_bass_kernel_spmd

F32 = mybir.dt.float32
BF16 = mybir.dt.bfloat16
I32 = mybir.dt.int32
U32 = mybir.dt.uint32
U8 = mybir.dt.uint8
AF = mybir.ActivationFunctionType
ALU = mybir.AluOpType
AX = mybir.AxisListType

D_MODEL = 1024
SEQ = 2048
NS = 4
TT = SEQ + NS
D_FF = 2816
NFC = D_FF // 128
HD = 64
N_HEADS = 8
PAST = 16384
PAGE = 128
NPAGES = PAST // PAGE
N_POOL = 5120
EPS = 1e-6
SCALE = HD ** -0.5
N_IN = 2328
CONV_W = 31
NCMP_P = 127
NCMP_S = 1023
NSEL_P = 32
ROPE_THETA = 500000.0
ESZ = {F32: 4, BF16: 2, I32: 4, U32: 4, U8: 1}

COLT = [(0, 512), (512, 512), (1024, 512), (1536, 512), (2048, NS)]


class Prod:
    def __init__(self, name, sem, inc):
        self.name, self.sem, self.inc, self.count = name, sem, inc, 0


class Space:
    def __init__(self, name):
        self.name = name
        self.segs = []

    def touch(self, lo, hi, write, deps):
        segs = self.segs
        out = []
        inside = []
        cur = lo
        for s in segs:
            slo, shi, w, rd = s
            if shi <= lo or slo >= hi:
                out.append(s)
                continue
            if slo < lo:
                out.append([slo, lo, w, dict(rd)])
            a, b = max(slo, lo), min(shi, hi)
            if a > cur:
                ns = [cur, a, None, {}]
                out.append(ns)
                inside.append(ns)
            ns = [a, b, w, dict(rd)]
            out.append(ns)
            inside.append(ns)
            cur = b
            if shi > hi:
                out.append([hi, shi, w, dict(rd)])
        if cur < hi:
            ns = [cur, hi, None, {}]
            out.append(ns)
            inside.append(ns)
        out.sort(key=lambda s: s[0])
        self.segs = out
        for s in inside:
            if s[2] is not None:
                p, t = s[2]
                if deps.get(p, 0) < t:
                    deps[p] = t
            if write:
                for p, t in s[3].items():
                    if deps.get(p, 0) < t:
                        deps[p] = t
        return inside

    def mark(self, lo, hi, write, who):
        inside = self.touch(lo, hi, write, {})
        if write:
            keep = [s for s in self.segs if s[1] <= lo or s[0] >= hi]
            keep.append([lo, hi, who, {}])
            keep.sort(key=lambda s: s[0])
            self.segs = keep
        else:
            p, t = who
            for s in inside:
                if s[3].get(p, 0) < t:
                    s[3][p] = t


class View:
    def __init__(self, ap, space, lo, hi):
        self.ap, self.space, self.lo, self.hi = ap, space, lo, hi


class Tile:
    def __init__(self, kb, name, shape, dtype, off, parts=128):
        self.kb, self.name, self.shape, self.dtype, self.off = kb, name, tuple(shape), dtype, off
        self.esz = ESZ[dtype]
        n = int(np.prod(shape))
        self.nbytes = n * self.esz
        ap = kb.arena[0:parts, off:off + self.nbytes].bitcast(dtype)
        if len(shape) == 2:
            ap = ap.rearrange("p (a b) -> p a b", a=shape[0])
        elif len(shape) == 3:
            ap = ap.rearrange("p (a b c) -> p a b c", a=shape[0], b=shape[1])
        elif len(shape) == 4:
            ap = ap.rearrange("p (a b c d) -> p a b c d", a=shape[0], b=shape[1], c=shape[2])
        self.full = ap
        st = []
        acc = 1
        for s in reversed(shape):
            st.append(acc)
            acc *= s
        self.strides = list(reversed(st))

    def __getitem__(self, idx):
        if not isinstance(idx, tuple):
            idx = (idx,)
        pidx = idx[0]
        fidx = list(idx[1:]) + [slice(None)] * (len(self.shape) - len(idx) + 1)
        lo = 0
        hi = 0
        for i, (ix, n, s) in enumerate(zip(fidx, self.shape, self.strides)):
            if isinstance(ix, int):
                a, b = ix, ix + 1
            else:
                a = 0 if ix.start is None else ix.start
                b = n if ix.stop is None else ix.stop
                step = 1 if ix.step is None else ix.step
                b = a + ((b - a - 1) // step) * step + 1
            assert 0 <= a < b <= n, (self.name, idx, self.shape)
            lo += a * s
            hi += (b - 1) * s
        hi += 1
        ap = self.full[(pidx,) + tuple(fidx)]
        return View(ap, self.kb.sb, self.off + lo * self.esz, self.off + hi * self.esz)

    def all(self):
        return self[:]


class KB:
    def __init__(self, nc):
        self.nc = nc
        self.sb = Space("sbuf")
        self.ps = Space("psum")
        self.dram = {}
        self.eng = {}
        for name, h in [("pe", nc.tensor), ("act", nc.scalar), ("dve", nc.vector), ("pool", nc.gpsimd), ("sp", nc.sync)]:
            sem = nc.semaphore("sem_" + name).__enter__()
            p = Prod(name, sem, 1)
            p.h = h
            p.waited = {}
            self.eng[name] = p
        self.dsem = {"sp": [], "pool": [], "act": []}
        for q, n in [("sp", 12), ("pool", 10), ("act", 2)]:
            for i in range(n):
                sem = nc.semaphore(f"dsem_{q}{i}").__enter__()
                self.dsem[q].append(Prod(f"d{q}{i}", sem, 16))
        self.drr = {"sp": 0, "pool": 0, "act": 0}
        self.arena = nc.sbuf_tensor("arena", [128, ARENA], U8).__enter__()
        self.psum = nc.psum_tensor("psum", [128, 8, 512], F32).__enter__()
        self.n_ops = 0

    def P(self, bank, a=0, b=512, p0=0, p1=128):
        return View(self.psum[p0:p1, bank, a:b], self.ps, bank * 2048, bank * 2048 + 2048)

    def Pbf(self, bank, a=0, b=1024, p0=0, p1=128):
        return View(self.psum[p0:p1, bank, :].bitcast(BF16)[:, a:b], self.ps, bank * 2048, bank * 2048 + 2048)

    def dview(self, name, ap):
        sp = self.dram.setdefault(name, Space(name))
        return View(ap, sp, 0, 1)

    def _sync(self, E, reads, writes, skip_self=False):
        deps = {}
        for v in reads:
            if v is not None and v.space is not None:
                v.space.touch(v.lo, v.hi, v.space is self.ps, deps)
        for v in writes:
            if v.space is not None:
                v.space.touch(v.lo, v.hi, True, deps)
        for P, t in deps.items():
            if P is E and skip_self:
                continue
            if E.waited.get(P, 0) < t:
                E.h.wait_ge(P.sem, t)
                E.waited[P] = t

    def _mark(self, who, reads, writes):
        for v in reads:
            if v is not None and v.space is not None:
                v.space.mark(v.lo, v.hi, v.space is self.ps, who)
        for v in writes:
            if v.space is not None:
                v.space.mark(v.lo, v.hi, True, who)

    def op(self, eng, fn, reads=(), writes=()):
        E = self.eng[eng]
        self._sync(E, reads, writes, skip_self=(eng == "pe"))
        inst = fn(E.h)
        E.count += 1
        inst.then_inc(E.sem, 1)
        self._mark((E, E.count), reads, writes)
        self.n_ops += 1
        return inst

    def dma(self, q, out, in_, fn=None):
        Q = self.eng[q]
        reads = [in_]
        writes = [out]
        self._sync(Q, reads, writes)
        lst = self.dsem[q]
        S = lst[self.drr[q] % len(lst)]
        self.drr[q] += 1
        if Q.waited.get(S, 0) < S.count:
            Q.h.wait_ge(S.sem, S.count)
            Q.waited[S] = S.count
        if fn is None:
            inst = Q.h.dma_start(out=out.ap, in_=in_.ap)
        else:
            inst = fn(Q.h)
        S.count += 16
        inst.then_inc(S.sem, 16)
        self._mark((S, S.count), reads, writes)
        self.n_ops += 1

    def finish(self):
        E = self.eng["sp"]
        for q in self.dsem:
            for S in self.dsem[q]:
                if S.count > 0 and E.waited.get(S, 0) < S.count:
                    E.h.wait_ge(S.sem, S.count)
                    E.waited[S] = S.count
        for name, P in self.eng.items():
            if name != "sp" and P.count > 0:
                E.h.wait_ge(P.sem, P.count)

    def mm(self, out, lhsT, rhs, start, stop, **kw):
        self.op("pe", lambda e: e.matmul(out.ap, lhsT=lhsT.ap, rhs=rhs.ap, start=start, stop=stop,
                                         skip_group_check=True, **kw), reads=[lhsT, rhs], writes=[out])

    def transpose(self, out, in_, ident):
        self.op("pe", lambda e: e.transpose(out=out.ap, in_=in_.ap, identity=ident.ap), reads=[in_, ident], writes=[out])

    def act(self, out, in_, func, scale=1.0, bias=0.0, accum=None):
        reads = [in_]
        kw = {}
        if isinstance(scale, View):
            reads.append(scale)
            kw["scale"] = scale.ap
        else:
            kw["scale"] = float(scale)
        if isinstance(bias, View):
            reads.append(bias)
            kw["bias"] = bias.ap
        else:
            kw["bias"] = float(bias)
        writes = [out]
        if accum is not None:
            writes.append(accum)
            kw["accum_out"] = accum.ap
        self.op("act", lambda e: e.activation(out=out.ap, in_=in_.ap, func=func, **kw), reads=reads, writes=writes)

    def tt(self, out, a, b, op, eng="dve"):
        self.op(eng, lambda e: e.tensor_tensor(out=out.ap, in0=a.ap, in1=b.ap, op=op), reads=[a, b], writes=[out])

    def ts(self, out, a, s1, op0, s2=None, op1=None, eng="dve", accum=None):
        reads = [a]
        k1 = s1.ap if isinstance(s1, View) else float(s1)
        if isinstance(s1, View):
            reads.append(s1)
        k2 = None
        if s2 is not None:
            k2 = s2.ap if isinstance(s2, View) else float(s2)
            if isinstance(s2, View):
                reads.append(s2)
        writes = [out]
        kw = {}
        if accum is not None:
            writes.append(accum)
            kw["accum_out"] = accum.ap
        if op1 is None:
            self.op(eng, lambda e: e.tensor_scalar(out=out.ap, in0=a.ap, scalar1=k1, scalar2=None, op0=op0, **kw),
                    reads=reads, writes=writes)
        else:
            self.op(eng, lambda e: e.tensor_scalar(out=out.ap, in0=a.ap, scalar1=k1, scalar2=k2, op0=op0, op1=op1, **kw),
                    reads=reads, writes=writes)

    def stt(self, out, a, s, b, op0, op1):
        reads = [a, b]
        k = s.ap if isinstance(s, View) else float(s)
        if isinstance(s, View):
            reads.append(s)
        self.op("dve", lambda e: e.scalar_tensor_tensor(out=out.ap, in0=a.ap, scalar=k, in1=b.ap, op0=op0, op1=op1),
                reads=reads, writes=[out])

    def copy(self, out, in_, eng="dve"):
        if eng == "act":
            self.op("act", lambda e: e.copy(out=out.ap, in_=in_.ap), reads=[in_], writes=[out])
        else:
            self.op(eng, lambda e: e.tensor_copy(out=out.ap, in_=in_.ap), reads=[in_], writes=[out])

    def memset(self, out, val, eng="dve"):
        self.op(eng, lambda e: e.memset(out.ap, val), reads=[], writes=[out])

    def recip(self, out, in_):
        self.op("dve", lambda e: e.reciprocal(out=out.ap, in_=in_.ap), reads=[in_], writes=[out])

    def reduce(self, out, in_, op=ALU.add, axis=AX.X):
        self.op("dve", lambda e: e.tensor_reduce(out=out.ap, in_=in_.ap, axis=axis, op=op), reads=[in_], writes=[out])


ARENA = 207 * 1024


NV_FFN1, NV_MIX, NV_FFN2, NV_FIN = 0, 8, 16, 24
NV_CONVW = 32
NV_CONVB = NV_CONVW + 124
NV_LNG = NV_CONVB + 4
NV_LNB = NV_LNG + 4
NV_ONC = NV_LNB + 4
NV_ONA = NV_ONC + 4
NV_QN = NV_ONA + 4
NV_TOT = NV_QN + 4

CB_ONES, CB_ID, CB_BLK, CB_PM = 0, 128, 256, 384
CB_E = 512
CB_CM = CB_E + 2048
CB_CMP = CB_CM + 8 * 512
CB_OVP = CB_CMP + 2048
CB_TOT = CB_OVP + 33


def _consts():
    cb = np.zeros((128, CB_TOT), np.float32)
    cb[:, CB_ONES:CB_ONES + 128] = 1.0
    cb[:, CB_ID:CB_ID + 128] = np.eye(128, dtype=np.float32)
    blk = np.zeros((128, 128), np.float32)
    blk[:64, :64] = 1
    blk[64:, 64:] = 1
    cb[:, CB_BLK:CB_BLK + 128] = blk
    pm = np.zeros((128, 128), np.float32)
    for base in (0, 64):
        for i in range(8):
            pm[base + i + 8, base + i] = -1.0
            pm[base + i, base + i + 8] = 1.0
    cb[:, CB_PM:CB_PM + 128] = pm
    k = np.arange(2048)
    for j in range(32):
        cb[j, CB_E:CB_E + 2048] = (k // 64 == j)
    kk = np.arange(128)[:, None]
    qq = np.arange(512)[None, :]
    for r in range(4):
        cb[:, CB_CM + r * 512:CB_CM + (r + 1) * 512] = (kk + 128 * r <= qq)
    for r in range(1, 5):
        cb[:, CB_CM + (3 + r) * 512:CB_CM + (4 + r) * 512] = (qq - kk + 128 * r < 512)
    n = np.arange(128)[:, None]
    q = np.arange(2048)[None, :]
    cb[:, CB_CMP:CB_CMP + 2048] = ((16 * n + 31 <= q) & (n < 127))
    cs = np.arange(127)[:, None] * 16
    ss = np.arange(32)[None, :] * 64
    ov = np.clip(np.minimum(cs + 32, ss + 64) - np.maximum(cs, ss), 0, None).astype(np.float32) / 32
    cb[:127, CB_OVP] = 1.0
    cb[:127, CB_OVP + 1:CB_OVP + 33] = ov
    return cb


def _rope_tab(pos):
    half = 8
    inv = (np.float32(ROPE_THETA) ** (-(np.arange(half, dtype=np.float32) * np.float32(2.0) / np.float32(16)))).astype(np.float32)
    ang = pos.astype(np.float32)[:, None] * inv[None, :]
    cos = np.cos(ang).astype(np.float32)
    sin = np.sin(ang).astype(np.float32)
    n = pos.shape[0]
    tab = np.zeros((128, 2, n), np.float32)
    tab[:, 0, :] = 1.0
    for base in (0, 64):
        for i in range(8):
            tab[base + i, 0] = cos[:, i]
            tab[base + i + 8, 0] = cos[:, i]
            tab[base + i, 1] = sin[:, i]
            tab[base + i + 8, 1] = sin[:, i]
    return tab


SC_SELC = 0
SC_BONS = SC_SELC + 64
SC_MK = SC_BONS + 256
SC_MSKR = SC_MK + 32
SC_MSKW = SC_MSKR + 1
SC_IND2 = SC_MSKW + 32
SC_INDB = SC_IND2 + 4
SC_IOTA = SC_INDB + 128
SC_H2 = SC_IOTA + 256
SC_TOT = SC_H2 + 2


def _sconst():
    sc = np.zeros((128, SC_TOT), np.float32)
    for bg in range(8):
        sc[0:4, SC_SELC + bg * 8 + bg] = 1.0
    sc[0:8, SC_BONS + 0] = 1e4
    sc[0:8, SC_BONS + 255] = 1e4
    sc[:, SC_MK:SC_MK + 32] = 1.0
    sc[127, SC_MK + 28:SC_MK + 32] = 0.0
    p = np.arange(128)
    r = p % 16
    b = (p // 16) % 4
    sc[:, SC_MSKR] = (r != 15)
    sc[:, SC_MSKW:SC_MSKW + 32] = 1.0
    sc[r == 0, SC_MSKW] = 0.0
    for bb in range(4):
        sc[:, SC_IND2 + bb] = (b == bb)
        sc[bb, SC_INDB:SC_INDB + 128] = (b == bb)
    sc[:, SC_IOTA:SC_IOTA + 256] = np.arange(256)[None, :]
    sc[:, SC_H2 + 1] = 2.0
    return sc


def _ovs():
    cs = np.arange(1024)[:, None] * 16
    ss = np.arange(257)[None, :] * 64
    ov = np.clip(np.minimum(cs + 32, ss + 64) - np.maximum(cs, ss), 0, None).astype(np.float32) / 32
    ov[1023] = 0.0
    return np.ascontiguousarray(ov.reshape(128, 8, 257))


def _bonus_prompt():
    q = np.arange(2048)
    cur = q // 64
    j = np.arange(32)[None, :]
    valid = j <= cur[:, None]
    forced = (j == 0) | (j == cur[:, None]) | (j == cur[:, None] - 1)
    b = np.where(valid, np.where(forced, 1e4, 0.0), -1e30).astype(np.float32)
    return np.ascontiguousarray(b.reshape(16, 128, 32).transpose(1, 0, 2))


class Alloc:
    def __init__(self, kb, base=0):
        self.kb, self.cur, self.peak = kb, base, base

    def __call__(self, name, shape, dtype, parts=128):
        self.cur = (self.cur + 31) // 32 * 32
        t = Tile(self.kb, name, shape, dtype, self.cur, parts)
        self.cur += t.nbytes
        self.peak = max(self.peak, self.cur)
        assert self.cur <= ARENA, (name, self.cur)
        return t


def build_program(debug=None):
    nc = bass.Bass("TRN2", target_bir_lowering=False)
    kb = KB(nc)
    dbg_outs = {}

    def din(name, shape, dtype=F32):
        return nc.dram_tensor(name, list(shape), dtype, kind="ExternalInput").ap()

    def dout(name, shape, dtype=F32):
        return nc.dram_tensor(name, list(shape), dtype, kind="ExternalOutput").ap()

    d_xT = din("xT", [128, 8, TT])
    d_up = [din("ffn1_up", [11, 128, 8, 2, 256]), din("ffn2_up", [11, 128, 8, 2, 256])]
    d_dn = [din("ffn1_dn", [4, 128, 22, 256]), din("ffn2_dn", [4, 128, 22, 256])]
    d_win = din("w_in_t", [5, 128, 8, 512])
    d_wout = din("w_out_t", [2, 128, 8, 512])
    d_vec = din("vec", [128, NV_TOT])
    d_cb = din("cb", [128, CB_TOT])
    d_idf = din("idf", [128, 128])
    d_rope = din("rope_tok", [128, 2, TT])
    d_ropec = din("rope_cmp", [128, 2, 1024])
    d_bonus = din("bonus_p", [128, 16, 32])
    d_w1a = [din("cmpk_w1a", [128, 32, 128]), din("cmpv_w1a", [128, 32, 128])]
    d_pea = [din("cmpk_pea", [128, 32]), din("cmpv_pea", [128, 32])]
    d_w2 = [din("cmpk_w2", [128, 64]), din("cmpv_w2", [128, 64])]

    o_y = dout("yT", [128, 8, TT])
    o_kv = dout("kvT", [128, 6, TT])
    o_pconv = dout("pconvT", [128, 4, 30])

    A = Alloc(kb)
    X = A("X", [8, TT], F32)
    H = A("H", [8, TT], BF16)
    VEC = A("VEC", [NV_TOT], F32)
    CB = A("CB", [512], BF16)
    IDF = A("IDF", [128], F32)
    WA = [A("WA0", [8, 512], BF16), A("WA1", [8, 512], BF16)]
    WB = [A("WB0", [22, 256], BF16), A("WB1", [22, 256], BF16)]
    SCR = A.cur

    ones = CB[:, CB_ONES:CB_ONES + 128]
    identb = CB[:, CB_ID:CB_ID + 128]
    blk = CB[:, CB_BLK:CB_BLK + 128]
    pmat = CB[:, CB_PM:CB_PM + 128]

    bank_rr = [0]

    reserved = set()

    def nb():
        while True:
            b = bank_rr[0] % 8
            bank_rr[0] += 1
            if b not in reserved:
                return b

    for k in range(8):
        kb.dma("sp", X[:, k, :], View(d_xT[:, k, :], None, 0, 0))
    kb.dma("sp", VEC[:, :], View(d_vec, None, 0, 0))
    kb.dma("sp", IDF[:, :], View(d_idf, None, 0, 0))
    kb.dma("pool", CB[:, 0:512], View(d_cb[:, 0:512], None, 0, 0))

    def rmsnorm(A2, src, dst_fn, gcol, ntile_list=COLT):
        SQ = A2("SQ", [8, 512], BF16)
        LNV = A2("LNV", [512], F32)
        RSTD = A2("RSTD", [512], F32)
        for (c0, w) in ntile_list:
            b = nb()
            for k in range(8):
                kb.act(SQ[:, k, 0:w], src[:, k, c0:c0 + w], AF.Square)
            for k in range(8):
                kb.mm(kb.P(b, 0, w), ones, SQ[:, k, 0:w], start=(k == 0), stop=(k == 7))
            kb.act(LNV[:, 0:w], kb.P(b, 0, w), AF.Ln, scale=1.0 / D_MODEL, bias=EPS)
            kb.act(RSTD[:, 0:w], LNV[:, 0:w], AF.Exp, scale=-0.5)
            for k in range(8):
                kb.stt(dst_fn(k, c0, w), src[:, k, c0:c0 + w], VEC[:, gcol + k:gcol + k + 1], RSTD[:, 0:w], ALU.mult, ALU.mult)

    def ffn(idx, gcol):
        A2 = Alloc(kb, SCR)
        G = A2("G", [NFC, 1028], BF16)
        SA = [A2("SA0", [512], BF16), A2("SA1", [512], BF16)]
        rmsnorm(A2, X, lambda k, c0, w: H[:, k, c0:c0 + w], gcol)
        halves = [[COLT[0], COLT[1], COLT[4]], [COLT[2], COLT[3]]]
        cnt_a = [0]
        for hi_, tiles in enumerate(halves):
            loc = {}
            o = 0
            for (c0, w) in tiles:
                loc[c0] = o
                o += w
            def load_up(g):
                slot = WA[g % 2]
                kb.dma("pool", slot[:, :, :], View(d_up[idx][g].rearrange("p a b c -> p a (b c)"), None, 0, 0))
            load_up(0)
            for g in range(11):
                if g + 1 < 11:
                    load_up(g + 1)
                slot = WA[g % 2]
                for pair in range(2):
                    i = g * 2 + pair
                    for (c0, w) in tiles:
                        ba, bb = nb(), nb()
                        for ab, bk in ((0, ba), (1, bb)):
                            for dc in range(8):
                                kb.mm(kb.P(bk, 0, w), slot[:, dc, ab * 256 + pair * 128: ab * 256 + pair * 128 + 128],
                                      H[:, dc, c0:c0 + w], start=(dc == 0), stop=(dc == 7))
                        sa = SA[cnt_a[0] % 2]
                        cnt_a[0] += 1
                        kb.act(sa[:, 0:w], kb.P(ba, 0, w), AF.Silu)
                        kb.tt(G[:, i, loc[c0]:loc[c0] + w], sa[:, 0:w], kb.P(bb, 0, w), ALU.mult)
            def load_dn(g):
                slot = WB[g % 2]
                kb.dma("pool", slot[:, :, :], View(d_dn[idx][g], None, 0, 0))
            load_dn(0)
            for g in range(4):
                if g + 1 < 4:
                    load_dn(g + 1)
                slot = WB[g % 2]
                for dch in range(2):
                    dk = g * 2 + dch
                    for (c0, w) in tiles:
                        b = nb()
                        for fc in range(NFC):
                            kb.mm(kb.P(b, 0, w), slot[:, fc, dch * 128:(dch + 1) * 128], G[:, fc, loc[c0]:loc[c0] + w],
                                  start=(fc == 0), stop=(fc == NFC - 1))
                        kb.stt(X[:, dk, c0:c0 + w], kb.P(b, 0, w), 0.5, X[:, dk, c0:c0 + w], ALU.mult, ALU.add)

    ffn(0, NV_FFN1)
    if debug == "ffn1":
        for k in range(8):
            kb.dma("sp", kb.dview("yT", o_y[:, k, :]), X[:, k, :])
        kb.finish()
        return nc


    A3 = Alloc(kb, SCR)
    CN = A3("CN", [4, TT], BF16)
    MSCR = A3.cur

    rmsnorm(Alloc(kb, MSCR), X, lambda k, c0, w: H[:, k, c0:c0 + w], NV_MIX)

    A4 = Alloc(kb, MSCR)
    C = A4("C", [4, TT], F32)
    U = A4("U", [30 + SEQ], BF16)
    UT = A4("UT", [30], F32)
    DG = A4("DG", [CONV_W, 128], BF16)
    SIG = A4("SIG", [512], F32)
    UB = A4("UB", [4, NS, 31], F32)
    TMPS = A4("TMPS", [NS, 31], F32)
    AW = Alloc(kb, WB[0].off)
    SQ2 = AW("SQ2", [8, 512], BF16)
    ST1 = AW("ST1", [512], F32)
    ST2 = AW("ST2", [512], F32)
    ST3 = AW("ST3", [512], F32)
    OST = [AW("OST%d" % i, [512], F32) for i in range(4)]
    assert AW.cur <= WB[1].off + WB[1].nbytes
    ost_rr = [0]

    def ost():
        t = OST[ost_rr[0] % 4]
        ost_rr[0] += 1
        return t

    def load_win(g, slot):
        kb.dma("pool", slot[:, :, :], View(d_win[g], None, 0, 0))

    load_win(0, WA[0])
    load_win(1, WA[1])
    kb.memset(U[:, 0:30], 0.0)
    d_sconv = din("sconvT", [128, 4, NS, 30])
    o_sconv = dout("sconvT_out", [128, 4, NS, 30])
    kb.dma("sp", UB[:, :, :, 0:30], View(d_sconv, None, 0, 0))
    for c in range(4):
        for (c0, w) in COLT:
            ba, bb = nb(), nb()
            for slot, bk in ((WA[0], ba), (WA[1], bb)):
                for dc in range(8):
                    kb.mm(kb.P(bk, 0, w), slot[:, dc, c * 128:(c + 1) * 128], H[:, dc, c0:c0 + w], start=(dc == 0), stop=(dc == 7))
            kb.act(SIG[:, 0:w], kb.P(bb, 0, w), AF.Exp, scale=-1.0)
            kb.ts(SIG[:, 0:w], SIG[:, 0:w], 1.0, ALU.add)
            kb.recip(SIG[:, 0:w], SIG[:, 0:w])
            if c0 < SEQ:
                kb.tt(U[:, 30 + c0:30 + c0 + w], SIG[:, 0:w], kb.P(ba, 0, w), ALU.mult)
                if c0 + w == SEQ:
                    kb.tt(UT[:, :], SIG[:, w - 30:w], kb.P(ba, w - 30, w), ALU.mult)
            else:
                kb.tt(UB[:, c, :, 30], SIG[:, 0:w], kb.P(ba, 0, w), ALU.mult)
        kb.dma("sp", kb.dview("pconv", o_pconv[:, c, :]), UT[:, :])
        wc = NV_CONVW + c * 31
        for j in range(CONV_W):
            kb.ts(DG[:, j, :], identb, VEC[:, wc + j:wc + j + 1], ALU.mult, eng=("pool" if j % 2 else "dve"))
        for (c0, w) in COLT[:4]:
            bcv = nb()
            for j in range(CONV_W):
                kb.mm(kb.P(bcv, 0, w), DG[:, j, :], U[:, c0 + j:c0 + j + w], start=(j == 0), stop=(j == CONV_W - 1))
            kb.ts(C[:, c, c0:c0 + w], kb.P(bcv, 0, w), VEC[:, NV_CONVB + c:NV_CONVB + c + 1], ALU.add)
        kb.tt(TMPS[:, :, :], UB[:, c, :, :], View(VEC.full[:, wc:wc + 31].unsqueeze(1).to_broadcast([128, NS, 31]), kb.sb, VEC.off, VEC.off + VEC.nbytes), ALU.mult)
        kb.reduce(C[:, c, SEQ:TT], TMPS[:, :, :])
        kb.ts(C[:, c, SEQ:TT], C[:, c, SEQ:TT], VEC[:, NV_CONVB + c:NV_CONVB + c + 1], ALU.add)
    kb.dma("sp", kb.dview("sconv", o_sconv), UB[:, :, :, 1:31])

    for (c0, w) in COLT:
        b1, b2 = nb(), nb()
        for c in range(4):
            kb.act(SQ2[:, c, 0:w], C[:, c, c0:c0 + w], AF.Copy)
            kb.act(SQ2[:, 4 + c, 0:w], C[:, c, c0:c0 + w], AF.Square)
        for c in range(4):
            kb.mm(kb.P(b1, 0, w), ones, SQ2[:, c, 0:w], start=(c == 0), stop=(c == 3))
        for c in range(4):
            kb.mm(kb.P(b2, 0, w), ones, SQ2[:, 4 + c, 0:w], start=(c == 0), stop=(c == 3))
        kb.ts(ST1[:, 0:w], kb.P(b1, 0, w), 1.0 / 512, ALU.mult)
        kb.tt(ST2[:, 0:w], ST1[:, 0:w], ST1[:, 0:w], ALU.mult)
        kb.stt(ST2[:, 0:w], kb.P(b2, 0, w), 1.0 / 512, ST2[:, 0:w], ALU.mult, ALU.subtract)
        kb.act(ST3[:, 0:w], ST2[:, 0:w], AF.Ln, bias=EPS)
        kb.act(ST3[:, 0:w], ST3[:, 0:w], AF.Exp, scale=-0.5)
        for c in range(4):
            kb.tt(C[:, c, c0:c0 + w], C[:, c, c0:c0 + w], ST1[:, 0:w], ALU.subtract)
            kb.tt(C[:, c, c0:c0 + w], C[:, c, c0:c0 + w], ST3[:, 0:w], ALU.mult)
    for c in range(4):
        kb.act(C[:, c, :], C[:, c, :], AF.Silu, scale=VEC[:, NV_LNG + c:NV_LNG + c + 1], bias=VEC[:, NV_LNB + c:NV_LNB + c + 1])
    for (c0, w) in COLT:
        b1 = nb()
        for c in range(4):
            kb.act(SQ2[:, c, 0:w], C[:, c, c0:c0 + w], AF.Square)
        for c in range(4):
            kb.mm(kb.P(b1, 0, w), ones, SQ2[:, c, 0:w], start=(c == 0), stop=(c == 3))
        kb.act(ST3[:, 0:w], kb.P(b1, 0, w), AF.Ln, scale=1.0 / 512, bias=EPS)
        kb.act(ST3[:, 0:w], ST3[:, 0:w], AF.Exp, scale=-0.5)
        for c in range(4):
            kb.stt(CN[:, c, c0:c0 + w], C[:, c, c0:c0 + w], VEC[:, NV_ONC + c:NV_ONC + c + 1], ST3[:, 0:w], ALU.mult, ALU.mult)

    if debug == "conv":
        o_dbg = dout("dbg", [128, 4, TT])
        for c in range(4):
            kb.copy(C[:, c, :], CN[:, c, :])
            kb.dma("sp", kb.dview("dbg", o_dbg[:, c, :]), C[:, c, :])
        kb.finish()
        return nc


    A5 = Alloc(kb, MSCR)
    QT = A5("QT", [4, TT], BF16)
    KST = A5("KST", [TT], BF16)
    KWT = A5("KWT", [TT], BF16)
    KCT = A5("KCT", [TT], BF16)
    VCT = A5("VCT", [TT], BF16)
    VAS = A5("VAS", [17, 2, 65], BF16)
    VAW = A5("VAW", [17, 2, 65], BF16)
    GT = A5("GT", [17, 24], F32)
    KCMPT = A5("KCMPT", [128], BF16)
    RC = A5("RC", [2, 97], BF16)
    M2END = A5.cur
    AW = Alloc(kb, WB[0].off)
    SQb = AW("SQb", [512], BF16)
    QNB = AW("QNB", [512], BF16)
    VFB = AW("VFB", [512], BF16)
    ST1 = AW("ST1", [512], F32)
    ST2 = AW("ST2", [512], F32)
    ST3 = AW("ST3", [512], F32)
    OST = [AW("OST%d" % i, [512], F32) for i in range(2)]
    ROPE = [AW("ROPE%d" % i, [2, 512], F32) for i in range(2)]
    assert AW.cur <= WB[1].off + WB[1].nbytes
    ost_rr = [0]

    def ost2():
        t = OST[ost_rr[0] % 2]
        ost_rr[0] += 1
        return t

    def norm_rope(ps, w, gcol, cosv, sinv, out_bf, out_f32):
        kb.act(SQb[:, 0:w], ps, AF.Square)
        b2 = nb()
        kb.mm(kb.P(b2, 0, w), blk, SQb[:, 0:w], start=True, stop=True)
        kb.act(ST3[:, 0:w], kb.P(b2, 0, w), AF.Ln, scale=1.0 / HD, bias=EPS)
        kb.act(ST3[:, 0:w], ST3[:, 0:w], AF.Exp, scale=-0.5)
        kb.stt(ST1[:, 0:w], ps, VEC[:, gcol:gcol + 1], ST3[:, 0:w], ALU.mult, ALU.mult)
        kb.act(QNB[:, 0:w], ST1[:, 0:w], AF.Copy)
        b3 = nb()
        kb.mm(kb.P(b3, 0, w), pmat, QNB[:, 0:w], start=True, stop=True)
        kb.tt(ST2[:, 0:w], kb.P(b3, 0, w), sinv, ALU.mult)
        kb.tt(ST1[:, 0:w], ST1[:, 0:w], cosv, ALU.mult)
        if out_f32 is not None:
            kb.tt(out_f32, ST1[:, 0:w], ST2[:, 0:w], ALU.add)
            kb.act(out_bf, out_f32, AF.Copy)
        else:
            kb.tt(out_bf, ST1[:, 0:w], ST2[:, 0:w], ALU.add)

    kb.memset(VAS[:, :, :, 64:65], 1.0)
    kb.memset(VAW[:, :, :, 64:65], 1.0)
    rope_rr = [0]
    KV_SLOT = {"kc": 0, "vc": 1, "ks": 2, "vs": 3, "kw": 4, "vw": 5}
    plan = [
        (2, [(0, "q", 0), (1, "q", 1), (2, "q", 2), (3, "q", 3)]),
        (3, [(0, "kc", None), (1, "vc", None), (2, "ks", None), (3, "vs", None)]),
        (4, [(0, "kw", None), (1, "vw", None), (2, "gate", None)]),
    ]
    items = []
    for gi, (grp, chunks) in enumerate(plan):
        for ti, (c0, w) in enumerate(COLT):
            for k_, (ci, kind, qi) in enumerate(chunks):
                items.append((gi, grp, ti, c0, w, ci, kind, qi, k_ == 0))
    loaded = set()
    state = {"rp": None}

    def m2_proj(it):
        gi, grp, ti, c0, w, ci, kind, qi, first_in_tile = it
        slot = WA[gi % 2]
        if gi not in loaded:
            loaded.add(gi)
            load_win(grp, slot)
        b = nb()
        mrows = 24 if kind == "gate" else 128
        for dc in range(8):
            kb.mm(kb.P(b, 0, w, 0, mrows), slot[:, dc, ci * 128:ci * 128 + mrows], H[:, dc, c0:c0 + w], start=(dc == 0), stop=(dc == 7))
        return b

    def m2_post(it, b):
        gi, grp, ti, c0, w, ci, kind, qi, first_in_tile = it
        if first_in_tile and grp in (2, 3, 4):
            rp_ = ROPE[rope_rr[0] % 2]
            rope_rr[0] += 1
            kb.dma("sp", rp_[:, :, 0:w], View(d_rope[:, :, c0:c0 + w], None, 0, 0))
            state["rp"] = rp_
        rp = state["rp"]
        ps = kb.P(b, 0, w)
        if kind == "q":
            norm_rope(ps, w, NV_QN + 0, rp[:, 0, 0:w], rp[:, 1, 0:w], QT[:, qi, c0:c0 + w], None)
        elif kind in ("ks", "kw"):
            o32 = ost2()
            dst = KST if kind == "ks" else KWT
            norm_rope(ps, w, NV_QN + (2 if kind == "ks" else 3), rp[:, 0, 0:w], rp[:, 1, 0:w], dst[:, c0:c0 + w], o32[:, 0:w])
            kb.dma("sp", kb.dview("kvT", o_kv[:, KV_SLOT[kind], c0:c0 + w]), o32[:, 0:w])
        elif kind in ("kc", "vc", "vs", "vw"):
            o32 = ost2()
            kb.copy(o32[:, 0:w], ps)
            kb.dma("sp", kb.dview("kvT", o_kv[:, KV_SLOT[kind], c0:c0 + w]), o32[:, 0:w])
            if kind == "kc":
                kb.act(KCT[:, c0:c0 + w], ps, AF.Copy)
            elif kind == "vc":
                kb.act(VCT[:, c0:c0 + w], ps, AF.Copy)
            else:
                VA = VAS if kind == "vs" else VAW
                kb.act(VFB[:, 0:w], ps, AF.Copy)
                nsub = (w + 127) // 128
                for sb_ in range(nsub):
                    ww = min(128, w - sb_ * 128)
                    bt = nb()
                    kb.transpose(kb.Pbf(bt, 0, 128, 0, ww), VFB[:, sb_ * 128:sb_ * 128 + ww], identb)
                    tix = c0 // 128 + sb_
                    kb.copy(VA[0:ww, tix, :, 0:64], View(kb.psum[0:ww, bt, :].bitcast(BF16)[:, 0:128].rearrange("p (g d) -> p g d", g=2), kb.ps, bt * 2048, bt * 2048 + 2048))
        else:
            kb.act(ST1[0:24, 0:w], kb.P(b, 0, w, 0, 24), AF.Exp, scale=-1.0)
            kb.ts(ST1[0:24, 0:w], ST1[0:24, 0:w], 1.0, ALU.add)
            kb.recip(ST1[0:24, 0:w], ST1[0:24, 0:w])
            nsub = (w + 127) // 128
            for sb_ in range(nsub):
                ww = min(128, w - sb_ * 128)
                bt = nb()
                kb.transpose(kb.P(bt, 0, 24, 0, ww), ST1[0:24, sb_ * 128:sb_ * 128 + ww], View(IDF.full[0:24, 0:24], kb.sb, IDF.off, IDF.off + IDF.nbytes))
                kb.copy(GT[0:ww, c0 // 128 + sb_, :], kb.P(bt, 0, 24, 0, ww))

    pend = None
    for it in items:
        b = m2_proj(it)
        reserved.add(b)
        if pend is not None:
            m2_post(*pend)
            reserved.discard(pend[1])
        pend = (it, b)
    m2_post(*pend)
    reserved.discard(pend[1])

    if debug == "proj":
        o_dbg = dout("dbg", [128, 7, TT])
        for i, t in enumerate([QT[:, 0, :], QT[:, 1, :], QT[:, 2, :], QT[:, 3, :], KST[:, :], KWT[:, :], KCT[:, :]]):
            for (c0, w) in COLT:
                o32 = ost2()
                kb.copy(o32[:, 0:w], View(t.ap[:, c0:c0 + w], t.space, t.lo, t.hi))
                kb.dma("sp", kb.dview("dbg", o_dbg[:, i, c0:c0 + w]), o32[:, 0:w])
        o_dbg2 = dout("dbg_va", [128, 17, 2, 65])
        o_dbg3 = dout("dbg_gt", [128, 17, 24])
        VAf = A5("VAf", [17, 2, 65], F32)
        kb.copy(VAf[:, :, :, :], VAS[:, :, :, :])
        kb.dma("sp", kb.dview("dbg2", o_dbg2), VAf[:, :, :, :])
        kb.dma("sp", kb.dview("dbg3", o_dbg3), GT[:, :, :])
        kb.finish()
        return nc


    W1A = [Tile(kb, "W1Ak", [32, 128], BF16, WA[0].off), Tile(kb, "W1Av", [32, 128], BF16, WA[1].off)]
    A6 = Alloc(kb, M2END)
    PEA = [A6("PEAk", [32], BF16), A6("PEAv", [32], BF16)]
    W2 = [A6("W2k", [64], BF16), A6("W2v", [64], BF16)]
    BIAS = [A6("BIASk", [1], F32), A6("BIASv", [1], F32)]
    NBIAS = [A6("NBIASk", [1], F32), A6("NBIASv", [1], F32)]
    EH = A6("EH", [128], F32)
    HB = A6("HB", [128], BF16)
    for kv in range(2):
        kb.dma("pool", W1A[kv][:, :, :], View(d_w1a[kv], None, 0, 0))
        kb.dma("pool", PEA[kv][:, :], View(d_pea[kv], None, 0, 0))
        kb.dma("pool", W2[kv][:, :], View(d_w2[kv], None, 0, 0))
    kb.dma("pool", RC[:, 0, 64:97], View(d_cb[:, CB_OVP:CB_OVP + 33], None, 0, 0))
    kb.dma("pool", RC[:, 1, 64:97], View(d_cb[:, CB_OVP:CB_OVP + 33], None, 0, 0))
    kb.dma("sp", ROPE[0][:, :, 0:127], View(d_ropec[:, :, 0:127], None, 0, 0))
    import os
    CMPDBG = int(os.environ.get("CMPDBG", "99"))
    for kv in range(2):
        if CMPDBG < 1:
            break
        b = nb()
        for s_ in range(32):
            kb.mm(kb.P(b, 0, 1), W1A[kv][0:64, s_, :], PEA[kv][0:64, s_:s_ + 1], start=(s_ == 0), stop=(s_ == 31))
        kb.copy(BIAS[kv][:, :], kb.P(b, 0, 1))
        kb.ts(NBIAS[kv][:, :], BIAS[kv][:, :], -1.0, ALU.mult)
    bk = nb()
    for kv in range(2):
        if CMPDBG < 2 + kv:
            break
        src = KCT if kv == 0 else VCT
        for g in range(2):
            b = nb()
            for s_ in range(32):
                kb.mm(kb.P(b, 0, 127), W1A[kv][g * 64:(g + 1) * 64, s_, :], src[g * 64:(g + 1) * 64, s_:s_ + 16 * 126 + 1:16],
                      start=(s_ == 0), stop=(s_ == 31))
            kb.act(EH[:, 0:127], kb.P(b, 0, 127), AF.Exp, scale=-1.0, bias=NBIAS[kv][:, 0:1])
            kb.ts(EH[:, 0:127], EH[:, 0:127], 1.0, ALU.add)
            kb.recip(EH[:, 0:127], EH[:, 0:127])
            kb.stt(HB[:, 0:127], kb.P(b, 0, 127), BIAS[kv][:, 0:1], EH[:, 0:127], ALU.add, ALU.mult)
            if kv == 0:
                kb.mm(kb.P(bk, 0, 127, g * 64, g * 64 + 64), W2[0][:, :], HB[:, 0:127], start=True, stop=True)
            else:
                b2 = nb()
                kb.mm(kb.P(b2, 0, 64, 0, 127), HB[:, 0:127], W2[1][:, :], start=True, stop=True)
                kb.copy(RC[0:127, g, 0:64], kb.P(b2, 0, 64, 0, 127))
        if kv == 0 and CMPDBG != 2:
            norm_rope(kb.P(bk, 0, 127), 127, NV_QN + 1, ROPE[0][:, 0, 0:127], ROPE[0][:, 1, 0:127], KCMPT[:, 0:127], None)

    if debug == "cmp":
        o_dbg = dout("dbg", [128, 128])
        o_dbg2 = dout("dbg2", [128, 2, 97])
        kb.memset(ST1[:, 0:128], 0.0)
        kb.copy(ST1[:, 0:127], KCMPT[:, 0:127])
        kb.dma("sp", kb.dview("dbg", o_dbg), ST1[:, 0:128])
        RCf = A6("RCf", [2, 97], F32)
        kb.memset(RCf[:, :, :], 0.0)
        kb.copy(RCf[0:127, :, :], RC[0:127, :, :])
        kb.dma("sp", kb.dview("dbg2", o_dbg2), RCf[:, :, :])
        kb.finish()
        return nc

    AWB = Alloc(kb, WB[0].off)
    CM = AWB("CM", [8, 512], BF16)
    CMPM = AWB("CMPM", [2048], BF16)
    EE = AWB("EE", [2048], BF16)
    ET = [AWB("ET%d" % i, [512], BF16) for i in range(3)]
    MSK = [AWB("MSK%d" % i, [512], BF16) for i in range(2)]
    ET.append(AWB("ET3", [512], BF16))
    assert AWB.cur <= WB[1].off + WB[1].nbytes
    AH = Alloc(kb, H.off)
    OTOK = AH("OTOK", [4, 512], F32)
    SELT = AH("SELT", [512], BF16)
    IMP = AH("IMP", [4, 32], F32)
    TMPO = AH("TMPO", [4, 64], F32)
    TMPI = AH("TMPI", [4, 32], F32)
    SCO = AH("SCO", [32], F32)
    SC2 = AH("SC2", [32], F32)
    M8 = AH("M8", [16], F32)
    SELF = AH("SELF", [32], F32)
    BON = AH("BON", [4, 32], F32)
    RD = AH("RD", [4], F32)
    WG = AH("WG", [4], F32)
    SS = AH("SS", [4], F32)
    SS2 = AH("SS2", [4], F32)
    ONB = AH("ONB", [512], BF16)
    assert AH.cur <= H.off + 4 * TT * 2
    kb.dma("pool", CM[:, :, :], View(d_cb[:, CB_CM:CB_CM + 4096].rearrange("p (a b) -> p a b", a=8), None, 0, 0))
    kb.dma("pool", CMPM[:, :], View(d_cb[:, CB_CMP:CB_CMP + 2048], None, 0, 0))
    kb.dma("pool", EE[:, :], View(d_cb[:, CB_E:CB_E + 2048], None, 0, 0))
    et_rr = [0]
    sc_rr = [0]
    msk_rr = [0]
    OB = [3, 4, 5, 6]

    def strided_ps(bank, start, step, n, width, rows=128):
        full = kb.psum[0:rows, bank, 0:n * step].rearrange("p (n s) -> p n s", s=step)[:, :, start:start + width]
        return View(full, kb.ps, bank * 2048, bank * 2048 + 2048)

    def strided_ps2(bank, start, step, n, rows=128):
        full = kb.psum[0:rows, bank, 0:n * step].rearrange("p (n s) -> p n s", s=step)[:, :, start]
        return View(full, kb.ps, bank * 2048, bank * 2048 + 2048)

    def evac_branch(bank, W, h, bi, qt0, nsub, rows, first, want_imp, imp_first, IMP):
        den = strided_ps2(bank, 64, W, nsub, rows)
        kb.ts(RD[0:rows, 0:nsub], den, 1e-30, ALU.max)
        kb.recip(RD[0:rows, 0:nsub], RD[0:rows, 0:nsub])
        kb.tt(WG[0:rows, 0:nsub], RD[0:rows, 0:nsub], GT[0:rows, qt0:qt0 + nsub, 3 * h + bi], ALU.mult)
        wgb = View(WG.full[0:rows, 0:nsub].unsqueeze(2).to_broadcast([rows, nsub, 64]), kb.sb, WG.off, WG.off + WG.nbytes)
        onum = strided_ps(bank, 0, W, nsub, 64, rows)
        dst = OTOK[0:rows, 0:nsub, h * 64:(h + 1) * 64]
        if first:
            kb.tt(dst, onum, wgb, ALU.mult)
        else:
            kb.tt(TMPO[0:rows, 0:nsub, :], onum, wgb, ALU.mult)
            kb.tt(dst, dst, TMPO[0:rows, 0:nsub, :], ALU.add)
        if want_imp:
            rdb = View(RD.full[0:rows, 0:nsub].unsqueeze(2).to_broadcast([rows, nsub, 32]), kb.sb, RD.off, RD.off + RD.nbytes)
            oimp = strided_ps(bank, 65, W, nsub, 32, rows)
            if imp_first:
                kb.tt(IMP[0:rows, 0:nsub, :], oimp, rdb, ALU.mult)
            else:
                kb.tt(TMPI[0:rows, 0:nsub, :], oimp, rdb, ALU.mult)
                kb.tt(IMP[0:rows, 0:nsub, :], IMP[0:rows, 0:nsub, :], TMPI[0:rows, 0:nsub, :], ALU.add)

    def out_norm_to_H(rows, nsub, col0):
        for sub in range(nsub):
            kb.act(ONB[0:rows, :], OTOK[0:rows, sub, :], AF.Square, accum=SS[0:rows, sub:sub + 1])
        kb.act(SS2[0:rows, 0:nsub], SS[0:rows, 0:nsub], AF.Ln, scale=1.0 / 512, bias=EPS)
        kb.act(SS2[0:rows, 0:nsub], SS2[0:rows, 0:nsub], AF.Exp, scale=-0.5)
        for sub in range(nsub):
            kb.ts(ONB[0:rows, :], OTOK[0:rows, sub, :], SS2[0:rows, sub:sub + 1], ALU.mult)
            for j in range(4):
                kb.transpose(kb.Pbf(7, j * 128, j * 128 + rows), ONB[0:rows, j * 128:(j + 1) * 128], View(identb.ap[0:rows, 0:rows], kb.sb, identb.lo, identb.hi))
            for j in range(4):
                kb.ts(H[:, 4 + j, col0 + sub * 128:col0 + sub * 128 + rows], kb.Pbf(7, j * 128, j * 128 + rows),
                      VEC[:, NV_ONA + j:NV_ONA + j + 1], ALU.mult)

    SELT2 = [SELT, AH("SELT1", [512], BF16)]
    IMP2 = [IMP, AH("IMP1", [4, 32], F32)]
    ET4 = ET
    assert AH.cur <= H.off + 4 * TT * 2

    def attend(Q):
        q0 = Q * 512
        qcols = slice(q0, q0 + 512)
        tasks = []
        if Q >= 2:
            kb.dma("sp", BON[:, :, :], View(d_bonus[:, Q * 4:Q * 4 + 4, :], None, 0, 0))

        def add(**kw):
            t = dict(pre=None, post=None)
            t.update(kw)
            tasks.append(t)

        def topk(g):
            imp, selt = IMP2[g], SELT2[g]
            for sub in range(4):
                kb.tt(SCO[:, :], imp[:, sub, :], BON[:, sub, :], ALU.add)
                kb.op("dve", lambda e: e.max(out=M8[:, 0:8].ap, in_=SCO[:, :].ap), reads=[SCO[:, :]], writes=[M8[:, 0:8]])
                kb.op("dve", lambda e: e.match_replace(out=SC2[:, :].ap, in_to_replace=M8[:, 0:8].ap, in_values=SCO[:, :].ap, imm_value=-3.0e38),
                      reads=[SCO[:, :], M8[:, 0:8]], writes=[SC2[:, :]])
                kb.op("dve", lambda e: e.max(out=M8[:, 8:16].ap, in_=SC2[:, :].ap), reads=[SC2[:, :]], writes=[M8[:, 8:16]])
                kb.ts(SELF[:, :], SCO[:, :], M8[:, 15:16], ALU.is_ge)
                kb.transpose(kb.P(7, sub * 128, (sub + 1) * 128, 0, 32), SELF[:, :], IDF[:, :])
            kb.copy(selt[0:32, :], kb.P(7, 0, 512, 0, 32))

        for g in range(2):
            gp = slice(g * 64, (g + 1) * 64)
            for hh in range(4):
                def qk(a, gp=gp, hh=hh):
                    kb.mm(kb.P(a, 0, 512, 0, 127), KCMPT[gp, 0:127], QT[gp, hh, qcols], start=True, stop=True)
                def ex(a, et):
                    kb.act(et[0:127, :], kb.P(a, 0, 512, 0, 127), AF.Exp, scale=SCALE)
                    kb.tt(et[0:127, :], et[0:127, :], CMPM[0:127, qcols], ALU.mult)
                def pv(et, g=g, hh=hh):
                    for sub in range(4):
                        kb.mm(kb.P(OB[hh], sub * 97, sub * 97 + 97), et[0:127, sub * 128:(sub + 1) * 128], RC[0:127, g, :],
                              start=(sub == 0), stop=True)
                def post(g=g, hh=hh):
                    evac_branch(OB[hh], 97, 4 * g + hh, 0, Q * 4, 4, 128, True, Q >= 2, hh == 0, IMP2[g])
                    if hh == 3 and Q >= 2:
                        topk(g)
                add(qk=qk, ex=ex, pv=pv, post=post)
        for g in range(2):
            gp = slice(g * 64, (g + 1) * 64)
            for bi, KT, VA in ((1, KST, VAS), (2, KWT, VAW)):
                kt_lo = 0 if bi == 1 else max(0, 4 * Q - 4)
                kts = list(range(kt_lo, 4 * Q + 4))
                started = [False] * 4
                for Kt in kts:
                    r = Kt - 4 * Q
                    pre = None
                    mask_box = [None]
                    if bi == 1:
                        if Q >= 2:
                            mk = MSK[msk_rr[0] % 2]
                            msk_rr[0] += 1
                            def pre(Kt=Kt, r=r, mk=mk, g=g):
                                kb.mm(kb.P(7), EE[0:32, Kt * 128:(Kt + 1) * 128], SELT2[g][0:32, :], start=True, stop=True)
                                if r >= 0:
                                    kb.tt(mk[:, :], kb.P(7), CM[:, r, :], ALU.mult)
                                else:
                                    kb.copy(mk[:, :], kb.P(7))
                            mask_box[0] = mk[:, :]
                        elif r >= 0:
                            mask_box[0] = CM[:, r, :]
                        subs = [s_ for s_ in range(4) if r <= s_]
                    else:
                        mask_box[0] = CM[:, r, :] if r >= 0 else CM[:, 3 - r, :]
                        subs = [s_ for s_ in range(4) if (r <= s_ and s_ - r < 5)]
                    for hh in range(4):
                        def qk(a, gp=gp, hh=hh, Kt=Kt, KT=KT):
                            kb.mm(kb.P(a), KT[gp, Kt * 128:(Kt + 1) * 128], QT[gp, hh, qcols], start=True, stop=True)
                        def ex(a, et, mask=mask_box[0]):
                            kb.act(et[:, :], kb.P(a), AF.Exp, scale=SCALE)
                            if mask is not None:
                                kb.tt(et[:, :], et[:, :], mask, ALU.mult)
                        first = not started[hh]
                        started[hh] = True
                        def pv(et, hh=hh, Kt=Kt, g=g, VA=VA, subs=subs, first=first):
                            f = first
                            for sub in subs:
                                kb.mm(kb.P(OB[hh], sub * 65, sub * 65 + 65), et[:, sub * 128:(sub + 1) * 128], VA[:, Kt, g, :],
                                      start=f, stop=True)
                                f = False
                        post = None
                        if Kt == kts[-1]:
                            def post(g=g, hh=hh, bi=bi):
                                evac_branch(OB[hh], 65, 4 * g + hh, bi, Q * 4, 4, 128, False, False, False, None)
                        add(qk=qk, ex=ex, pv=pv, post=post, pre=(pre if hh == 0 else None))
        n = len(tasks)
        for i in range(n + 2):
            if i < n:
                t = tasks[i]
                if t["pre"] is not None:
                    t["pre"]()
                t["qk"](i % 3)
            if 1 <= i <= n:
                tasks[i - 1]["ex"]((i - 1) % 3, ET4[(i - 1) % 4])
            if i >= 2:
                t = tasks[i - 2]
                t["pv"](ET4[(i - 2) % 4])
                if t["post"] is not None:
                    t["post"]()
        out_norm_to_H(128, 4, q0)

    for Q in range(4):
        attend(Q)
        if debug == "att0":
            break

    if debug in ("att0", "att"):
        o_dbg = dout("dbg", [128, 4, TT])
        for j in range(4):
            for (c0, w) in COLT[:4]:
                o32 = ost2() if False else None
            kb.copy(X[:, j, 0:SEQ], H[:, 4 + j, 0:SEQ])
            kb.dma("sp", kb.dview("dbg", o_dbg[:, j, 0:SEQ]), X[:, j, 0:SEQ])
        kb.finish()
        return nc


    d_pools = [din("pool_kc", [N_POOL * 4, 4096]), din("pool_vc", [N_POOL * 4, 4096]),
               din("pool_ks", [N_POOL * 4, 4096]), din("pool_vs", [N_POOL * 4, 4096])]
    d_wins = [din("win_k", [NS, 512, 128]), din("win_v", [NS, 512, 128])]
    d_ptT = din("ptT", [128, NS], I32)
    d_ptB = din("ptB", [128, 128], I32)
    d_sc = din("sconst", [128, SC_TOT])
    d_ovs = din("ovs", [128, 8, 257])
    o_swin = [dout("swin_k", [NS, 512, 128]), dout("swin_v", [NS, 512, 128])]
    sc_idx = nc.dram_tensor("sc_idx", [128], U32, kind="Internal").ap()
    sc_oc = nc.dram_tensor("sc_oc", [NS, 2, 4, 64], F32, kind="Internal").ap()

    def sb_view(tile, ap):
        return View(ap, kb.sb, tile.off, tile.off + tile.nbytes)

    def _sample_attention():
        AS = Alloc(kb, M2END + 1536)
        SCN = AS("SCN", [SC_TOT], F32)
        kb.dma("sp", SCN[:, :], View(d_sc, None, 0, 0))
        QTOK = AS("QTOK", [4, 2, 64], F32)
        KNS = AS("KNS", [2, 64], F32)
        KNW = AS("KNW", [2, 64], F32)
        PTI = AS("PTI", [NS], I32)
        PTF = AS("PTF", [NS], F32)
        IDXF = AS("IDXF", [NS, 4], F32)
        IDX = AS("IDX", [NS, 4], I32)
        QSF = AS("QSF", [4, NS], BF16)
        VNS = AS("VNS", [2, 64], BF16)
        VNW = AS("VNW", [2, 64], BF16)
        assert AS.cur <= ARENA, AS.cur
        kb.copy(QSF[:, :, :], QT[:, :, SEQ:TT])
        kb.copy(VNS[0:NS, :, :], VAS[0:NS, 16, :, 0:64])
        kb.copy(VNW[0:NS, :, :], VAW[0:NS, 16, :, 0:64])
        AL = Alloc(kb, MSCR)
        QB = AL("QB", [4, 64], F32)
        ETS = AL("ETS", [32], BF16)
        RDS = AL("RDS", [1], F32)
        LSEL = AL("LSEL", [8], F32)
        IMPR = AL("IMPR", [257], F32)
        OCS = AL("OCS", [64], F32)
        SCOS = AL("SCOS", [256], F32)
        SC2S = AL("SC2S", [256], F32)
        M8S = AL("M8S", [16], F32)
        IXS = AL("IXS", [16], U32)
        JI = AL("JI", [1], U32)
        JF = AL("JF", [6], F32)
        IDX2F = AL("IDX2F", [2], F32)
        IDX2 = AL("IDX2", [2], I32)
        PTBI = AL("PTBI", [128], I32)
        PTBF = AL("PTBF", [128], F32)
        OH = AL("OH", [256], F32)
        TAB = AL("TAB", [256], F32)
        SCD = AL("SCD", [4, 32], F32)
        ED = AL("ED", [4, 32], F32)
        PARTS = [AL("PART%d" % i, [4, 65], F32) for i in range(2)]
        NUM = AL("NUM", [8, 65], F32)
        SNE = AL("SNE", [8], F32)
        WG8 = AL("WG8", [8], F32)
        assert AL.cur <= MSCR + 16384, AL.cur
        sconst = lambda a, n, p0=0, p1=128: sb_view(SCN, SCN.full[p0:p1, a:a + n])

        def to_tok(src_view, dst_view):
            bt = nb()
            kb.transpose(kb.Pbf(bt, 0, 128, 0, NS), src_view, identb)
            kb.copy(dst_view, View(kb.psum[0:NS, bt, :].bitcast(BF16)[:, 0:128].rearrange("p (g d) -> p g d", g=2), kb.ps, bt * 2048, bt * 2048 + 2048))
        for c in range(4):
            to_tok(QT[:, c, SEQ:TT], QTOK[0:NS, c, :, :])
        to_tok(KST[:, SEQ:TT], KNS[0:NS, :, :])
        to_tok(KWT[:, SEQ:TT], KNW[0:NS, :, :])

        for kv, slot in ((0, 4), (1, 5)):
            kb.dma("sp", kb.dview("swin%d" % kv, o_swin[kv][:, 0:511, :]), View(d_wins[kv][:, 1:512, :], None, 0, 0))
            kb.dma("sp", kb.dview("swin%d" % kv, o_swin[kv][:, 511, :].rearrange("b p -> p b")), kb.dview("kvT", o_kv[:, slot, SEQ:TT]),
                   fn=lambda e, kv=kv, slot=slot: e.dma_start(out=o_swin[kv][:, 511, :].rearrange("b p -> p b"), in_=o_kv[:, slot, SEQ:TT],
                                                             allow_slow_non_contiguous=True))

        AG = Alloc(kb, WA[0].off)
        GA = [AG("GA0", [4096], F32), AG("GA1", [4096], F32)]
        HBS = AG("HBS", [128], BF16)
        EHS = AG("EHS", [128], F32)
        SQb_ = AG("SQbs", [128], BF16)
        QNB_ = AG("QNBs", [128], BF16)
        S1_ = AG("S1s", [128], F32)
        S2_ = AG("S2s", [128], F32)
        S3_ = AG("S3s", [128], F32)
        AX_ = Alloc(kb, H.off)
        XT = [AX_("XT%d" % i, [16, 128], BF16) for i in range(4)]
        assert AX_.cur <= H.off + 4 * TT * 2
        AC = Alloc(kb, MSCR)
        W1S = AC("W1S", [32, 128], BF16)
        ROPC = AC("ROPC", [2, 1024], F32)
        OVS = AC("OVS", [8, 257], BF16)
        KCS = [AC("KCS%d" % b_, [1024], BF16) for b_ in range(NS)]
        VCS = [[AC("VCS%d%d" % (b_, g), [8, 65], BF16) for g in range(2)] for b_ in range(NS)]
        assert AC.cur <= M2END, (AC.cur, M2END)
        assert OVS.off >= MSCR + 16384
        AL2 = Alloc(kb, OVS.off)
        OCT = AL2("OCT", [8, 64], F32)
        TMPQ = AL2("TMPQ", [8, 64], F32)
        assert AL2.cur <= OVS.off + OVS.nbytes
        kb.dma("sp", ROPC[:, :, :], View(d_ropec, None, 0, 0))
        kb.dma("pool", OVS[:, :, :], View(d_ovs, None, 0, 0))
        kb.dma("sp", PTI[:, :], View(d_ptT, None, 0, 0))
        kb.copy(PTF[:, :], PTI[:, :])
        for q4 in range(4):
            kb.ts(IDXF[:, :, q4], PTF[:, :], 4.0, ALU.mult, float(q4), ALU.add)
        kb.copy(IDX[:, :, :], IDXF[:, :, :])
        for b_ in range(NS):
            kb.memset(KCS[b_][:, 1016:1024], 0.0)
            for g in range(2):
                kb.memset(VCS[b_][g][:, :, :], 0.0)
                kb.memset(VCS[b_][g][:, :, 64:65], 1.0)

        HB2 = [[AG("HBS%d%d" % (i, g), [128], BF16) for g in range(2)] for i in range(2)]
        S1B = [S1_, AG("S1b", [128], F32)]
        QNB2 = [QNB_, AG("QNBb", [128], BF16)]
        assert AG.cur <= WB[1].off + WB[1].nbytes, AG.cur

        ga_rr = [0]
        xt_slot = lambda ci: 0 if ci == 0 else 1 + (ci - 1) % 3
        for kv in range(2):
            kb.dma("pool", W1S[:, :, :], View(d_w1a[kv], None, 0, 0))
            for b_ in range(NS):
                wof = lambda ci: 128 if ci < 7 else 127

                def part1(ci):
                    w = wof(ci)
                    for g in range(2):
                        bh = nb()
                        gp = slice(g * 64, (g + 1) * 64)
                        for sp_ in range(32):
                            s16 = sp_ % 16
                            if sp_ < 16:
                                rhs = XT[xt_slot(ci)][gp, s16, 0:w]
                            elif ci < 7:
                                rhs = XT[xt_slot(ci + 1)][gp, s16, 0:w]
                            else:
                                rhs = XT[0][gp, s16, 1:128]
                            kb.mm(kb.P(bh, 0, w), W1S[gp, sp_, :], rhs, start=(sp_ == 0), stop=(sp_ == 31))
                        hb = HB2[ci % 2][g]
                        kb.act(EHS[:, 0:w], kb.P(bh, 0, w), AF.Exp, scale=-1.0, bias=NBIAS[kv][:, 0:1])
                        kb.ts(EHS[:, 0:w], EHS[:, 0:w], 1.0, ALU.add)
                        kb.recip(EHS[:, 0:w], EHS[:, 0:w])
                        kb.stt(hb[:, 0:w], kb.P(bh, 0, w), BIAS[kv][:, 0:1], EHS[:, 0:w], ALU.add, ALU.mult)

                def part2(ci):
                    w = wof(ci)
                    if kv == 0:
                        bkk = nb()
                        for g in range(2):
                            kb.mm(kb.P(bkk, 0, w, g * 64, g * 64 + 64), W2[0][:, :], HB2[ci % 2][g][:, 0:w], start=True, stop=True)
                        ps = kb.P(bkk, 0, w)
                        kb.act(SQb_[:, 0:w], ps, AF.Square)
                        b2 = nb()
                        kb.mm(kb.P(b2, 0, w), blk, SQb_[:, 0:w], start=True, stop=True)
                        kb.act(S3_[:, 0:w], kb.P(b2, 0, w), AF.Ln, scale=1.0 / HD, bias=EPS)
                        kb.act(S3_[:, 0:w], S3_[:, 0:w], AF.Exp, scale=-0.5)
                        s1 = S1B[ci % 2]
                        kb.stt(s1[:, 0:w], ps, VEC[:, NV_QN + 1:NV_QN + 2], S3_[:, 0:w], ALU.mult, ALU.mult)
                        kb.act(QNB2[ci % 2][:, 0:w], s1[:, 0:w], AF.Copy)
                    else:
                        for g in range(2):
                            b2 = nb()
                            kb.mm(kb.P(b2, 0, 64, 0, w), HB2[ci % 2][g][:, 0:w], W2[1][:, :], start=True, stop=True)
                            kb.copy(VCS[b_][g][0:w, ci, 0:64], kb.P(b2, 0, 64, 0, w))

                def part3(ci):
                    if kv != 0:
                        return
                    w = wof(ci)
                    hi_ = ci + 8 * (w - 1) + 1
                    s1 = S1B[ci % 2]
                    b3 = nb()
                    kb.mm(kb.P(b3, 0, w), pmat, QNB2[ci % 2][:, 0:w], start=True, stop=True)
                    kb.tt(S2_[:, 0:w], kb.P(b3, 0, w), ROPC[:, 1, ci:hi_:8], ALU.mult)
                    kb.tt(s1[:, 0:w], s1[:, 0:w], ROPC[:, 0, ci:hi_:8], ALU.mult)
                    kb.tt(KCS[b_][:, ci:hi_:8], s1[:, 0:w], S2_[:, 0:w], ALU.add)

                def step(k):
                    if 0 <= k - 4 <= 7:
                        part3(k - 4)
                    if 0 <= k - 3 <= 7:
                        part2(k - 3)
                    if 0 <= k - 2 <= 7:
                        part1(k - 2)

                for q4 in range(4):
                    ga = GA[ga_rr[0] % 2]
                    ga_rr[0] += 1
                    idxv = IDX[:, b_, q4:q4 + 1]
                    kb.dma("pool", ga[:, :], idxv, fn=lambda e, ga=ga, idxv=idxv, kv=kv: e.indirect_dma_start(
                        out=ga[:, :].ap, out_offset=None, in_=d_pools[kv], in_offset=bass.IndirectOffsetOnAxis(ap=idxv.ap, axis=0)))
                    for cl in range(2):
                        ci = 2 * q4 + cl
                        xt = XT[xt_slot(ci)]
                        for s4 in range(4):
                            bt = nb()
                            for t4 in range(4):
                                t0 = cl * 16 + s4 * 4 + t4
                                kb.transpose(kb.P(bt, t4 * 128, (t4 + 1) * 128), ga[:, t0 * 128:(t0 + 1) * 128], IDF[:, :])
                            kb.copy(sb_view(xt, xt.full[:, s4 * 4:s4 * 4 + 4, :]),
                                    View(kb.psum[:, bt, :].rearrange("p (s n) -> p s n", s=4), kb.ps, bt * 2048, bt * 2048 + 2048),
                                    eng=("act" if s4 % 2 else "dve"))
                        step(ci + 1)
                for k in range(9, 13):
                    step(k)

        reserved.add(7)
        for b_ in range(NS):
            for g in range(2):
                gp = slice(g * 64, (g + 1) * 64)
                bgi = g * NS + b_
                a = nb()
                for ci in range(8):
                    kb.mm(kb.P(a, ci * 4, ci * 4 + 4), KCS[b_][gp, ci:1024:8], QSF[gp, :, b_], start=(ci == 0), stop=True)
                kb.act(ETS[:, :], kb.P(a, 0, 32), AF.Exp, scale=SCALE)
                kb.tt(ETS[:, :], ETS[:, :], sconst(SC_MK, 32), ALU.mult)
                o1, o2 = nb(), nb()
                for ci in range(8):
                    kb.mm(kb.P(o1, 0, 65, 0, 4), ETS[:, ci * 4:ci * 4 + 4], VCS[b_][g][:, ci, :], start=(ci == 0), stop=(ci == 7))
                for ci in range(8):
                    kb.mm(kb.P(o2, 0, 257, 0, 4), ETS[:, ci * 4:ci * 4 + 4], OVS[:, ci, :], start=(ci == 0), stop=(ci == 7))
                kb.ts(RDS[0:4, :], kb.P(o1, 64, 65, 0, 4), 1e-30, ALU.max)
                kb.recip(RDS[0:4, :], RDS[0:4, :])
                kb.ts(OCS[0:4, :], kb.P(o1, 0, 64, 0, 4), RDS[0:4, 0:1], ALU.mult)
                kb.dma("sp", kb.dview("sc_oc", sc_oc[b_, g]), OCS[0:4, :])
                kb.copy(IMPR[0:4, :], kb.P(o2, 0, 257, 0, 4))
                kb.ts(LSEL[0:4, :], sconst(SC_SELC + bgi * 8, 8, 0, 4), RDS[0:4, 0:1], ALU.mult)
                kb.mm(kb.P(7, 0, 257, 0, 8), LSEL[0:4, :], IMPR[0:4, :], start=(b_ == 0 and g == 0), stop=(b_ == NS - 1 and g == 1))
        kb.tt(SCOS[0:8, :], kb.P(7, 0, 256, 0, 8), sconst(SC_BONS, 256, 0, 8), ALU.add)
        reserved.discard(7)
        kb.op("dve", lambda e: e.max(out=M8S[0:8, 0:8].ap, in_=SCOS[0:8, :].ap), reads=[SCOS[0:8, :]], writes=[M8S[0:8, 0:8]])
        kb.op("dve", lambda e: e.max_index(out=IXS[0:8, 0:8].ap, in_max=M8S[0:8, 0:8].ap, in_values=SCOS[0:8, :].ap),
              reads=[SCOS[0:8, :], M8S[0:8, 0:8]], writes=[IXS[0:8, 0:8]])
        kb.op("dve", lambda e: e.match_replace(out=SC2S[0:8, :].ap, in_to_replace=M8S[0:8, 0:8].ap, in_values=SCOS[0:8, :].ap, imm_value=-3.0e38),
              reads=[SCOS[0:8, :], M8S[0:8, 0:8]], writes=[SC2S[0:8, :]])
        kb.op("dve", lambda e: e.max(out=M8S[0:8, 8:16].ap, in_=SC2S[0:8, :].ap), reads=[SC2S[0:8, :]], writes=[M8S[0:8, 8:16]])
        kb.op("dve", lambda e: e.max_index(out=IXS[0:8, 8:16].ap, in_max=M8S[0:8, 8:16].ap, in_values=SC2S[0:8, :].ap),
              reads=[SC2S[0:8, :], M8S[0:8, 8:16]], writes=[IXS[0:8, 8:16]])
        kb.dma("sp", kb.dview("sc_idx", sc_idx.rearrange("(a r) -> a r", r=16)), IXS[0:8, :])
        kb.dma("sp", JI[:, :], kb.dview("sc_idx", sc_idx.rearrange("(p o) -> p o", o=1)))
        kb.dma("sp", PTBI[:, :], View(d_ptB, None, 0, 0))
        kb.copy(PTBF[:, :], PTBI[:, :])
        kb.copy(JF[:, 0:1], JI[:, :])
        ptb3 = sb_view(PTBF, PTBF.full[:, :].unsqueeze(2).to_broadcast([128, 128, 2]))
        tab3 = sb_view(TAB, TAB.full[:, :].rearrange("p (a h) -> p a h", h=2))
        kb.ts(tab3, ptb3, 4.0, ALU.mult)
        h2b = sb_view(SCN, SCN.full[:, SC_H2:SC_H2 + 2].unsqueeze(1).to_broadcast([128, 128, 2]))
        kb.tt(tab3, tab3, h2b, ALU.add)
        kb.ts(OH[:, :], sconst(SC_IOTA, 256), JF[:, 0:1], ALU.is_equal)
        kb.tt(OH[:, :], OH[:, :], TAB[:, :], ALU.mult)
        kb.reduce(JF[:, 5:6], OH[:, :])
        for pc in range(2):
            kb.ts(IDX2F[:, pc:pc + 1], JF[:, 5:6], float(pc), ALU.add)
        kb.copy(IDX2[:, :], IDX2F[:, :])

        if debug == "samp":
            o_d1 = dout("d_kcs", [128, 1024])
            o_d2 = dout("d_ixs", [8, 16], U32)
            o_d3 = dout("d_imp", [8, 256])
            o_d4 = dout("d_idx2", [128, 2], I32)
            kb.copy(GA[0][:, 0:1024], KCS[0][:, :])
            kb.dma("sp", kb.dview("d1", o_d1), GA[0][:, 0:1024])
            kb.dma("sp", kb.dview("d2", o_d2), IXS[0:8, :])
            kb.dma("sp", kb.dview("d3", o_d3), SCOS[0:8, :])
            kb.dma("sp", kb.dview("d4", o_d4), IDX2[:, :])
        bq = nb()
        for g in range(2):
            kb.mm(kb.P(bq, 0, 256, g * 64, g * 64 + 64), sconst(SC_INDB + g * 64, 64, 0, NS), QTOK[0:NS, :, g, :], start=True, stop=True)
        kb.copy(QB[:, :, :], View(kb.psum[:, bq, 0:256].rearrange("p (h d) -> p h d", h=4), kb.ps, bq * 2048, bq * 2048 + 2048))
        GW = [Tile(kb, "GWk", [32, 64], F32, GA[0].off), Tile(kb, "GWv", [32, 64], F32, GA[1].off)]
        TMPD = Tile(kb, "TMPD", [32, 64], F32, KCS[0].off)
        assert TMPD.nbytes <= 4 * KCS[0].nbytes

        TMPD2 = Tile(kb, "TMPD2", [32, 64], F32, VCS[0][0].off)
        assert TMPD2.nbytes <= 8 * VCS[0][0].nbytes

        def dve_attend(kview_fn, vview_fn, halves, part, mask_fn):
            for gp_, g in halves:
                kv_ = kview_fn(gp_, g)
                n = gp_.stop - gp_.start
                for hh in range(4):
                    tm, eng = (TMPD2, "pool") if hh % 2 else (TMPD, "dve")
                    qb = sb_view(QB, QB.full[gp_, hh, :].unsqueeze(1).to_broadcast([n, 32, 64]))
                    kb.tt(tm[gp_, :, :], kv_, qb, ALU.mult, eng=eng)
                    kb.reduce(SCD[gp_, hh, :], tm[gp_, :, :])
            kb.act(ED[:, :, :], SCD[:, :, :], AF.Exp, scale=SCALE)
            mask_fn()
            kb.reduce(sb_view(part, part.full[:, :, 64]), ED[:, :, :])
            for gp_, g in halves:
                vv_ = vview_fn(gp_, g)
                n = gp_.stop - gp_.start
                for hh in range(4):
                    tm, eng = (TMPD2, "pool") if hh % 2 else (TMPD, "dve")
                    eb = sb_view(ED, ED.full[gp_, hh, :].unsqueeze(1).to_broadcast([n, 64, 32]))
                    tv = sb_view(tm, tm.full[gp_, :, :].rearrange("p t d -> p d t"))
                    kb.tt(tv, vv_, eb, ALU.mult, eng=eng)
                    kb.reduce(part[gp_, hh, 0:64], tv)

        def finish_branch(part, knew, va_new, bi):
            for g in range(2):
                bn = nb()
                kb.mm(kb.P(bn, 0, 260, 0, NS), sconst(SC_IND2, NS, g * 64, g * 64 + 64), sb_view(part, part.full[g * 64:g * 64 + 64, :, :]),
                      start=True, stop=True)
                kb.copy(NUM[0:NS, g * 4:g * 4 + 4, :], View(kb.psum[0:NS, bn, 0:260].rearrange("p (h w) -> p h w", h=4), kb.ps, bn * 2048, bn * 2048 + 2048))
            knb = sb_view(knew, knew.full[0:NS, :, :].unsqueeze(1).to_broadcast([NS, 4, 2, 64]))
            kb.tt(sb_view(TMPQ, TMPQ.full[0:NS, :, :].rearrange("p (c g) d -> p c g d", c=4)), QTOK[0:NS, :, :, :], knb, ALU.mult)
            kb.reduce(SNE[0:NS, :], TMPQ[0:NS, :, :])
            kb.act(SNE[0:NS, :], SNE[0:NS, :], AF.Exp, scale=SCALE)
            en = sb_view(SNE, SNE.full[0:NS, :].rearrange("p (c g) -> p g c", c=4))
            num3 = sb_view(NUM, NUM.full[0:NS, :, 64].rearrange("p (g c) -> p g c", g=2))
            kb.tt(num3, num3, en, ALU.add)
            vnb = sb_view(va_new, va_new.full[0:NS, :, :].unsqueeze(2).to_broadcast([NS, 2, 4, 64]))
            enb = sb_view(SNE, SNE.full[0:NS, :].rearrange("p (c g) -> p g c", c=4).unsqueeze(3).to_broadcast([NS, 2, 4, 64]))
            t4 = sb_view(TMPQ, TMPQ.full[0:NS, :, :].rearrange("p (g c) d -> p g c d", g=2))
            kb.tt(t4, vnb, enb, ALU.mult)
            kb.tt(NUM[0:NS, :, 0:64], NUM[0:NS, :, 0:64], TMPQ[0:NS, :, :], ALU.add)
            kb.recip(WG8[0:NS, :], sb_view(NUM, NUM.full[0:NS, :, 64]))
            kb.tt(WG8[0:NS, :], WG8[0:NS, :], sb_view(GT, GT.full[0:NS, 16, bi:24:3]), ALU.mult)
            wgb = sb_view(WG8, WG8.full[0:NS, :].unsqueeze(2).to_broadcast([NS, 8, 64]))
            kb.tt(TMPQ[0:NS, :, :], NUM[0:NS, :, 0:64], wgb, ALU.mult)
            o3 = sb_view(OTOK, OTOK.full[0:NS, 0, :].rearrange("p (h d) -> p h d", h=8))
            kb.tt(o3, o3, TMPQ[0:NS, :, :], ALU.add)

        kb.dma("sp", OCT[0:NS, :, :], kb.dview("sc_oc", sc_oc.rearrange("b g h d -> b (g h) d")))
        g0b = sb_view(GT, GT.full[0:NS, 16, 0:24:3].unsqueeze(2).to_broadcast([NS, 8, 64]))
        kb.tt(sb_view(OTOK, OTOK.full[0:NS, 0, :].rearrange("p (h d) -> p h d", h=8)), OCT[0:NS, :, :], g0b, ALU.mult)

        dbg_n = [0]
        def dump_otok():
            if debug == "samp":
                o_ = dout("d_otok%d" % dbg_n[0], [NS, 512])
                dbg_n[0] += 1
                kb.dma("sp", kb.dview("dotok%d" % dbg_n[0], o_), OTOK[0:NS, 0, :])
        dump_otok()
        all_halves = [(slice(0, 64), 0), (slice(64, 128), 1)]
        for pc in range(2):
            idxv = IDX2[:, pc:pc + 1]
            for t_, pool_i in ((GA[0], 2), (GA[1], 3)):
                kb.dma("pool", t_[:, :], idxv, fn=lambda e, t_=t_, idxv=idxv, pool_i=pool_i: e.indirect_dma_start(
                    out=t_[:, :].ap, out_offset=None, in_=d_pools[pool_i], in_offset=bass.IndirectOffsetOnAxis(ap=idxv.ap, axis=0)))
            kfn = lambda gp_, g: sb_view(GA[0], GA[0].full[gp_, :].rearrange("p (t g d) -> p t g d", g=2, d=64)[:, :, g, :])
            vfn = lambda gp_, g: sb_view(GA[1], GA[1].full[gp_, :].rearrange("p (t g d) -> p d g t", g=2, d=64)[:, :, g, :])
            mfn = lambda: kb.ts(ED[:, :, :], ED[:, :, :], sconst(SC_MSKR, 1), ALU.mult)
            dve_attend(kfn, vfn, all_halves, PARTS[pc], mfn)
        kb.tt(PARTS[0][:, :, :], PARTS[0][:, :, :], PARTS[1][:, :, :], ALU.add)
        finish_branch(PARTS[0], KNS, VNS, 1)
        dump_otok()

        for kv in range(2):
            for g in range(2):
                src = d_wins[kv].rearrange("b (c t) (g d) -> g (b c) t d", c=16, g=2)[g]
                kb.dma("sp", GW[kv][g * 64:(g + 1) * 64, :, :], View(src, None, 0, 0))
        kfn = lambda gp_, g: GW[0][gp_, :, :]
        vfn = lambda gp_, g: sb_view(GW[1], GW[1].full[gp_, :, :].rearrange("p t d -> p d t"))
        def mfn_w():
            mb = sb_view(SCN, SCN.full[:, SC_MSKW:SC_MSKW + 32].unsqueeze(1).to_broadcast([128, 4, 32]))
            kb.tt(ED[:, :, :], ED[:, :, :], mb, ALU.mult)
        dve_attend(kfn, vfn, [(slice(0, 128), 0)], PARTS[1], mfn_w)
        finish_branch(PARTS[1], KNW, VNW, 2)
        dump_otok()
        out_norm_to_H(NS, 1, SEQ)

    def mixer_out():
        for half in range(2):
            kb.dma("pool", WA[half][:, :, :], View(d_wout[half], None, 0, 0))
        for half in range(2):
            for dch in range(4):
                dk = half * 4 + dch
                for (c0, w) in COLT:
                    b = nb()
                    for kc in range(8):
                        rhs = CN[:, kc, c0:c0 + w] if kc < 4 else H[:, kc, c0:c0 + w]
                        kb.mm(kb.P(b, 0, w), WA[half][:, kc, dch * 128:(dch + 1) * 128], rhs, start=(kc == 0), stop=(kc == 7))
                    kb.tt(X[:, dk, c0:c0 + w], X[:, dk, c0:c0 + w], kb.P(b, 0, w), ALU.add)

    _sample_attention()
    mixer_out()
    ffn(1, NV_FFN2)
    A7 = Alloc(kb, SCR)
    rmsnorm(A7, X, lambda k, c0, w: X[:, k, c0:c0 + w], NV_FIN)
    for k in range(8):
        kb.dma("sp", kb.dview("yT", o_y[:, k, :]), X[:, k, :])
    kb.finish()
    return nc


def _tile_up(w):
    w = w.reshape(8, 128, 2, 11, 256)
    return np.ascontiguousarray(w.transpose(3, 1, 0, 2, 4))


def _tile_dn(w):
    w = w.reshape(22, 128, 4, 256)
    return np.ascontiguousarray(w.transpose(2, 1, 0, 3))


def _fm(v, nchunk):
    return np.ascontiguousarray(v.reshape(nchunk, 128).T)


def prep_shared(inp):
    f = lambda k: np.asarray(inp[k], dtype=np.float32)[0]
    sh = {}
    sh["ffn1_up"] = _tile_up(f("ffn1_w_in"))
    sh["ffn2_up"] = _tile_up(f("ffn2_w_in"))
    sh["ffn1_dn"] = _tile_dn(f("ffn1_w_out"))
    sh["ffn2_dn"] = _tile_dn(f("ffn2_w_out"))
    w_in = f("w_in")
    cols = list(range(1024))
    for c in range(4):
        cols += list(range(1024 + c * 64, 1024 + c * 64 + 64)) + list(range(1024 + (4 + c) * 64, 1024 + (4 + c) * 64 + 64))
    cols += list(range(1536, N_IN))
    wp = np.zeros((1024, 2560), np.float32)
    wp[:, :N_IN] = w_in[:, cols]
    sh["w_in_t"] = np.ascontiguousarray(wp.reshape(8, 128, 5, 512).transpose(2, 1, 0, 3))
    sh["w_out_t"] = np.ascontiguousarray(f("w_out").reshape(8, 128, 2, 512).transpose(2, 1, 0, 3))
    vec = np.zeros((128, NV_TOT), np.float32)
    vec[:, NV_FFN1:NV_FFN1 + 8] = _fm(f("ffn1_norm"), 8)
    vec[:, NV_MIX:NV_MIX + 8] = _fm(f("mix_norm"), 8)
    vec[:, NV_FFN2:NV_FFN2 + 8] = _fm(f("ffn2_norm"), 8)
    vec[:, NV_FIN:NV_FIN + 8] = _fm(f("final_norm"), 8)
    cw = f("conv_w")
    vec[:, NV_CONVW:NV_CONVW + 124] = cw.reshape(31, 4, 128).transpose(2, 1, 0).reshape(128, 124)
    vec[:, NV_CONVB:NV_CONVB + 4] = _fm(f("conv_b"), 4)
    vec[:, NV_LNG:NV_LNG + 4] = _fm(f("conv_ln_g"), 4)
    vec[:, NV_LNB:NV_LNB + 4] = _fm(f("conv_ln_b"), 4)
    vec[:, NV_ONC:NV_ONC + 4] = _fm(f("out_norm_conv"), 4)
    vec[:, NV_ONA:NV_ONA + 4] = _fm(f("out_norm_attn"), 4)
    for i, k in enumerate(["q_norm", "k_cmp_norm", "k_sel_norm", "k_win_norm"]):
        vec[:, NV_QN + i] = np.tile(f(k), 2)
    sh["vec"] = vec
    sh["cb"] = _consts()
    sh["idf"] = np.eye(128, dtype=np.float32)
    pos = np.concatenate([np.arange(SEQ), np.full(NS, PAST)]).astype(np.float32)
    sh["rope_tok"] = _rope_tab(pos)
    sh["rope_cmp"] = _rope_tab((np.arange(1024) * 16 + 31).astype(np.float32))
    sh["bonus_p"] = _bonus_prompt()
    for nm, key in (("cmpk", "cmp_k"), ("cmpv", "cmp_v")):
        w1 = f(key + "_w1")
        a = np.ascontiguousarray(w1.transpose(1, 0, 2))
        sh[nm + "_w1a"] = np.concatenate([a, a], axis=0)
        pe = np.ascontiguousarray(f(key + "_pos").T)
        sh[nm + "_pea"] = np.concatenate([pe, pe], axis=0)
        sh[nm + "_w2"] = f(key + "_w2")
    sh["sconst"] = _sconst()
    sh["ovs"] = _ovs()
    for nm, key in (("pool_kc", "cache_k_cmp"), ("pool_vc", "cache_v_cmp"), ("pool_ks", "cache_k_sel"), ("pool_vs", "cache_v_sel")):
        sh[nm] = np.asarray(inp[key], dtype=np.float32).reshape(N_POOL * 4, 4096)
    return sh


def prep_core(inp, c):
    xp = np.asarray(inp["x_prompt"], dtype=np.float32)[c]
    xs = np.asarray(inp["x_sample"], dtype=np.float32)[NS * c:NS * c + NS, 0]
    x = np.concatenate([xp, xs], axis=0)
    m = {"xT": np.ascontiguousarray(x.T.reshape(8, 128, TT).transpose(1, 0, 2))}
    sc = np.asarray(inp["state_conv"], dtype=np.float32)[0, NS * c:NS * c + NS]
    m["sconvT"] = np.ascontiguousarray(sc.reshape(NS, 30, 4, 128).transpose(3, 2, 0, 1))
    pt = np.asarray(inp["page_table"], dtype=np.int32)[NS * c:NS * c + NS]
    m["ptT"] = np.ascontiguousarray(pt.T)
    p = np.arange(128)
    m["ptB"] = np.ascontiguousarray(pt[(p // 16) % 4])
    m["win_k"] = np.asarray(inp["state_k_win"], dtype=np.float32)[0, NS * c:NS * c + NS].reshape(NS, 512, 128)
    m["win_v"] = np.asarray(inp["state_v_win"], dtype=np.float32)[0, NS * c:NS * c + NS].reshape(NS, 512, 128)
    return m


def assemble(results):
    n = len(results)
    yp, ys = [], []
    kv = [[] for _ in range(6)]
    skv = [[] for _ in range(4)]
    pw = [[], []]
    sw = [[], []]
    pconv, sconv = [], []
    for r in results:
        yT = np.asarray(r["yT"])
        y = yT.transpose(2, 1, 0).reshape(TT, D_MODEL)
        yp.append(y[:SEQ])
        ys.append(y[SEQ:])
        kvT = np.asarray(r["kvT"])
        for i in range(6):
            t = kvT[:, i, :].T
            if i < 4:
                kv[i].append(t[:SEQ].reshape(SEQ, 2, HD))
                skv[i].append(t[SEQ:].reshape(NS, 1, 2, HD))
            else:
                pw[i - 4].append(t[SEQ - 512:SEQ].reshape(512, 2, HD))
        for i in range(2):
            sw[i].append(np.asarray(r["swin_k" if i == 0 else "swin_v"]).reshape(NS, 512, 2, HD))
        pconv.append(np.asarray(r["pconvT"]).transpose(2, 1, 0).reshape(30, 512))
        sconv.append(np.asarray(r["sconvT_out"]).transpose(2, 3, 1, 0).reshape(NS, 30, 512))
    f32 = lambda a: np.ascontiguousarray(a, dtype=np.float32)
    outs = [f32(np.stack(yp)), f32(np.concatenate(ys)[:, None, :])]
    outs += [f32(np.stack(kv[i])[None]) for i in range(4)]
    outs += [f32(np.stack(pw[i])[None]) for i in range(2)]
    outs += [f32(np.stack(pconv)[None])]
    outs += [f32(np.concatenate(skv[i])[None]) for i in range(4)]
    outs += [f32(np.concatenate(sw[i])[None]) for i in range(2)]
    outs += [f32(np.concatenate(sconv)[None])]
    return tuple(outs)


def kernel(**inputs):
    n = 8
    sh = prep_shared(inputs)
    in_maps = []
    for c in range(n):
        m = dict(sh)
        m.update(prep_core(inputs, c))
        in_maps.append(m)
    nc = build_program()
    res = run_bass_kernel_spmd(nc, in_maps, core_ids=list(range(n)))
    return assemble(res.results)
```
